# Optimizing a Trainium2 kernel written in Bass

```python
import math
import jax, jax.numpy as jnp
from jax import lax
import numpy as np

D_MODEL = 1024
BATCH = 2
SEQ = 16384
DEPTH = 2

HEAD_DIM = 64
N_HEADS_A = 8
N_HEADS_B = 8
D_A = N_HEADS_A * HEAD_DIM
D_B = N_HEADS_B * HEAD_DIM
N_IDX_HEADS = 8
IDX_DIM = 64
D_RNN = 512
N_RNN_BLOCKS = 8
RNN_BLOCK = D_RNN // N_RNN_BLOCKS
CONV_WIDTH = 4
LRU_C = 8.0
D_FF = 4 * D_MODEL
MOBA_BLOCK = 256
MOBA_TOPK = 3
DSA_MAX_TOPK = 256
Q_BLOCK = 128
N_BUCKETS = 32
MAX_DISTANCE = 128
N_BRANCHES = 3
N_ATTN_HEADS = N_HEADS_A + N_HEADS_B
EPS = 1e-6
NEG = -1e30

IN_SPLITS = (D_A, D_A, D_A,
             D_B, D_B, D_B,
             N_IDX_HEADS * IDX_DIM,
             IDX_DIM,
             N_IDX_HEADS,
             D_RNN, D_RNN,
             N_BRANCHES * D_MODEL)
D_IN = sum(IN_SPLITS)
IN_OFFSETS = tuple(int(o) for o in np.cumsum(IN_SPLITS)[:-1])

kernel_name = "hybrid_moba_dsa_rglru_block"


def rms_norm(x, g):
    xf = x.astype(jnp.float32)
    y = xf * lax.rsqrt(jnp.mean(xf * xf, axis=-1, keepdims=True) + EPS)
    return (y * g.astype(jnp.float32)).astype(x.dtype)


def t5_bucket(dist):
    n = jnp.maximum(dist, 0)
    max_exact = N_BUCKETS // 2
    nf = jnp.maximum(n, 1).astype(jnp.float32)
    large = max_exact + (jnp.log(nf / max_exact) / math.log(MAX_DISTANCE / max_exact)
                         * (N_BUCKETS - max_exact)).astype(jnp.int32)
    large = jnp.minimum(large, N_BUCKETS - 1)
    return jnp.where(n < max_exact, n, large)


def moba_attention(q, k, v, bias_tab):
    B, S, H, dh = q.shape
    nb = -(-S // MOBA_BLOCK)
    pad = nb * MOBA_BLOCK - S
    kp = jnp.pad(k, ((0, 0), (0, pad), (0, 0), (0, 0)))
    vp = jnp.pad(v, ((0, 0), (0, pad), (0, 0), (0, 0)))
    kblk = kp.reshape(B, nb, MOBA_BLOCK, H, dh).transpose(0, 3, 1, 2, 4)
    vblk = vp.reshape(B, nb, MOBA_BLOCK, H, dh).transpose(0, 3, 1, 2, 4)
    kmean = jnp.mean(kblk, axis=3)
    n_sel = min(MOBA_TOPK, nb)
    scale = dh ** -0.5
    b_ix = jnp.arange(B)[:, None, None, None]
    h_ix = jnp.arange(H)[None, None, :, None]
    blk_ids = jnp.arange(nb)
    bias_t = bias_tab.T

    def one_block(qb):
        t0 = qb * Q_BLOCK
        cur = t0 // MOBA_BLOCK
        t = t0 + jnp.arange(Q_BLOCK)
        qq = lax.dynamic_slice_in_dim(q, t0, Q_BLOCK, axis=1)
        gate = jnp.einsum('bqhd,bhnd->bqhn', qq, kmean).astype(jnp.float32)
        gate = jnp.where(blk_ids < cur, gate, NEG)
        _, sel = lax.top_k(gate, n_sel)
        sel_ok = sel < cur
        kg = kblk[b_ix, h_ix, sel]
        vg = vblk[b_ix, h_ix, sel]
        s_past = jnp.einsum('bqhd,bqhnkd->bqhnk', qq, kg).astype(jnp.float32) * scale
        pos_past = sel[..., None] * MOBA_BLOCK + jnp.arange(MOBA_BLOCK)
        bias_past = bias_t[h_ix[..., None], t5_bucket(t[None, :, None, None, None] - pos_past)]
        s_past = jnp.where(sel_ok[..., None], s_past + bias_past, NEG)
        k_own = lax.dynamic_slice_in_dim(kp, cur * MOBA_BLOCK, MOBA_BLOCK, axis=1)
        v_own = lax.dynamic_slice_in_dim(vp, cur * MOBA_BLOCK, MOBA_BLOCK, axis=1)
        rel = t[:, None] - (cur * MOBA_BLOCK + jnp.arange(MOBA_BLOCK))[None, :]
        bias_own = bias_tab[t5_bucket(rel)].transpose(0, 2, 1)[None]
        s_own = jnp.einsum('bqhd,bkhd->bqhk', qq, k_own).astype(jnp.float32) * scale + bias_own
        s_own = jnp.where((rel >= 0)[None, :, None, :], s_own, NEG)
        logits = jnp.concatenate([s_past.reshape(B, Q_BLOCK, H, n_sel * MOBA_BLOCK), s_own], axis=-1)
        p = jax.nn.softmax(logits, axis=-1).astype(v.dtype)
        p_past = p[..., :n_sel * MOBA_BLOCK].reshape(B, Q_BLOCK, H, n_sel, MOBA_BLOCK)
        p_own = p[..., n_sel * MOBA_BLOCK:]
        return (jnp.einsum('bqhnk,bqhnkd->bqhd', p_past, vg)
                + jnp.einsum('bqhk,bkhd->bqhd', p_own, v_own))

    out = lax.map(one_block, jnp.arange(S // Q_BLOCK))
    return out.transpose(1, 0, 2, 3, 4).reshape(B, S, H * dh)


def dsa_attention(q, k, v, qi, ki, wi, bias_tab):
    B, S, H, dh = q.shape
    n_keep = min(DSA_MAX_TOPK, S // 4)
    scale = dh ** -0.5
    idx_scale = (N_IDX_HEADS ** -0.5) * (IDX_DIM ** -0.5)
    s_all = jnp.arange(S)

    def one_block(qb):
        t0 = qb * Q_BLOCK
        t = t0 + jnp.arange(Q_BLOCK)
        qq = lax.dynamic_slice_in_dim(q, t0, Q_BLOCK, axis=1)
        qiq = lax.dynamic_slice_in_dim(qi, t0, Q_BLOCK, axis=1)
        wq = lax.dynamic_slice_in_dim(wi, t0, Q_BLOCK, axis=1)
        dots = jnp.einsum('bqhd,bsd->bqhs', qiq, ki)
        score = jnp.einsum('bqh,bqhs->bqs', wq, jax.nn.relu(dots)).astype(jnp.float32) * idx_scale
        score = jnp.where((s_all[None, :] <= t[:, None])[None], score, NEG)
        _, idx = lax.top_k(score, n_keep)
        ok = idx <= t[None, :, None]
        kg = jax.vmap(lambda kk, ii: kk[ii])(k, idx)
        vg = jax.vmap(lambda vv, ii: vv[ii])(v, idx)
        bias = bias_tab[t5_bucket(t[None, :, None] - idx)].transpose(0, 1, 3, 2)
        logits = jnp.einsum('bqhd,bqkhd->bqhk', qq, kg).astype(jnp.float32) * scale + bias
        logits = jnp.where(ok[:, :, None, :], logits, NEG)
        p = jax.nn.softmax(logits, axis=-1).astype(v.dtype)
        return jnp.einsum('bqhk,bqkhd->bqhd', p, vg)

    out = lax.map(one_block, jnp.arange(S // Q_BLOCK))
    return out.transpose(1, 0, 2, 3, 4).reshape(B, S, H * dh)


def causal_depthwise_conv(x, w, b):
    S = x.shape[1]
    xp = jnp.pad(x, ((0, 0), (CONV_WIDTH - 1, 0), (0, 0)))
    y = b
    for i in range(CONV_WIDTH):
        y = y + w[i] * xp[:, i:i + S]
    return y


def rg_lru(x, w_r, b_r, w_i, b_i, lam):
    B, S, C = x.shape
    xb = x.reshape(B, S, N_RNN_BLOCKS, RNN_BLOCK)
    r = jax.nn.sigmoid(jnp.einsum('bsnc,ncd->bsnd', xb, w_r).reshape(B, S, C) + b_r)
    i = jax.nn.sigmoid(jnp.einsum('bsnc,ncd->bsnd', xb, w_i).reshape(B, S, C) + b_i)
    log_a = -LRU_C * r.astype(jnp.float32) * jax.nn.softplus(-lam.astype(jnp.float32))
    a = jnp.exp(log_a)
    u = jnp.sqrt(-jnp.expm1(2.0 * log_a)) * (i * x).astype(jnp.float32)

    def combine(left, right):
        a1, b1 = left
        a2, b2 = right
        return a1 * a2, a2 * b1 + b2

    _, h = lax.associative_scan(combine, (a, u), axis=1)
    return h.astype(x.dtype)


def token_mixer(xn, w_in, conv_w, conv_b, w_r, b_r, w_i, b_i, lam, w_pa, w_pb, w_pc, w_o, rel_bias):
    B, S, _ = xn.shape
    proj = xn @ w_in
    (qa, ka, va, qb, kb, vb, qi, ki, wi, xc, yc, gates) = jnp.split(proj, IN_OFFSETS, axis=-1)
    heads_a = lambda z: z.reshape(B, S, N_HEADS_A, HEAD_DIM)
    heads_b = lambda z: z.reshape(B, S, N_HEADS_B, HEAD_DIM)
    y_a = moba_attention(heads_a(qa), heads_a(ka), heads_a(va), rel_bias[:, :N_HEADS_A])
    y_b = dsa_attention(heads_b(qb), heads_b(kb), heads_b(vb),
                        qi.reshape(B, S, N_IDX_HEADS, IDX_DIM), ki, wi, rel_bias[:, N_HEADS_A:])
    y_c = rg_lru(causal_depthwise_conv(xc, conv_w, conv_b), w_r, b_r, w_i, b_i, lam) * jax.nn.gelu(yc)
    g = jax.nn.sigmoid(gates).reshape(B, S, N_BRANCHES, D_MODEL)
    merged = g[:, :, 0] * (y_a @ w_pa) + g[:, :, 1] * (y_b @ w_pb) + g[:, :, 2] * (y_c @ w_pc)
    return merged @ w_o


def squared_relu_mlp(xn, w_up, w_down):
    return jnp.square(jax.nn.relu(xn @ w_up)) @ w_down


def setup_inputs(seed: int = 0) -> dict:
    key = jax.random.key(seed)
    ks = jax.random.split(key, 20)
    f32 = jnp.float32
    nrm = lambda k, shape, s: jax.random.normal(k, shape, f32) * s
    u = jax.random.uniform(ks[10], (DEPTH, D_RNN), f32, minval=0.9, maxval=0.999)
    a = u ** (1.0 / LRU_C)
    lam = jnp.log(a) - jnp.log1p(-a)
    return {
        "x": nrm(ks[0], (BATCH, SEQ, D_MODEL), 1.0),
        "rel_bias": nrm(ks[1], (N_BUCKETS, N_ATTN_HEADS), 0.5),
        "norm_mix_g": 1.0 + nrm(ks[2], (DEPTH, D_MODEL), 0.05),
        "w_in": nrm(ks[3], (DEPTH, D_MODEL, D_IN), D_MODEL ** -0.5),
        "conv_w": nrm(ks[4], (DEPTH, CONV_WIDTH, D_RNN), CONV_WIDTH ** -0.5),
        "conv_b": nrm(ks[5], (DEPTH, D_RNN), 0.02),
        "w_r": nrm(ks[6], (DEPTH, N_RNN_BLOCKS, RNN_BLOCK, RNN_BLOCK), RNN_BLOCK ** -0.5),
        "b_r": nrm(ks[7], (DEPTH, D_RNN), 0.1),
        "w_i": nrm(ks[8], (DEPTH, N_RNN_BLOCKS, RNN_BLOCK, RNN_BLOCK), RNN_BLOCK ** -0.5),
        "b_i": nrm(ks[9], (DEPTH, D_RNN), 0.1),
        "lru_lambda": lam,
        "w_pa": nrm(ks[11], (DEPTH, D_A, D_MODEL), D_A ** -0.5),
        "w_pb": nrm(ks[12], (DEPTH, D_B, D_MODEL), D_B ** -0.5),
        "w_pc": nrm(ks[13], (DEPTH, D_RNN, D_MODEL), D_RNN ** -0.5),
        "w_o": nrm(ks[14], (DEPTH, D_MODEL, D_MODEL), D_MODEL ** -0.5),
        "norm_mlp_g": 1.0 + nrm(ks[15], (DEPTH, D_MODEL), 0.05),
        "w_up": nrm(ks[16], (DEPTH, D_MODEL, D_FF), D_MODEL ** -0.5),
        "w_down": nrm(ks[17], (DEPTH, D_FF, D_MODEL), D_FF ** -0.5),
        "final_norm_g": 1.0 + nrm(ks[18], (D_MODEL,), 0.05),
    }


def reference(x, rel_bias, norm_mix_g, w_in, conv_w, conv_b, w_r, b_r, w_i, b_i, lru_lambda,
              w_pa, w_pb, w_pc, w_o, norm_mlp_g, w_up, w_down, final_norm_g):
    for l in range(DEPTH):
        x = x + token_mixer(rms_norm(x, norm_mix_g[l]), w_in[l], conv_w[l], conv_b[l],
                            w_r[l], b_r[l], w_i[l], b_i[l], lru_lambda[l],
                            w_pa[l], w_pb[l], w_pc[l], w_o[l], rel_bias)
        x = x + squared_relu_mlp(rms_norm(x, norm_mlp_g[l]), w_up[l], w_down[l])
    return rms_norm(x, final_norm_g)
```

```python
import math
import contextlib
import numpy as np
import concourse.bass as bass
import concourse.mybir as mybir
from concourse.bass_utils import run_bass_kernel_spmd

F32 = mybir.dt.float32
BF16 = mybir.dt.bfloat16
AF = mybir.ActivationFunctionType
ALU = mybir.AluOpType
AX = mybir.AxisListType

ENGS = ("pe", "act", "dve", "pool", "sp")
NDSEM = 8


class Buf:
    __slots__ = ("name", "last_w", "readers")

    def __init__(self, name=""):
        self.name = name
        self.last_w = None
        self.readers = []


class Ins:
    __slots__ = ("eng", "fn", "deps", "is_dma", "sig", "sigval", "dsem", "dval", "idx")


class Sched:
    def __init__(self, nc, strict_same_engine=("pool", "dve", "act")):
        self.nc = nc
        self.q = {e: [] for e in ENGS}
        self.ndma = {e: 0 for e in ENGS}
        self.dma_last = {e: [None] * NDSEM for e in ENGS}
        self.strict = set(strict_same_engine)
        self.es = contextlib.ExitStack()
        self.n = 0
        self.all_dmas = []

    def sbuf(self, name, shape, dtype):
        return self.es.enter_context(self.nc.sbuf_tensor("sb_" + name, list(shape), dtype))

    def psum(self, name, shape, dtype):
        return self.es.enter_context(self.nc.psum_tensor("ps_" + name, list(shape), dtype))

    def _mk(self, eng, fn, reads, writes, is_dma):
        I = Ins()
        I.eng = eng
        I.fn = fn
        I.is_dma = is_dma
        I.sig = False
        I.sigval = None
        I.dsem = None
        I.dval = None
        I.idx = self.n
        self.n += 1
        deps = []
        for b in reads:
            if b.last_w is not None:
                deps.append(b.last_w)
        for b in writes:
            if b.last_w is not None:
                deps.append(b.last_w)
            deps.extend(b.readers)
        out = []
        seen = set()
        for d in deps:
            if id(d) in seen or d is I:
                continue
            seen.add(id(d))
            if (not d.is_dma) and d.eng == eng and eng not in self.strict:
                continue
            out.append(d)
        I.deps = out
        for b in writes:
            b.last_w = I
            b.readers = []
        for b in reads:
            if b.last_w is not I:
                b.readers.append(I)
        if is_dma:
            k = self.ndma[eng]
            self.ndma[eng] = k + 1
            slot = k % NDSEM
            prev = self.dma_last[eng][slot]
            if prev is not None:
                I.deps.append(prev)
            self.dma_last[eng][slot] = I
            I.dsem = (eng, slot)
            I.dval = 16 * (k // NDSEM + 1)
            self.all_dmas.append(I)
        self.q[eng].append(I)
        return I

    def op(self, eng, fn, reads=(), writes=()):
        return self._mk(eng, fn, list(reads), list(writes), False)

    def dma(self, eng, fn, reads=(), writes=()):
        return self._mk(eng, fn, list(reads), list(writes), True)

    def barrier(self):
        lasts = []
        for e in ENGS:
            comp = [i for i in self.q[e] if not i.is_dma]
            if comp:
                lasts.append(comp[-1])
        dl = []
        for e in ENGS:
            for s in range(NDSEM):
                if self.dma_last[e][s] is not None:
                    dl.append(self.dma_last[e][s])
        for e in ENGS:
            I = Ins()
            I.eng = e
            I.fn = None
            I.is_dma = False
            I.sig = False
            I.sigval = None
            I.dsem = None
            I.dval = None
            I.idx = self.n
            self.n += 1
            I.deps = [d for d in lasts if d.eng != e] + list(dl)
            self.q[e].append(I)

    def finish(self):
        nc = self.nc
        self.barrier()
        es = self.es
        sem = {}
        for e in ("pe", "act", "dve", "pool"):
            sem[e] = es.enter_context(nc.semaphore("s_" + e))
        dsem = {}
        for e in ENGS:
            if self.ndma[e]:
                for s in range(NDSEM):
                    dsem[(e, s)] = es.enter_context(nc.semaphore("d_%s%d" % (e, s)))
        for e in ENGS:
            for I in self.q[e]:
                for d in I.deps:
                    if not d.is_dma:
                        d.sig = True
        for e in ENGS:
            c = 0
            for I in self.q[e]:
                if I.sig:
                    c += 1
                    I.sigval = c
        handles = {"pe": "tensor", "act": "scalar", "dve": "vector", "pool": "gpsimd", "sp": "sync"}
        with nc.Block() as block:
            for e in ENGS:
                lst = self.q[e]
                if not lst:
                    continue

                def body(eng, lst=lst, e=e):
                    waited = {}
                    for I in lst:
                        for d in I.deps:
                            if d.is_dma:
                                key = ("d",) + d.dsem
                                val = d.dval
                                s = dsem[d.dsem]
                            else:
                                key = ("c", d.eng)
                                val = d.sigval
                                s = sem[d.eng]
                            if waited.get(key, 0) >= val:
                                continue
                            waited[key] = val
                            eng.wait_ge(s, val)
                        if I.fn is None:
                            continue
                        r = I.fn(eng)
                        if I.is_dma:
                            r.then_inc(dsem[I.dsem], 16)
                        elif I.sig:
                            r.then_inc(sem[e], 1)

                getattr(block, handles[e])(body)
        es.close()


def mm(S, out, lhsT, rhs, start, stop, R, W):
    return S.op("pe", lambda e: e.matmul(out, lhsT, rhs, start=start, stop=stop), R, W)


def tr(S, out, in_, ident, R, W):
    return S.op("pe", lambda e: e.transpose(out, in_, ident), R, W)


def act(S, out, in_, func, R, W, bias=None, scale=None, accum_out=None, eng="act"):
    kw = {}
    if bias is not None:
        kw["bias"] = bias
    if scale is not None:
        kw["scale"] = scale
    if accum_out is not None:
        kw["accum_out"] = accum_out
    return S.op(eng, lambda e: e.activation(out, in_, func, **kw), R, W)


def ts(S, eng, out, in0, s1, s2, op0, op1, R, W, accum_out=None):
    kw = {}
    if accum_out is not None:
        kw["accum_out"] = accum_out
    if op1 is None:
        return S.op(eng, lambda e: e.tensor_scalar(out, in0, s1, None, op0, **kw), R, W)
    return S.op(eng, lambda e: e.tensor_scalar(out, in0, s1, s2, op0, op1, **kw), R, W)


def tt(S, eng, out, in0, in1, op, R, W):
    return S.op(eng, lambda e: e.tensor_tensor(out, in0, in1, op), R, W)


def stt(S, out, in0, scalar, in1, op0, op1, R, W):
    return S.op("dve", lambda e: e.scalar_tensor_tensor(out, in0, scalar, in1, op0, op1), R, W)


def cp(S, eng, out, in_, R, W):
    if eng == "act":
        return S.op("act", lambda e: e.copy(out, in_), R, W)
    return S.op(eng, lambda e: e.tensor_copy(out, in_), R, W)


def dma(S, eng, out, in_, R, W, **kw):
    return S.dma(eng, lambda e: e.dma_start(out=out, in_=in_, **kw), R, W)


class Ring:
    def __init__(self, items):
        self.items = items
        self.i = 0

    def next(self):
        it = self.items[self.i % len(self.items)]
        self.i += 1
        return it


def rmsnorm_rstd(S, x_ap, xb, junk, junkb, ss, ssb, rstd, rstdb, D, eps):
    act(S, junk, x_ap, AF.Square, [xb], [junkb, ssb], accum_out=ss)
    act(S, rstd, ss, AF.Sqrt, [ssb], [rstdb], bias=None, scale=1.0 / D)
    return None


D = 1024
DFF = 4096
EPS = 1e-6


def new_nc():
    return bass.Bass("TRN2", target_bir_lowering=False)


def load_cast_weight(S, w_dram, w_sb, wb, nk, ncols, row0=0, col0=0):
    for kc in range(nk):
        for c0 in range(0, ncols, 2048):
            cw = min(2048, ncols - c0)
            dma(S, "pool", w_sb[:, kc, c0:c0 + cw],
                w_dram[row0 + kc * 128:row0 + (kc + 1) * 128, col0 + c0:col0 + c0 + cw], [], [wb])


def norm_tile(S, x_ap, xb, g_bc, gb, xn_ap, xnb, scr, D_=D):
    junk, junkb, ss, ssb, rstd, rstdb = scr
    act(S, junk, x_ap, AF.Square, [xb], [junkb, ssb], accum_out=ss)
    ts(S, "dve", rstd, ss, 1.0 / D_, EPS, ALU.mult, ALU.add, [ssb], [rstdb])
    act(S, rstd, rstd, AF.Sqrt, [rstdb], [rstdb])
    S.op("dve", lambda e: e.reciprocal(rstd, rstd), [rstdb], [rstdb])
    stt(S, xn_ap, x_ap, rstd, g_bc, ALU.mult, ALU.mult, [xb, rstdb, gb], [xnb])


def build_mlp(NT, final):
    nc = new_nc()
    NTOK = NT * 128
    x1 = nc.dram_tensor("x1", [NTOK, D], F32, kind="ExternalInput").ap()
    g = nc.dram_tensor("g", [1, D], F32, kind="ExternalInput").ap()
    gf = nc.dram_tensor("gf", [1, D], F32, kind="ExternalInput").ap()
    w_up = nc.dram_tensor("w_up", [D, DFF], F32, kind="ExternalInput").ap()
    w_dn = nc.dram_tensor("w_dn", [DFF, D], F32, kind="ExternalInput").ap()
    ident_d = nc.dram_tensor("ident", [128, 128], F32, kind="ExternalInput").ap()
    x2 = nc.dram_tensor("x2", [NTOK, D], F32, kind="ExternalOutput").ap()
    S = Sched(nc)
    wup = S.sbuf("wup", [128, 8, DFF], BF16)
    wupb = Buf()
    wdn = S.sbuf("wdn", [128, 32, D], BF16)
    wdnb = Buf()
    identf = S.sbuf("identf", [128, 128], F32)
    ident = S.sbuf("identb16", [128, 128], BF16)
    identb = Buf()
    gbc = S.sbuf("gbc", [128, D], F32)
    gfbc = S.sbuf("gfbc", [128, D], F32)
    gb = Buf()
    dma(S, "sp", identf[:], ident_d[:, :], [], [identb])
    cp(S, "dve", ident[:], identf[:], [identb], [identb])
    dma(S, "sp", gbc[:], g[0:1, :].partition_broadcast(128), [], [gb])
    dma(S, "sp", gfbc[:], gf[0:1, :].partition_broadcast(128), [], [gb])
    load_cast_weight(S, w_up, wup, wupb, 8, DFF)
    load_cast_weight(S, w_dn, wdn, wdnb, 32, D)

    G = 2
    GT = G * 128
    NG = NT // G
    xt = [S.sbuf("xt%d" % i, [128, D], F32) for i in range(G)]
    xtb = [Buf() for _ in range(G)]
    xn = [S.sbuf("xn%d" % i, [128, D], BF16) for i in range(1)]
    xnb = [Buf() for _ in range(1)]
    xnT = [S.sbuf("xnT%d" % i, [128, 8, GT], BF16) for i in range(1)]
    xnTb = [Buf() for _ in range(1)]
    hT = S.sbuf("hT", [128, 32, GT], BF16)
    hTb = [Buf() for _ in range(32)]
    rr = [S.sbuf("rr%d" % i, [128, GT], F32) for i in range(2)]
    rrb = [Buf() for _ in range(2)]
    xo = [S.sbuf("xo%d" % i, [128, D], F32) for i in range(2)]
    xob = [Buf() for _ in range(2)]
    junk = S.sbuf("junk", [128, D], BF16)
    junkb = Buf()
    ss = S.sbuf("ss", [128, 4], F32)
    ssb = Buf()
    rstd = S.sbuf("rstd", [128, 4], F32)
    rstdb = Buf()
    pT = [S.psum("pT%d" % i, [128, D], BF16) for i in range(2)]
    pTb = [Buf() for _ in range(2)]
    pU = [S.psum("pU%d" % i, [128, GT], F32) for i in range(3)]
    pUb = [Buf() for _ in range(3)]
    pD = [S.psum("pD%d" % i, [128, 512], F32) for i in range(2)]
    pDb = [Buf() for _ in range(2)]
    cnt = {"t": 0, "u": 0, "d": 0, "o": 0}
    for gi in range(NG):
        par = 0
        for t in range(G):
            ti = gi * G + t
            xs = t
            dma(S, "sp", xt[xs][:], x1[ti * 128:(ti + 1) * 128, :], [], [xtb[xs]])
            k = cnt["t"] % 2
            cnt["t"] += 1
            norm_tile(S, xt[xs][:], xtb[xs], gbc[:], gb, xn[0][:], xnb[0],
                      (junk[:], junkb, ss[:, 0:1], ssb, rstd[:, 0:1], rstdb))
            for c in range(8):
                tr(S, pT[k][:, c * 128:(c + 1) * 128], xn[0][:, c * 128:(c + 1) * 128], ident[:],
                   [xnb[0], identb], [pTb[k]])
            cp(S, "act", xnT[par][:, :, t * 128:(t + 1) * 128],
               pT[k][:].rearrange("p (c t) -> p c t", c=8), [pTb[k]], [xnTb[par]])
        for fb in range(32):
            k = cnt["u"] % 3
            cnt["u"] += 1
            for kc in range(8):
                mm(S, pU[k][:], wup[:, kc, fb * 128:(fb + 1) * 128], xnT[par][:, kc, :],
                   kc == 0, kc == 7, [wupb, xnTb[par]], [pUb[k]])
            act(S, rr[k % 2][:], pU[k][:], AF.Relu, [pUb[k]], [rrb[k % 2]])
            tt(S, "pool", hT[:, fb, :], rr[k % 2][:], rr[k % 2][:], ALU.mult, [rrb[k % 2]], [hTb[fb]])
        for t in range(G):
            ti = gi * G + t
            xs = t
            ko = cnt["o"] % 2
            cnt["o"] += 1
            for half in range(2):
                k = cnt["d"] % 2
                cnt["d"] += 1
                for fb in range(32):
                    mm(S, pD[k][:], hT[:, fb, t * 128:(t + 1) * 128], wdn[:, fb, half * 512:(half + 1) * 512],
                       fb == 0, fb == 31, [hTb[fb], wdnb], [pDb[k]])
                tt(S, "dve", xo[ko][:, half * 512:(half + 1) * 512], pD[k][:],
                   xt[xs][:, half * 512:(half + 1) * 512], ALU.add, [pDb[k], xtb[xs]], [xob[ko]])
            if final:
                norm_tile(S, xo[ko][:], xob[ko], gfbc[:], gb, xt[xs][:], xtb[xs],
                          (junk[:], junkb, ss[:, 1:2], ssb, rstd[:, 1:2], rstdb))
                dma(S, "sp", x2[ti * 128:(ti + 1) * 128, :], xt[xs][:], [xtb[xs]], [])
            else:
                dma(S, "sp", x2[ti * 128:(ti + 1) * 128, :], xo[ko][:], [xob[ko]], [])
    S.finish()
    return nc


HD = 64
IDX_SCALE = (8 ** -0.5) * (64 ** -0.5)
FM_SEGS = [("qa", 0, 512, 0.125), ("ka", 512, 512, 1.0), ("qb", 1536, 512, 0.125), ("kb", 2048, 512, 1.0),
           ("qi", 3072, 512, 1.0), ("ki", 3584, 128, 1.0), ("xc", 3656, 512, 1.0)]
DIN = 7752


def build_proj(NT, skip=()):
    nc = new_nc()
    NTOK = NT * 128
    x = nc.dram_tensor("x", [NTOK, D], F32, kind="ExternalInput").ap()
    g = nc.dram_tensor("g", [1, D], F32, kind="ExternalInput").ap()
    w_in = nc.dram_tensor("w_in", [D, DIN], F32, kind="ExternalInput").ap()
    ident_d = nc.dram_tensor("ident", [128, 128], F32, kind="ExternalInput").ap()
    outs = {}
    for nm, c0, ncol, sc in FM_SEGS:
        dt_ = F32 if nm == "xc" else BF16
        outs[nm] = nc.dram_tensor("o_" + nm, [ncol, NTOK], dt_, kind="ExternalOutput").ap()
    o_va = nc.dram_tensor("o_va", [NTOK, 8 * 65], BF16, kind="ExternalOutput").ap()
    o_vb = nc.dram_tensor("o_vb", [NTOK, 8 * 65], BF16, kind="ExternalOutput").ap()
    o_wi = nc.dram_tensor("o_wi", [NTOK, 8], F32, kind="ExternalOutput").ap()
    o_ks = nc.dram_tensor("o_ks", [512, NT], F32, kind="ExternalOutput").ap()
    S = Sched(nc)
    identf = S.sbuf("identf", [128, 128], F32)
    ident = S.sbuf("identh", [128, 128], BF16)
    identb = Buf()
    gbc = S.sbuf("gbc", [128, D], F32)
    gb = Buf()
    dma(S, "sp", identf[:], ident_d[:, :], [], [identb])
    cp(S, "dve", ident[:], identf[:], [identb], [identb])
    dma(S, "sp", gbc[:], g[0:1, :].partition_broadcast(128), [], [gb])
    xnT = S.sbuf("xnT", [128, 8, NTOK], BF16)
    xnTb = [Buf() for _ in range(NT)]
    xt = [S.sbuf("xt%d" % i, [128, D], F32) for i in range(2)]
    xtb = [Buf() for _ in range(2)]
    xn = S.sbuf("xn", [128, D], BF16)
    xnb = Buf()
    junk = S.sbuf("junk", [128, D], BF16)
    junkb = Buf()
    ss = S.sbuf("ss", [128, 4], F32)
    ssb = Buf()
    rstd = S.sbuf("rstd", [128, 4], F32)
    rstdb = Buf()
    pT = [S.psum("pT%d" % i, [128, D], BF16) for i in range(2)]
    pTb = [Buf() for _ in range(2)]
    pM = [S.psum("pM%d" % i, [128, 512], F32) for i in range(4)]
    pMb = [Buf() for _ in range(4)]
    for ti in range(NT):
        k = ti % 2
        dma(S, "sp", xt[k][:], x[ti * 128:(ti + 1) * 128, :], [], [xtb[k]])
        norm_tile(S, xt[k][:], xtb[k], gbc[:], gb, xn[:], xnb,
                  (junk[:], junkb, ss[:, 0:1], ssb, rstd[:, 0:1], rstdb))
        for c in range(8):
            tr(S, pT[k][:, c * 128:(c + 1) * 128], xn[:, c * 128:(c + 1) * 128], ident[:],
               [xnb, identb], [pTb[k]])
        cp(S, "act", xnT[:, :, ti * 128:(ti + 1) * 128],
           pT[k][:].rearrange("p (c t) -> p c t", c=8), [pTb[k]], [xnTb[ti]])
    wsb = [S.sbuf("wsb%d" % i, [128, 8, 512], BF16) for i in range(2)]
    wsbb = [Buf() for _ in range(2)]
    stg = [S.sbuf("stg%d" % i, [128, NTOK], F32) for i in range(2)]
    stgb = [Buf() for _ in range(2)]
    stgh = [S.sbuf("stgh%d" % i, [128, NTOK], BF16) for i in range(2)]
    stghb = [Buf() for _ in range(2)]
    ks = S.sbuf("ks", [128, 4, NT], F32)
    ksb = Buf()
    NG = (NTOK + 511) // 512
    cn = {"w": 0, "p": 0, "s": 0}
    for nm, c0, ncol, sc in FM_SEGS:
        wk = cn["w"] % 2
        cn["w"] += 1
        load_cast_weight(S, w_in, wsb[wk], wsbb[wk], 8, ncol, 0, c0)
        for cb in range((ncol + 127) // 128):
            cw = min(128, ncol - cb * 128)
            sk = cn["s"] % 2
            cn["s"] += 1
            isf = nm == "xc"
            dst = stg[sk] if isf else stgh[sk]
            dstb = stgb[sk] if isf else stghb[sk]
            for gi in range(NG):
                n0 = gi * 512
                nw = min(512, NTOK - n0)
                pk = cn["p"] % 4
                cn["p"] += 1
                for kc in range(8):
                    mm(S, pM[pk][:cw, :nw], wsb[wk][:, kc, cb * 128:cb * 128 + cw], xnT[:, kc, n0:n0 + nw],
                       kc == 0, kc == 7, [wsbb[wk]] + xnTb, [pMb[pk]])
                if nm == "ka":
                    for t_ in range(nw // 128):
                        ts(S, "dve", dst[:, n0 + t_ * 128:n0 + (t_ + 1) * 128], pM[pk][:, t_ * 128:(t_ + 1) * 128],
                           1.0, None, ALU.mult, ALU.add, [pMb[pk]], [dstb, ksb],
                           accum_out=ks[:, cb, n0 // 128 + t_:n0 // 128 + t_ + 1])
                elif gi % 2 == 0:
                    act(S, dst[:cw, n0:n0 + nw], pM[pk][:cw, :nw], AF.Copy, [pMb[pk]], [dstb], scale=sc)
                else:
                    ts(S, "dve", dst[:cw, n0:n0 + nw], pM[pk][:cw, :nw], sc, None, ALU.mult, None, [pMb[pk]], [dstb])
            dma(S, "sp", outs[nm][cb * 128:cb * 128 + cw, :], dst[:cw, :], [dstb], [])
    if "ks" not in skip:
        dma(S, "sp", o_ks.rearrange("(c p) t -> p c t", p=128), ks[:], [ksb], [])
    vst = [S.sbuf("vst%d" % i, [128, 8, 65], BF16) for i in range(2)]
    vstb = [Buf() for _ in range(2)]
    for i in range(2):
        S.op("pool", lambda e, o=vst[i][:]: e.memset(o, 1.0), [], [vstb[i]])
    wst = S.sbuf("wst", [128, NT, 8], F32)
    wstb = Buf()
    cn["v"] = 0
    for nm, c0, od in ((("va", 1024, o_va), ("vb", 2560, o_vb)) if "v" not in skip else ()):
        wk = cn["w"] % 2
        cn["w"] += 1
        load_cast_weight(S, w_in, wsb[wk], wsbb[wk], 8, 512, 0, c0)
        for ti in range(NT):
            pk = cn["p"] % 4
            cn["p"] += 1
            for kc in range(8):
                mm(S, pM[pk][:], xnT[:, kc, ti * 128:(ti + 1) * 128], wsb[wk][:, kc, :],
                   kc == 0, kc == 7, [wsbb[wk]] + xnTb, [pMb[pk]])
            vk = cn["v"] % 2
            cn["v"] += 1
            cp(S, "act", vst[vk][:, :, 0:64], pM[pk][:].rearrange("p (h d) -> p h d", h=8), [pMb[pk]], [vstb[vk]])
            dma(S, "sp", od[ti * 128:(ti + 1) * 128, :], vst[vk][:].rearrange("p h d -> p (h d)"), [vstb[vk]], [])
    wk = cn["w"] % 2
    cn["w"] += 1
    load_cast_weight(S, w_in, wsb[wk], wsbb[wk], 8, 512, 0, 3648)
    for ti in (range(NT) if "wi" not in skip else ()):
        pk = cn["p"] % 4
        cn["p"] += 1
        for kc in range(8):
            mm(S, pM[pk][:, 0:8], xnT[:, kc, ti * 128:(ti + 1) * 128], wsb[wk][:, kc, 0:8],
               kc == 0, kc == 7, [wsbb[wk]] + xnTb, [pMb[pk]])
        ts(S, "dve", wst[:, ti, :], pM[pk][:, 0:8], IDX_SCALE, None, ALU.mult, None, [pMb[pk]], [wstb])
    if "wi" not in skip:
        dma(S, "sp", o_wi.rearrange("(t p) h -> p t h", p=128), wst[:], [wstb], [])
    S.finish()
    return nc


def build_rglru(SL):
    nc = new_nc()
    xc = nc.dram_tensor("xc", [128, SL], F32, kind="ExternalInput").ap()
    prm = nc.dram_tensor("prm", [128, 8], F32, kind="ExternalInput").ap()
    wr = nc.dram_tensor("wr", [128, 128], F32, kind="ExternalInput").ap()
    wi = nc.dram_tensor("wi", [128, 128], F32, kind="ExternalInput").ap()
    ho = nc.dram_tensor("h", [128, SL], F32, kind="ExternalOutput").ap()
    S = Sched(nc)
    CH = min(2048, SL)
    NCH = SL // CH
    xp = S.sbuf("xp", [128, 4 + SL], F32)
    xpb = [Buf() for _ in range(NCH)]
    padb = Buf()
    S.op("dve", lambda e: e.memset(xp[:, 0:4], 0.0), [], [padb])
    for c in range(NCH):
        dma(S, "sp", xp[:, 4 + c * CH:4 + (c + 1) * CH], xc[:, c * CH:(c + 1) * CH], [], [xpb[c]])
    P = S.sbuf("prm", [128, 8], F32)
    Pb = Buf()
    dma(S, "sp", P[:], prm[:, :], [], [Pb])
    wrs = S.sbuf("wrs", [128, 1, 128], BF16)
    wis = S.sbuf("wis", [128, 1, 128], BF16)
    wb = Buf()
    load_cast_weight(S, wr, wrs, wb, 1, 128)
    load_cast_weight(S, wi, wis, wb, 1, 128)
    c8 = S.sbuf("c8", [128, 2], F32)
    c8b = Buf()
    act(S, c8[:, 0:1], P[:, 7:8], AF.Exp, [Pb], [c8b], scale=-1.0)
    act(S, c8[:, 0:1], c8[:, 0:1], AF.Ln, [c8b], [c8b], bias=1.0)
    ts(S, "dve", c8[:, 1:2], c8[:, 0:1], -8.0, None, ALU.mult, None, [c8b], [c8b])

    def T(name, dt_=F32, n=1):
        return [S.sbuf("%s%d" % (name, i), [128, CH], dt_) for i in range(n)], [Buf() for _ in range(n)]
    y, yb = T("y", F32, 2)
    yh, yhb = T("yh", BF16, 1)
    r, rb = T("r", F32, 1)
    ig, igb = T("ig", F32, 1)
    a, ab = T("a", F32, 2)
    u, ub = T("u", F32, 2)
    h, hb = T("h", F32, 2)
    pG = [S.psum("pG%d" % i, [128, 512], F32) for i in range(4)]
    pGb = [Buf() for _ in range(4)]
    pc = 0
    for c in range(NCH):
        k = c % 2
        o = 4 + c * CH
        deps = [xpb[c], padb] + ([xpb[c - 1]] if c else [])
        ts(S, "dve", y[k][:], xp[:, o - 3:o - 3 + CH], P[:, 0:1], P[:, 4:5], ALU.mult, ALU.add, deps + [Pb], [yb[k]])
        for i in (1, 2, 3):
            stt(S, y[k][:], xp[:, o - 3 + i:o - 3 + i + CH], P[:, i:i + 1], y[k][:], ALU.mult, ALU.add,
                deps + [Pb, yb[k]], [yb[k]])
        cp(S, "pool", yh[0][:], y[k][:], [yb[k]], [yhb[0]])
        for gi in range(CH // 512):
            for (wsb_, dst, dstb, bcol) in ((wrs, r, rb, 5), (wis, ig, igb, 6)):
                pk = pc % 4
                pc += 1
                mm(S, pG[pk][:], wsb_[:, 0, :], yh[0][:, gi * 512:(gi + 1) * 512], True, True, [wb, yhb[0]], [pGb[pk]])
                act(S, dst[0][:, gi * 512:(gi + 1) * 512], pG[pk][:], AF.Sigmoid, [pGb[pk], Pb], [dstb[0]],
                    bias=P[:, bcol:bcol + 1])
        act(S, a[k][:], r[0][:], AF.Exp, [rb[0], c8b], [ab[k]], scale=c8[:, 1:2])
        tt(S, "pool", ig[0][:], ig[0][:], y[k][:], ALU.mult, [igb[0], yb[k]], [igb[0]])
        tt(S, "dve", u[k][:], a[k][:], a[k][:], ALU.mult, [ab[k]], [ub[k]])
        ts(S, "dve", u[k][:], u[k][:], -1.0, 1.0, ALU.mult, ALU.add, [ub[k]], [ub[k]])
        act(S, u[k][:], u[k][:], AF.Sqrt, [ub[k]], [ub[k]])
        tt(S, "dve", u[k][:], u[k][:], ig[0][:], ALU.mult, [ub[k], igb[0]], [ub[k]])
        init = 0.0 if c == 0 else h[1 - k][:, CH - 1:CH]
        S.op("dve", lambda e, o_=h[k][:], a_=a[k][:], u_=u[k][:], i_=init: e.tensor_tensor_scan(o_, a_, u_, i_, ALU.mult, ALU.add),
             [ab[k], ub[k]] + ([hb[1 - k]] if c else []), [hb[k]])
        dma(S, "sp", ho[:, c * CH:(c + 1) * CH], h[k][:], [hb[k]], [])
    S.finish()
    return nc


def build_tail(NT):
    nc = new_nc()
    NTOK = NT * 128
    x = nc.dram_tensor("x", [NTOK, D], F32, kind="ExternalInput").ap()
    g = nc.dram_tensor("g", [1, D], F32, kind="ExternalInput").ap()
    w_in = nc.dram_tensor("w_in", [D, DIN], F32, kind="ExternalInput").ap()
    ya = nc.dram_tensor("ya", [NTOK, 512], F32, kind="ExternalInput").ap()
    yb = nc.dram_tensor("yb", [NTOK, 512], F32, kind="ExternalInput").ap()
    hT = nc.dram_tensor("hT", [512, NTOK], F32, kind="ExternalInput").ap()
    w_pa = nc.dram_tensor("w_pa", [512, D], F32, kind="ExternalInput").ap()
    w_pb = nc.dram_tensor("w_pb", [512, D], F32, kind="ExternalInput").ap()
    w_pc = nc.dram_tensor("w_pc", [512, D], F32, kind="ExternalInput").ap()
    w_o = nc.dram_tensor("w_o", [D, D], F32, kind="ExternalInput").ap()
    ident_d = nc.dram_tensor("ident", [128, 128], F32, kind="ExternalInput").ap()
    x1 = nc.dram_tensor("x1", [NTOK, D], F32, kind="ExternalOutput").ap()
    S = Sched(nc)
    identf = S.sbuf("identf", [128, 128], F32)
    ident = S.sbuf("identh", [128, 128], BF16)
    identb = Buf()
    gbc = S.sbuf("gbc", [128, D], F32)
    gb = Buf()
    dma(S, "sp", identf[:], ident_d[:, :], [], [identb])
    cp(S, "dve", ident[:], identf[:], [identb], [identb])
    dma(S, "sp", gbc[:], g[0:1, :].partition_broadcast(128), [], [gb])
    wg = S.sbuf("wg", [128, 8, 3584], BF16)
    wgb = Buf()
    load_cast_weight(S, w_in, wg, wgb, 8, 3584, 0, 4168)
    wp = [S.sbuf("wp%d" % i, [128, 4, D], BF16) for i in range(3)]
    wpb = Buf()
    for i, wd_ in enumerate((w_pa, w_pb, w_pc)):
        load_cast_weight(S, wd_, wp[i], wpb, 4, D)
    wo = S.sbuf("wo", [128, 8, D], BF16)
    wob = Buf()
    load_cast_weight(S, w_o, wo, wob, 8, D)
    G = 4
    GT = 512
    NG = NT // G
    xt = [S.sbuf("xt%d" % i, [128, D], F32) for i in range(G)]
    xtb = [Buf() for _ in range(G)]
    xn = S.sbuf("xn", [128, D], BF16)
    xnb = Buf()
    xnT = S.sbuf("xnT", [128, 8, GT], BF16)
    xnTb = Buf()
    yT = [S.sbuf("yT%d" % i, [128, 4, GT], BF16) for i in range(3)]
    yTb = [Buf() for _ in range(3)]
    yst = [S.sbuf("yst%d" % i, [128, 512], BF16) for i in range(2)]
    ystb = [Buf() for _ in range(2)]
    mT = S.sbuf("mT", [128, 8, GT], BF16)
    mTb = [Buf() for _ in range(8)]
    ht = S.sbuf("ht", [128, GT], F32)
    htb = Buf()
    tmp = [S.sbuf("tmp%d" % i, [128, GT], F32) for i in range(4)]
    tmpb = [Buf() for _ in range(4)]
    sg = [S.sbuf("sg%d" % i, [128, GT], F32) for i in range(3)]
    sgb = [Buf() for _ in range(3)]
    mm_ = [S.sbuf("mm%d" % i, [128, GT], F32) for i in range(3)]
    mmb = [Buf() for _ in range(3)]
    xo = [S.sbuf("xo%d" % i, [128, D], F32) for i in range(2)]
    xob = [Buf() for _ in range(2)]
    junk = S.sbuf("junk", [128, D], BF16)
    junkb = Buf()
    ss = S.sbuf("ss", [128, 4], F32)
    ssb = Buf()
    rstd = S.sbuf("rstd", [128, 4], F32)
    rstdb = Buf()
    pT = [S.psum("pT%d" % i, [128, D], BF16) for i in range(2)]
    pTb = [Buf() for _ in range(2)]
    pM = [S.psum("pM%d" % i, [128, 512], F32) for i in range(4)]
    pMb = [Buf() for _ in range(4)]
    pO = [S.psum("pO%d" % i, [128, 512], F32) for i in range(2)]
    pOb = [Buf() for _ in range(2)]
    cn = {"t": 0, "p": 0, "o": 0, "y": 0, "x": 0}
    C0 = 0.7978845608028654
    for gi in range(NG):
        for t in range(G):
            ti = gi * G + t
            dma(S, "sp", xt[t][:], x[ti * 128:(ti + 1) * 128, :], [], [xtb[t]])
            k = cn["t"] % 2
            cn["t"] += 1
            norm_tile(S, xt[t][:], xtb[t], gbc[:], gb, xn[:], xnb,
                      (junk[:], junkb, ss[:, 0:1], ssb, rstd[:, 0:1], rstdb))
            for c in range(8):
                tr(S, pT[k][:, c * 128:(c + 1) * 128], xn[:, c * 128:(c + 1) * 128], ident[:], [xnb, identb], [pTb[k]])
            cp(S, "act", xnT[:, :, t * 128:(t + 1) * 128], pT[k][:].rearrange("p (c t) -> p c t", c=8), [pTb[k]], [xnTb])
            for bi, ysrc in enumerate((ya, yb)):
                yk = cn["y"] % 2
                cn["y"] += 1
                dma(S, "pool", yst[yk][:], ysrc[ti * 128:(ti + 1) * 128, :], [], [ystb[yk]])
                k = cn["t"] % 2
                cn["t"] += 1
                for c in range(4):
                    tr(S, pT[k][:, c * 128:(c + 1) * 128], yst[yk][:, c * 128:(c + 1) * 128], ident[:], [ystb[yk], identb], [pTb[k]])
                cp(S, "dve", yT[bi][:, :, t * 128:(t + 1) * 128], pT[k][:, 0:512].rearrange("p (c t) -> p c t", c=4), [pTb[k]], [yTb[bi]])
        for cb in range(4):
            pk = cn["p"] % 4
            cn["p"] += 1
            for kc in range(8):
                mm(S, pM[pk][:], wg[:, kc, cb * 128:(cb + 1) * 128], xnT[:, kc, :], kc == 0, kc == 7, [wgb, xnTb], [pMb[pk]])
            dma(S, "sp", ht[:], hT[cb * 128:(cb + 1) * 128, gi * GT:(gi + 1) * GT], [], [htb])
            xs, x2, t3 = tmp[0], tmp[1], tmp[2]
            cp(S, "act", xs[:], pM[pk][:], [pMb[pk]], [tmpb[0]])
            tt(S, "pool", x2[:], xs[:], xs[:], ALU.mult, [tmpb[0]], [tmpb[1]])
            ts(S, "dve", x2[:], x2[:], 0.044715, 1.0, ALU.mult, ALU.add, [tmpb[1]], [tmpb[1]])
            tt(S, "dve", x2[:], x2[:], xs[:], ALU.mult, [tmpb[1], tmpb[0]], [tmpb[1]])
            act(S, t3[:], x2[:], AF.Tanh, [tmpb[1]], [tmpb[2]], scale=C0)
            ts(S, "dve", t3[:], t3[:], 0.5, 0.5, ALU.mult, ALU.add, [tmpb[2]], [tmpb[2]])
            tt(S, "pool", t3[:], t3[:], xs[:], ALU.mult, [tmpb[2], tmpb[0]], [tmpb[2]])
            tt(S, "dve", yT[2][:, cb, :], t3[:], ht[:], ALU.mult, [tmpb[2], htb], [yTb[2]])
        for cb in range(8):
            for br in range(3):
                pk = cn["p"] % 4
                cn["p"] += 1
                c0 = 512 + br * 1024 + cb * 128
                for kc in range(8):
                    mm(S, pM[pk][:], wg[:, kc, c0:c0 + 128], xnT[:, kc, :], kc == 0, kc == 7, [wgb, xnTb], [pMb[pk]])
                act(S, sg[br][:], pM[pk][:], AF.Sigmoid, [pMb[pk]], [sgb[br]])
                pk = cn["p"] % 4
                cn["p"] += 1
                for c in range(4):
                    mm(S, pM[pk][:], wp[br][:, c, cb * 128:(cb + 1) * 128], yT[br][:, c, :], c == 0, c == 3, [wpb, yTb[br]], [pMb[pk]])
                tt(S, "dve", mm_[br][:], pM[pk][:], sg[br][:], ALU.mult, [pMb[pk], sgb[br]], [mmb[br]])
            tt(S, "pool", mm_[0][:], mm_[0][:], mm_[1][:], ALU.add, [mmb[0], mmb[1]], [mmb[0]])
            tt(S, "pool", mT[:, cb, :], mm_[0][:], mm_[2][:], ALU.add, [mmb[0], mmb[2]], [mTb[cb]])
        for t in range(G):
            ti = gi * G + t
            ko = cn["x"] % 2
            cn["x"] += 1
            for half in range(2):
                k = cn["o"] % 2
                cn["o"] += 1
                for cb in range(8):
                    mm(S, pO[k][:], mT[:, cb, t * 128:(t + 1) * 128], wo[:, cb, half * 512:(half + 1) * 512],
                       cb == 0, cb == 7, [mTb[cb], wob], [pOb[k]])
                tt(S, "dve", xo[ko][:, half * 512:(half + 1) * 512], pO[k][:], xt[t][:, half * 512:(half + 1) * 512],
                   ALU.add, [pOb[k], xtb[t]], [xob[ko]])
            dma(S, "sp", x1[ti * 128:(ti + 1) * 128, :], xo[ko][:], [xob[ko]], [])
    S.finish()
    return nc


NEGM = -30000.0
NSEL = 256.0


def build_attn(SL, NT, NIT=26, do_a=True, do_b=True):
    nc = new_nc()
    NTOK = NT * 128
    NKT = SL // 128
    NB = SL // 256
    qt = {"a": nc.dram_tensor("qta", [512, NTOK], BF16, kind="ExternalInput").ap(),
          "b": nc.dram_tensor("qtb", [512, NTOK], BF16, kind="ExternalInput").ap()}
    kt = {"a": nc.dram_tensor("kta", [512, SL], BF16, kind="ExternalInput").ap(),
          "b": nc.dram_tensor("ktb", [512, SL], BF16, kind="ExternalInput").ap()}
    vv = {"a": nc.dram_tensor("va", [8, 128, NKT, 65], BF16, kind="ExternalInput").ap(),
          "b": nc.dram_tensor("vb", [8, 128, NKT, 65], BF16, kind="ExternalInput").ap()}
    qit = nc.dram_tensor("qit", [512, NTOK], BF16, kind="ExternalInput").ap()
    kit = nc.dram_tensor("kit", [64, SL], BF16, kind="ExternalInput").ap()
    wi = nc.dram_tensor("wi", [NTOK, 8], F32, kind="ExternalInput").ap()
    ksum = nc.dram_tensor("ksum", [512, NKT], F32, kind="ExternalInput").ap()
    swt = nc.dram_tensor("swt", [16, 8, 128, 128], F32, kind="ExternalInput").ap()
    b31 = nc.dram_tensor("b31", [128, 16], F32, kind="ExternalInput").ap()
    cm = nc.dram_tensor("cm", [128, 512], F32, kind="ExternalInput").ap()
    vcon = nc.dram_tensor("vcon", [NT, 3, 64], F32, kind="ExternalInput").ap()
    ident_d = nc.dram_tensor("ident", [128, 128], F32, kind="ExternalInput").ap()
    yo = {"a": nc.dram_tensor("ya", [NTOK, 512], F32, kind="ExternalOutput").ap(),
          "b": nc.dram_tensor("yb", [NTOK, 512], F32, kind="ExternalOutput").ap()}
    negB = nc.dram_tensor("negB", [NT, 128, SL], BF16, kind="Internal").ap()
    negBb = [Buf() for _ in range(NT)]
    S = Sched(nc)
    identf = S.sbuf("identf", [128, 128], F32)
    ident = S.sbuf("identh", [128, 128], BF16)
    identb = Buf()
    dma(S, "sp", identf[:], ident_d[:, :], [], [identb])
    cp(S, "dve", ident[:], identf[:], [identb], [identb])
    B31 = S.sbuf("B31", [128, 16], F32)
    CM = S.sbuf("CM", [128, 512], F32)
    WI = S.sbuf("WI", [128, NT, 8], F32)
    half = S.sbuf("half", [128, 1], F32)
    cb_ = Buf()
    dma(S, "sp", B31[:], b31[:, :], [], [cb_])
    dma(S, "sp", CM[:], cm[:, :], [], [cb_])
    dma(S, "sp", WI[:], wi.rearrange("(t p) h -> p t h", p=128), [], [cb_])
    S.op("dve", lambda e: e.memset(half[:], 0.5), [], [cb_])
    sc = S.sbuf("sc", [128, SL], F32)
    scb = Buf()
    neg = S.sbuf("neg", [128, SL], BF16)
    negb = Buf()
    if do_b:
        qim = [S.sbuf("qim%d" % i, [64, 8, 128], BF16) for i in range(2)]
        qimb = [Buf() for _ in range(2)]
        kig = [S.sbuf("kig%d" % i, [64, 512], BF16) for i in range(2)]
        kigb = [Buf() for _ in range(2)]
        dg = S.sbuf("dg", [128, 8, 128], BF16)
        dgb = Buf()
        Rr = [S.sbuf("Rr%d" % i, [128, 512], BF16) for i in range(8)]
        Rrb = [Buf() for _ in range(8)]
        st = S.sbuf("st", [128, 8], F32)
        stb = Buf()
        st2 = S.sbuf("st2", [128, 2], F32)
        st2b = Buf()
        pD = [S.psum("pD%d" % i, [128, 512], F32) for i in range(2)]
        pDb = [Buf() for _ in range(2)]
        pC = S.psum("pC", [128, 512], F32)
        pCb = Buf()
        cn = {"d": 0, "k": 0}
        for m in range(NT):
            L = (m + 1) * 512
            qk = m % 2
            dma(S, "sp", qim[qk][:], qit.rearrange("(h d) t -> d h t", d=64)[:, :, m * 128:(m + 1) * 128], [], [qimb[qk]])
            for h in range(8):
                ts(S, "dve", dg[:, h, :], ident[:], WI[:, m, h:h + 1], None, ALU.mult, None, [identb, cb_], [dgb])
            for u in range(m + 1):
                kk = cn["k"] % 2
                cn["k"] += 1
                dma(S, "sp", kig[kk][:], kit[:, u * 512:(u + 1) * 512], [], [kigb[kk]])
                for h in range(8):
                    pk = cn["d"] % 2
                    cn["d"] += 1
                    mm(S, pD[pk][:], qim[qk][:, h, :], kig[kk][:], True, True, [qimb[qk], kigb[kk]], [pDb[pk]])
                    if h % 2 == 0:
                        act(S, Rr[h][:], pD[pk][:], AF.Relu, [pDb[pk]], [Rrb[h]])
                    else:
                        ts(S, "dve", Rr[h][:], pD[pk][:], 0.0, None, ALU.max, None, [pDb[pk]], [Rrb[h]])
                for h in range(8):
                    mm(S, pC[:], dg[:, h, :], Rr[h][:], h == 0, h == 7, [dgb, Rrb[h]], [pCb])
                cp(S, "act", sc[:, u * 512:(u + 1) * 512], pC[:], [pCb], [scb])
            ts(S, "dve", neg[:, :L], sc[:, :L], 1.0, None, ALU.mult, ALU.max, [scb], [negb, stb], accum_out=st[:, 7:8])
            ts(S, "dve", neg[:, :L], sc[:, :L], -1.0, None, ALU.mult, ALU.max, [scb], [negb, stb], accum_out=st[:, 6:7])
            tt(S, "dve", st[:, 7:8], st[:, 7:8], st[:, 6:7], ALU.max, [stb], [stb])
            tt(S, "dve", sc[:, L - 512:L], sc[:, L - 512:L], CM[:], ALU.add, [scb, cb_], [scb])
            ts(S, "dve", st[:, 1:2], st[:, 7:8], 1.0001, 1e-6, ALU.mult, ALU.add, [stb], [stb])
            ts(S, "dve", st[:, 0:1], st[:, 1:2], -1.0, None, ALU.mult, None, [stb], [stb])
            for it in range(NIT):
                stt(S, st[:, 2:3], st[:, 0:1], st[:, 1:2], half[:], ALU.add, ALU.mult, [stb, cb_], [stb])
                ts(S, "dve", st2[:, 0:1], st[:, 2:3], -1.0, None, ALU.mult, None, [stb], [st2b])
                act(S, neg[:, :L], sc[:, :L], AF.Sign, [scb, st2b], [negb, st2b], bias=st2[:, 0:1], accum_out=st2[:, 1:2])
                ts(S, "dve", st[:, 4:5], st2[:, 1:2], 2.0 * NSEL - L - 0.5, None, ALU.is_ge, None, [st2b], [stb])
                tt(S, "dve", st[:, 5:6], st[:, 2:3], st[:, 0:1], ALU.subtract, [stb], [stb])
                tt(S, "dve", st[:, 6:7], st[:, 1:2], st[:, 2:3], ALU.subtract, [stb], [stb])
                stt(S, st[:, 0:1], st[:, 5:6], st[:, 4:5], st[:, 0:1], ALU.mult, ALU.add, [stb], [stb])
                stt(S, st[:, 1:2], st[:, 6:7], st[:, 4:5], st[:, 2:3], ALU.mult, ALU.add, [stb], [stb])
            ts(S, "dve", neg[:, :L], sc[:, :L], st[:, 0:1], NEGM, ALU.is_lt, ALU.mult, [scb, stb], [negb])
            dma(S, "sp", negB[m, :, 0:L], neg[:, :L], [negb], [negBb[m]])
    kth = S.sbuf("kth", [64, SL], BF16)
    kthb = Buf()
    vth = S.sbuf("vth", [128, NKT, 65], BF16)
    vthb = Buf()
    qth = S.sbuf("qth", [64, NTOK], BF16)
    qthb = Buf()
    swh = S.sbuf("swh", [128, 8, 128], F32)
    swhb = Buf()
    PT = [S.sbuf("PT%d" % i, [128, 512], BF16) for i in range(2)]
    PTb = [Buf() for _ in range(2)]
    ksh = S.sbuf("ksh", [64, NKT], F32)
    kmf = S.sbuf("kmf", [64, NB], F32)
    kmT = S.sbuf("kmT", [64, 64], BF16)
    kmb = Buf()
    vc = S.sbuf("vc", [128, 3, 64], F32)
    vcb = Buf()
    gp = S.sbuf("gp", [128, 64], F32)
    mx8 = S.sbuf("mx8", [128, 8], F32)
    sel = S.sbuf("sel", [128, 64], F32)
    gb_ = Buf()
    rc = S.sbuf("rc", [128, 2], F32)
    yt = [S.sbuf("yt%d" % i, [128, 64], F32) for i in range(2)]
    ytb = [Buf() for _ in range(2)]
    pS = [S.psum("pS%d" % i, [128, 512], F32) for i in range(2)]
    pSb = [Buf() for _ in range(2)]
    pO = [S.psum("pO%d" % i, [128, 128], F32) for i in range(2)]
    pOb = [Buf() for _ in range(2)]
    pG = S.psum("pG", [128, 64], F32)
    pGb = Buf()
    c2 = {"s": 0, "o": 0, "y": 0}
    S.op("pool", lambda e: e.memset(kmT[:], 0.0), [], [kmb])
    heads = ([("b", h) for h in range(8)] if do_b else []) + ([("a", h) for h in range(8)] if do_a else [])
    for br, h in heads:
        hh = h if br == "a" else 8 + h
        dma(S, "sp", kth[:], kt[br][h * 64:(h + 1) * 64, :], [], [kthb])
        dma(S, "sp", vth[:], vv[br][h], [], [vthb])
        dma(S, "sp", qth[:], qt[br][h * 64:(h + 1) * 64, :], [], [qthb])
        dma(S, "sp", swh[:], swt[hh].rearrange("p k q -> k p q"), [], [swhb])
        if br == "a":
            dma(S, "sp", ksh[:], ksum[h * 64:(h + 1) * 64, :], [], [kmb])
            v2 = ksh[:].rearrange("d (n two) -> d n two", two=2)
            tt(S, "dve", kmf[:], v2[:, :, 0], v2[:, :, 1], ALU.add, [kmb], [kmb])
            ts(S, "dve", kmT[:, :NB], kmf[:], 1.0 / 256.0, None, ALU.mult, None, [kmb], [kmb])
        for m in range(NT):
            L = (m + 1) * 512
            qs = qth[:, m * 128:(m + 1) * 128]
            if br == "b":
                dma(S, "sp", neg[:, :L], negB[m, :, 0:L], [negBb[m]], [negb])
            else:
                dma(S, "sp", vc[:], vcon[m:m + 1].partition_broadcast(128), [], [vcb])
                mm(S, pG[:], qs, kmT[:], True, True, [qthb, kmb], [pGb])
                tt(S, "dve", gp[:], pG[:], vc[:, 0, :], ALU.add, [pGb, vcb], [gb_])
                S.op("dve", lambda e: e.max(out=mx8[:], in_=gp[:]), [gb_], [gb_])
                ts(S, "dve", sel[:], gp[:], mx8[:, 2:3], None, ALU.is_ge, None, [gb_], [gb_])
                tt(S, "dve", sel[:], sel[:], vc[:, 1, :], ALU.mult, [gb_, vcb], [gb_])
                tt(S, "dve", sel[:], sel[:], vc[:, 2, :], ALU.max, [gb_, vcb], [gb_])
                ts(S, "dve", sel[:], sel[:], -1.0, -NEGM, ALU.add, ALU.mult, [gb_], [gb_])
                nb = L // 256
                cp(S, "dve", neg[:, :L].rearrange("p (n k) -> p n k", k=256),
                   sel[:, :nb].unsqueeze(2).to_broadcast([128, nb, 256]), [gb_], [negb])
            ok = c2["o"] % 2
            c2["o"] += 1
            for u in range(m + 1):
                win = u >= m - 1
                sk = c2["s"] % 2
                c2["s"] += 1
                for tj in range(4):
                    ktile = 4 * u + tj
                    o_ = pS[sk][:, tj * 128:(tj + 1) * 128]
                    mm(S, o_, kth[:, ktile * 128:(ktile + 1) * 128], qs, True, False, [kthb, qthb], [pSb[sk]])
                    mm(S, o_, neg[:, ktile * 128:(ktile + 1) * 128], ident[:], False, not win, [negb, identb], [pSb[sk]])
                    if win:
                        p = (u - (m - 1)) * 4 + tj
                        mm(S, o_, identf[:], swh[:, p, :], False, True, [identb, swhb], [pSb[sk]])
                if win:
                    act(S, PT[sk][:], pS[sk][:], AF.Exp, [pSb[sk]], [PTb[sk]])
                else:
                    act(S, PT[sk][:], pS[sk][:], AF.Exp, [pSb[sk], cb_], [PTb[sk]], bias=B31[:, hh:hh + 1])
                for tj in range(4):
                    ktile = 4 * u + tj
                    mm(S, pO[ok][:, 0:65], PT[sk][:, tj * 128:(tj + 1) * 128], vth[:, ktile, :],
                       u == 0 and tj == 0, u == m and tj == 3, [PTb[sk], vthb], [pOb[ok]])
            yk = c2["y"] % 2
            c2["y"] += 1
            S.op("dve", lambda e, o=rc[:, 0:1], i=pO[ok][:, 64:65]: e.reciprocal(o, i), [pOb[ok]], [gb_])
            ts(S, "dve", yt[yk][:], pO[ok][:, 0:64], rc[:, 0:1], None, ALU.mult, None, [pOb[ok], gb_], [ytb[yk]])
            dma(S, "sp", yo[br][m * 128:(m + 1) * 128, h * 64:(h + 1) * 64], yt[yk][:], [ytb[yk]], [])
    S.finish()
    return nc


N_BUCKETS = 32
MAX_DISTANCE = 128


def t5_bucket_np(dist):
    n = np.maximum(dist, 0)
    max_exact = N_BUCKETS // 2
    nf = np.maximum(n, 1).astype(np.float32)
    large = max_exact + (np.log(nf / np.float32(max_exact)) / np.float32(math.log(MAX_DISTANCE / max_exact))
                         * np.float32(N_BUCKETS - max_exact)).astype(np.int32)
    large = np.minimum(large, N_BUCKETS - 1)
    return np.where(n < max_exact, n, large)


def attn_consts(j, NT, rel_bias):
    k = np.arange(128)[:, None]
    q = np.arange(128)[None, :]
    swt = np.empty((16, 8, 128, 128), np.float32)
    for p in range(8):
        delta = 4 + j - p
        d = delta * 128 + q - k
        bk = t5_bucket_np(d)
        for hh in range(16):
            swt[hh, p] = np.where(d >= 0, rel_bias[bk, hh], np.float32(-30000.0))
    b31 = np.broadcast_to(rel_bias[31][None, :], (128, 16)).astype(np.float32).copy()
    qq = np.arange(128)[:, None]
    c = np.arange(512)[None, :]
    ktile = c // 128
    cm = np.where((ktile < j) | ((ktile == j) & ((c % 128) <= qq)), np.float32(0.0), np.float32(-1e30)).astype(np.float32)
    vcon = np.zeros((NT, 3, 64), np.float32)
    n = np.arange(64)
    for m in range(NT):
        cur = (4 * m + j) // 2
        vcon[m, 0] = np.where(n < cur, 0.0, -30000.0)
        vcon[m, 1] = (n < cur).astype(np.float32)
        vcon[m, 2] = (n == cur).astype(np.float32)
    return {"swt": swt, "b31": b31, "cm": cm, "vcon": vcon, "ident": np.eye(128, dtype=np.float32)}


def own_tokens(j, NT):
    return (np.arange(NT)[:, None] * 4 + j)[:, :, None].reshape(NT, 1) * 128 + np.arange(128)[None, :]


import ml_dtypes

_PROGS = {}


def _prog(key, fn):
    if key not in _PROGS:
        _PROGS[key] = fn()
    return _PROGS[key]


def _run(nc, in_maps):
    res = run_bass_kernel_spmd(nc, in_maps, core_ids=list(range(8)))
    return res.results


def _blockdiag(w2):
    m = np.zeros((128, 128), np.float32)
    m[:64, :64] = w2[0]
    m[64:, 64:] = w2[1]
    return m


def kernel(x, rel_bias, norm_mix_g, w_in, conv_w, conv_b, w_r, b_r, w_i, b_i, lru_lambda,
           w_pa, w_pb, w_pc, w_o, norm_mlp_g, w_up, w_down, final_norm_g):
    f32 = np.float32
    x = np.asarray(x, f32)
    B, SL, _ = x.shape
    depth = int(np.asarray(w_in).shape[0])
    assert B == 2 and SL % 512 == 0
    NT = SL // 512
    NKT = SL // 128
    NTOK = NT * 128
    rel_bias = np.asarray(rel_bias, f32)
    ident = np.eye(128, dtype=f32)
    toks = [own_tokens(j, NT).reshape(-1) for j in range(4)]
    consts = [attn_consts(j, NT, rel_bias) for j in range(4)]
    x_own = [np.ascontiguousarray(x[c // 4][toks[c % 4]]) for c in range(8)]
    p_proj = _prog(("proj", NT), lambda: build_proj(NT))
    p_attn = _prog(("attn", SL, NT), lambda: build_attn(SL, NT))
    p_rg = _prog(("rg", SL), lambda: build_rglru(SL))
    p_tail = _prog(("tail", NT), lambda: build_tail(NT))
    gfin = np.asarray(final_norm_g, f32).reshape(1, -1)
    for l in range(depth):
        wl = np.ascontiguousarray(np.asarray(w_in[l], f32))
        gmix = np.asarray(norm_mix_g[l], f32).reshape(1, -1)
        r1 = _run(p_proj, [{"x": x_own[c], "g": gmix, "w_in": wl, "ident": ident} for c in range(8)])
        shared = []
        for b in range(2):
            def full_cols(name, rows, dt):
                a = np.empty((rows, SL), dt)
                for j in range(4):
                    a[:, toks[j]] = np.asarray(r1[b * 4 + j][name])[:rows]
                return a
            def full_rows(name, cols, dt):
                a = np.empty((SL, cols), dt)
                for j in range(4):
                    a[toks[j]] = np.asarray(r1[b * 4 + j][name])
                return a
            def vlay(v):
                return np.ascontiguousarray(v.reshape(NKT, 128, 8, 65).transpose(2, 1, 0, 3))
            bf = ml_dtypes.bfloat16
            ks = np.empty((512, NKT), f32)
            for j in range(4):
                ks[:, j::4] = np.asarray(r1[b * 4 + j]["o_ks"])
            shared.append({
                "kta": full_cols("o_ka", 512, bf), "ktb": full_cols("o_kb", 512, bf), "kit": full_cols("o_ki", 64, bf),
                "va": vlay(full_rows("o_va", 520, bf)), "vb": vlay(full_rows("o_vb", 520, bf)),
                "ksum": ks, "xc": full_cols("o_xc", 512, f32)})
        im = []
        for c in range(8):
            b, j = c // 4, c % 4
            d = dict(consts[j])
            sh = shared[b]
            d.update({"qta": np.asarray(r1[c]["o_qa"]), "qtb": np.asarray(r1[c]["o_qb"]), "qit": np.asarray(r1[c]["o_qi"]),
                      "wi": np.asarray(r1[c]["o_wi"]), "kta": sh["kta"], "ktb": sh["ktb"], "kit": sh["kit"],
                      "va": sh["va"], "vb": sh["vb"], "ksum": sh["ksum"]})
            im.append(d)
        r2 = _run(p_attn, im)
        im = []
        for c in range(8):
            b, cc = c // 4, c % 4
            ch = slice(cc * 128, (cc + 1) * 128)
            prm = np.stack([np.asarray(conv_w[l][0], f32)[ch], np.asarray(conv_w[l][1], f32)[ch],
                            np.asarray(conv_w[l][2], f32)[ch], np.asarray(conv_w[l][3], f32)[ch],
                            np.asarray(conv_b[l], f32)[ch], np.asarray(b_r[l], f32)[ch], np.asarray(b_i[l], f32)[ch],
                            np.asarray(lru_lambda[l], f32)[ch]], axis=1)
            im.append({"xc": np.ascontiguousarray(shared[b]["xc"][ch]), "prm": np.ascontiguousarray(prm),
                       "wr": _blockdiag(np.asarray(w_r[l], f32)[2 * cc:2 * cc + 2]),
                       "wi": _blockdiag(np.asarray(w_i[l], f32)[2 * cc:2 * cc + 2])})
        r3 = _run(p_rg, im)
        hfull = [np.concatenate([np.asarray(r3[b * 4 + cc]["h"]) for cc in range(4)], axis=0) for b in range(2)]
        im = []
        for c in range(8):
            b, j = c // 4, c % 4
            im.append({"x": x_own[c], "g": gmix, "w_in": wl, "ya": np.asarray(r2[c]["ya"]), "yb": np.asarray(r2[c]["yb"]),
                       "hT": np.ascontiguousarray(hfull[b][:, toks[j]]),
                       "w_pa": np.asarray(w_pa[l], f32), "w_pb": np.asarray(w_pb[l], f32), "w_pc": np.asarray(w_pc[l], f32),
                       "w_o": np.asarray(w_o[l], f32), "ident": ident})
        r4 = _run(p_tail, im)
        final = l == depth - 1
        p_mlp = _prog(("mlp", NT, final), lambda: build_mlp(NT, final))
        gm = np.asarray(norm_mlp_g[l], f32).reshape(1, -1)
        r5 = _run(p_mlp, [{"x1": np.asarray(r4[c]["x1"]), "g": gm, "gf": gfin, "w_up": np.asarray(w_up[l], f32),
                           "w_dn": np.asarray(w_down[l], f32), "ident": ident} for c in range(8)])
        x_own = [np.asarray(r5[c]["x2"]) for c in range(8)]
    out = np.empty((B, SL, x.shape[2]), f32)
    for c in range(8):
        out[c // 4][toks[c % 4]] = x_own[c]
    return out
```

```python
import math
import contextlib
import numpy as np
import concourse.bass as bass
import concourse.mybir as mybir
from concourse.bass_utils import run_bass_kernel_spmd

F32 = mybir.dt.float32
BF16 = mybir.dt.bfloat16
AF = mybir.ActivationFunctionType
ALU = mybir.AluOpType
AX = mybir.AxisListType

ENGS = ("pe", "act", "dve", "pool", "sp")
NDSEM = 8


class Buf:
    __slots__ = ("name", "last_w", "readers")

    def __init__(self, name=""):
        self.name = name
        self.last_w = None
        self.readers = []


class Ins:
    __slots__ = ("eng", "fn", "deps", "is_dma", "sig", "sigval", "dsem", "dval", "idx")


class Sched:
    def __init__(self, nc, strict_same_engine=("pool", "dve", "act")):
        self.nc = nc
        self.q = {e: [] for e in ENGS}
        self.ndma = {e: 0 for e in ENGS}
        self.dma_last = {e: [None] * NDSEM for e in ENGS}
        self.strict = set(strict_same_engine)
        self.es = contextlib.ExitStack()
        self.n = 0
        self.all_dmas = []

    def sbuf(self, name, shape, dtype):
        return self.es.enter_context(self.nc.sbuf_tensor("sb_" + name, list(shape), dtype))

    def psum(self, name, shape, dtype):
        return self.es.enter_context(self.nc.psum_tensor("ps_" + name, list(shape), dtype))

    def _mk(self, eng, fn, reads, writes, is_dma):
        I = Ins()
        I.eng = eng
        I.fn = fn
        I.is_dma = is_dma
        I.sig = False
        I.sigval = None
        I.dsem = None
        I.dval = None
        I.idx = self.n
        self.n += 1
        deps = []
        for b in reads:
            if b.last_w is not None:
                deps.append(b.last_w)
        for b in writes:
            if b.last_w is not None:
                deps.append(b.last_w)
            deps.extend(b.readers)
        out = []
        seen = set()
        for d in deps:
            if id(d) in seen or d is I:
                continue
            seen.add(id(d))
            if (not d.is_dma) and d.eng == eng and eng not in self.strict:
                continue
            out.append(d)
        I.deps = out
        for b in writes:
            b.last_w = I
            b.readers = []
        for b in reads:
            if b.last_w is not I:
                b.readers.append(I)
        if is_dma:
            k = self.ndma[eng]
            self.ndma[eng] = k + 1
            slot = k % NDSEM
            prev = self.dma_last[eng][slot]
            if prev is not None:
                I.deps.append(prev)
            self.dma_last[eng][slot] = I
            I.dsem = (eng, slot)
            I.dval = 16 * (k // NDSEM + 1)
            self.all_dmas.append(I)
        self.q[eng].append(I)
        return I

    def op(self, eng, fn, reads=(), writes=()):
        return self._mk(eng, fn, list(reads), list(writes), False)

    def dma(self, eng, fn, reads=(), writes=()):
        return self._mk(eng, fn, list(reads), list(writes), True)

    def barrier(self):
        lasts = []
        for e in ENGS:
            comp = [i for i in self.q[e] if not i.is_dma]
            if comp:
                lasts.append(comp[-1])
        dl = []
        for e in ENGS:
            for s in range(NDSEM):
                if self.dma_last[e][s] is not None:
                    dl.append(self.dma_last[e][s])
        for e in ENGS:
            I = Ins()
            I.eng = e
            I.fn = None
            I.is_dma = False
            I.sig = False
            I.sigval = None
            I.dsem = None
            I.dval = None
            I.idx = self.n
            self.n += 1
            I.deps = [d for d in lasts if d.eng != e] + list(dl)
            self.q[e].append(I)

    def finish(self):
        nc = self.nc
        self.barrier()
        es = self.es
        sem = {}
        for e in ("pe", "act", "dve", "pool"):
            sem[e] = es.enter_context(nc.semaphore("s_" + e))
        dsem = {}
        for e in ENGS:
            if self.ndma[e]:
                for s in range(NDSEM):
                    dsem[(e, s)] = es.enter_context(nc.semaphore("d_%s%d" % (e, s)))
        for e in ENGS:
            for I in self.q[e]:
                for d in I.deps:
                    if not d.is_dma:
                        d.sig = True
        for e in ENGS:
            c = 0
            for I in self.q[e]:
                if I.sig:
                    c += 1
                    I.sigval = c
        handles = {"pe": "tensor", "act": "scalar", "dve": "vector", "pool": "gpsimd", "sp": "sync"}
        with nc.Block() as block:
            for e in ENGS:
                lst = self.q[e]
                if not lst:
                    continue

                def body(eng, lst=lst, e=e):
                    waited = {}
                    for I in lst:
                        for d in I.deps:
                            if d.is_dma:
                                key = ("d",) + d.dsem
                                val = d.dval
                                s = dsem[d.dsem]
                            else:
                                key = ("c", d.eng)
                                val = d.sigval
                                s = sem[d.eng]
                            if waited.get(key, 0) >= val:
                                continue
                            waited[key] = val
                            eng.wait_ge(s, val)
                        if I.fn is None:
                            continue
                        r = I.fn(eng)
                        if I.is_dma:
                            r.then_inc(dsem[I.dsem], 16)
                        elif I.sig:
                            r.then_inc(sem[e], 1)

                getattr(block, handles[e])(body)
        es.close()


def mm(S, out, lhsT, rhs, start, stop, R, W):
    return S.op("pe", lambda e: e.matmul(out, lhsT, rhs, start=start, stop=stop), R, W)


def tr(S, out, in_, ident, R, W):
    return S.op("pe", lambda e: e.transpose(out, in_, ident), R, W)


def act(S, out, in_, func, R, W, bias=None, scale=None, accum_out=None, eng="act"):
    kw = {}
    if bias is not None:
        kw["bias"] = bias
    if scale is not None:
        kw["scale"] = scale
    if accum_out is not None:
        kw["accum_out"] = accum_out
    return S.op(eng, lambda e: e.activation(out, in_, func, **kw), R, W)


def ts(S, eng, out, in0, s1, s2, op0, op1, R, W, accum_out=None):
    kw = {}
    if accum_out is not None:
        kw["accum_out"] = accum_out
    if op1 is None:
        return S.op(eng, lambda e: e.tensor_scalar(out, in0, s1, None, op0, **kw), R, W)
    return S.op(eng, lambda e: e.tensor_scalar(out, in0, s1, s2, op0, op1, **kw), R, W)


def tt(S, eng, out, in0, in1, op, R, W):
    return S.op(eng, lambda e: e.tensor_tensor(out, in0, in1, op), R, W)


def stt(S, out, in0, scalar, in1, op0, op1, R, W):
    return S.op("dve", lambda e: e.scalar_tensor_tensor(out, in0, scalar, in1, op0, op1), R, W)


def cp(S, eng, out, in_, R, W):
    if eng == "act":
        return S.op("act", lambda e: e.copy(out, in_), R, W)
    return S.op(eng, lambda e: e.tensor_copy(out, in_), R, W)


def dma(S, eng, out, in_, R, W, **kw):
    return S.dma(eng, lambda e: e.dma_start(out=out, in_=in_, **kw), R, W)


class Ring:
    def __init__(self, items):
        self.items = items
        self.i = 0

    def next(self):
        it = self.items[self.i % len(self.items)]
        self.i += 1
        return it


def rmsnorm_rstd(S, x_ap, xb, junk, junkb, ss, ssb, rstd, rstdb, D, eps):
    act(S, junk, x_ap, AF.Square, [xb], [junkb, ssb], accum_out=ss)
    act(S, rstd, ss, AF.Sqrt, [ssb], [rstdb], bias=None, scale=1.0 / D)
    return None


D = 1024
DFF = 4096
EPS = 1e-6


def new_nc():
    return bass.Bass("TRN2", target_bir_lowering=False)


def load_cast_weight(S, w_dram, w_sb, wb, nk, ncols, row0=0, col0=0):
    for kc in range(nk):
        for c0 in range(0, ncols, 2048):
            cw = min(2048, ncols - c0)
            dma(S, "pool", w_sb[:, kc, c0:c0 + cw],
                w_dram[row0 + kc * 128:row0 + (kc + 1) * 128, col0 + c0:col0 + c0 + cw], [], [wb])


def norm_tile(S, x_ap, xb, g_bc, gb, xn_ap, xnb, scr, D_=D):
    junk, junkb, ss, ssb, rstd, rstdb = scr
    act(S, junk, x_ap, AF.Square, [xb], [junkb, ssb], accum_out=ss)
    ts(S, "dve", rstd, ss, 1.0 / D_, EPS, ALU.mult, ALU.add, [ssb], [rstdb])
    act(S, rstd, rstd, AF.Sqrt, [rstdb], [rstdb])
    S.op("dve", lambda e: e.reciprocal(rstd, rstd), [rstdb], [rstdb])
    stt(S, xn_ap, x_ap, rstd, g_bc, ALU.mult, ALU.mult, [xb, rstdb, gb], [xnb])


def build_mlp(NT, final):
    nc = new_nc()
    NTOK = NT * 128
    x1 = nc.dram_tensor("x1", [NTOK, D], F32, kind="ExternalInput").ap()
    g = nc.dram_tensor("g", [1, D], F32, kind="ExternalInput").ap()
    gf = nc.dram_tensor("gf", [1, D], F32, kind="ExternalInput").ap()
    w_up = nc.dram_tensor("w_up", [D, DFF], F32, kind="ExternalInput").ap()
    w_dn = nc.dram_tensor("w_dn", [DFF, D], F32, kind="ExternalInput").ap()
    ident_d = nc.dram_tensor("ident", [128, 128], F32, kind="ExternalInput").ap()
    x2 = nc.dram_tensor("x2", [NTOK, D], F32, kind="ExternalOutput").ap()
    S = Sched(nc)
    wup = S.sbuf("wup", [128, 8, DFF], BF16)
    wupb = Buf()
    wdn = S.sbuf("wdn", [128, 32, D], BF16)
    wdnb = Buf()
    identf = S.sbuf("identf", [128, 128], F32)
    ident = S.sbuf("identb16", [128, 128], BF16)
    identb = Buf()
    gbc = S.sbuf("gbc", [128, D], F32)
    gfbc = S.sbuf("gfbc", [128, D], F32)
    gb = Buf()
    dma(S, "sp", identf[:], ident_d[:, :], [], [identb])
    cp(S, "dve", ident[:], identf[:], [identb], [identb])
    dma(S, "sp", gbc[:], g[0:1, :].partition_broadcast(128), [], [gb])
    dma(S, "sp", gfbc[:], gf[0:1, :].partition_broadcast(128), [], [gb])
    load_cast_weight(S, w_up, wup, wupb, 8, DFF)
    load_cast_weight(S, w_dn, wdn, wdnb, 32, D)

    G = 2
    GT = G * 128
    NG = NT // G
    xt = [S.sbuf("xt%d" % i, [128, D], F32) for i in range(G)]
    xtb = [Buf() for _ in range(G)]
    xn = [S.sbuf("xn%d" % i, [128, D], BF16) for i in range(1)]
    xnb = [Buf() for _ in range(1)]
    xnT = [S.sbuf("xnT%d" % i, [128, 8, GT], BF16) for i in range(1)]
    xnTb = [Buf() for _ in range(1)]
    hT = S.sbuf("hT", [128, 32, GT], BF16)
    hTb = [Buf() for _ in range(32)]
    rr = [S.sbuf("rr%d" % i, [128, GT], F32) for i in range(2)]
    rrb = [Buf() for _ in range(2)]
    xo = [S.sbuf("xo%d" % i, [128, D], F32) for i in range(2)]
    xob = [Buf() for _ in range(2)]
    junk = S.sbuf("junk", [128, D], BF16)
    junkb = Buf()
    ss = S.sbuf("ss", [128, 4], F32)
    ssb = Buf()
    rstd = S.sbuf("rstd", [128, 4], F32)
    rstdb = Buf()
    pT = [S.psum("pT%d" % i, [128, D], BF16) for i in range(2)]
    pTb = [Buf() for _ in range(2)]
    pU = [S.psum("pU%d" % i, [128, GT], F32) for i in range(3)]
    pUb = [Buf() for _ in range(3)]
    pD = [S.psum("pD%d" % i, [128, 512], F32) for i in range(2)]
    pDb = [Buf() for _ in range(2)]
    cnt = {"t": 0, "u": 0, "d": 0, "o": 0}
    for gi in range(NG):
        par = 0
        for t in range(G):
            ti = gi * G + t
            xs = t
            dma(S, "sp", xt[xs][:], x1[ti * 128:(ti + 1) * 128, :], [], [xtb[xs]])
            k = cnt["t"] % 2
            cnt["t"] += 1
            norm_tile(S, xt[xs][:], xtb[xs], gbc[:], gb, xn[0][:], xnb[0],
                      (junk[:], junkb, ss[:, 0:1], ssb, rstd[:, 0:1], rstdb))
            for c in range(8):
                tr(S, pT[k][:, c * 128:(c + 1) * 128], xn[0][:, c * 128:(c + 1) * 128], ident[:],
                   [xnb[0], identb], [pTb[k]])
            cp(S, "act", xnT[par][:, :, t * 128:(t + 1) * 128],
               pT[k][:].rearrange("p (c t) -> p c t", c=8), [pTb[k]], [xnTb[par]])
        for fb in range(32):
            k = cnt["u"] % 3
            cnt["u"] += 1
            for kc in range(8):
                mm(S, pU[k][:], wup[:, kc, fb * 128:(fb + 1) * 128], xnT[par][:, kc, :],
                   kc == 0, kc == 7, [wupb, xnTb[par]], [pUb[k]])
            act(S, rr[k % 2][:], pU[k][:], AF.Relu, [pUb[k]], [rrb[k % 2]])
            tt(S, "pool", hT[:, fb, :], rr[k % 2][:], rr[k % 2][:], ALU.mult, [rrb[k % 2]], [hTb[fb]])
        for t in range(G):
            ti = gi * G + t
            xs = t
            ko = cnt["o"] % 2
            cnt["o"] += 1
            for half in range(2):
                k = cnt["d"] % 2
                cnt["d"] += 1
                for fb in range(32):
                    mm(S, pD[k][:], hT[:, fb, t * 128:(t + 1) * 128], wdn[:, fb, half * 512:(half + 1) * 512],
                       fb == 0, fb == 31, [hTb[fb], wdnb], [pDb[k]])
                tt(S, "dve", xo[ko][:, half * 512:(half + 1) * 512], pD[k][:],
                   xt[xs][:, half * 512:(half + 1) * 512], ALU.add, [pDb[k], xtb[xs]], [xob[ko]])
            if final:
                norm_tile(S, xo[ko][:], xob[ko], gfbc[:], gb, xt[xs][:], xtb[xs],
                          (junk[:], junkb, ss[:, 1:2], ssb, rstd[:, 1:2], rstdb))
                dma(S, "sp", x2[ti * 128:(ti + 1) * 128, :], xt[xs][:], [xtb[xs]], [])
            else:
                dma(S, "sp", x2[ti * 128:(ti + 1) * 128, :], xo[ko][:], [xob[ko]], [])
    S.finish()
    return nc


HD = 64
IDX_SCALE = (8 ** -0.5) * (64 ** -0.5)
FM_SEGS = [("qa", 0, 512, 0.125), ("ka", 512, 512, 1.0), ("qb", 1536, 512, 0.125), ("kb", 2048, 512, 1.0),
           ("qi", 3072, 512, 1.0), ("ki", 3584, 128, 1.0), ("xc", 3656, 512, 1.0)]
DIN = 7752


def build_proj(NT, skip=()):
    nc = new_nc()
    NTOK = NT * 128
    x = nc.dram_tensor("x", [NTOK, D], F32, kind="ExternalInput").ap()
    g = nc.dram_tensor("g", [1, D], F32, kind="ExternalInput").ap()
    w_in = nc.dram_tensor("w_in", [D, DIN], F32, kind="ExternalInput").ap()
    ident_d = nc.dram_tensor("ident", [128, 128], F32, kind="ExternalInput").ap()
    outs = {}
    for nm, c0, ncol, sc in FM_SEGS:
        dt_ = F32 if nm == "xc" else BF16
        outs[nm] = nc.dram_tensor("o_" + nm, [ncol, NTOK], dt_, kind="ExternalOutput").ap()
    o_va = nc.dram_tensor("o_va", [NTOK, 8 * 65], BF16, kind="ExternalOutput").ap()
    o_vb = nc.dram_tensor("o_vb", [NTOK, 8 * 65], BF16, kind="ExternalOutput").ap()
    o_wi = nc.dram_tensor("o_wi", [NTOK, 8], F32, kind="ExternalOutput").ap()
    o_ks = nc.dram_tensor("o_ks", [512, NT], F32, kind="ExternalOutput").ap()
    S = Sched(nc)
    identf = S.sbuf("identf", [128, 128], F32)
    ident = S.sbuf("identh", [128, 128], BF16)
    identb = Buf()
    gbc = S.sbuf("gbc", [128, D], F32)
    gb = Buf()
    dma(S, "sp", identf[:], ident_d[:, :], [], [identb])
    cp(S, "dve", ident[:], identf[:], [identb], [identb])
    dma(S, "sp", gbc[:], g[0:1, :].partition_broadcast(128), [], [gb])
    xnT = S.sbuf("xnT", [128, 8, NTOK], BF16)
    xnTb = [Buf() for _ in range(NT)]
    xt = [S.sbuf("xt%d" % i, [128, D], F32) for i in range(2)]
    xtb = [Buf() for _ in range(2)]
    xn = S.sbuf("xn", [128, D], BF16)
    xnb = Buf()
    junk = S.sbuf("junk", [128, D], BF16)
    junkb = Buf()
    ss = S.sbuf("ss", [128, 4], F32)
    ssb = Buf()
    rstd = S.sbuf("rstd", [128, 4], F32)
    rstdb = Buf()
    pT = [S.psum("pT%d" % i, [128, D], BF16) for i in range(2)]
    pTb = [Buf() for _ in range(2)]
    pM = [S.psum("pM%d" % i, [128, 512], F32) for i in range(4)]
    pMb = [Buf() for _ in range(4)]
    for ti in range(NT):
        k = ti % 2
        dma(S, "sp", xt[k][:], x[ti * 128:(ti + 1) * 128, :], [], [xtb[k]])
        norm_tile(S, xt[k][:], xtb[k], gbc[:], gb, xn[:], xnb,
                  (junk[:], junkb, ss[:, 0:1], ssb, rstd[:, 0:1], rstdb))
        for c in range(8):
            tr(S, pT[k][:, c * 128:(c + 1) * 128], xn[:, c * 128:(c + 1) * 128], ident[:],
               [xnb, identb], [pTb[k]])
        cp(S, "act", xnT[:, :, ti * 128:(ti + 1) * 128],
           pT[k][:].rearrange("p (c t) -> p c t", c=8), [pTb[k]], [xnTb[ti]])
    wsb = [S.sbuf("wsb%d" % i, [128, 8, 512], BF16) for i in range(2)]
    wsbb = [Buf() for _ in range(2)]
    stg = [S.sbuf("stg%d" % i, [128, NTOK], F32) for i in range(2)]
    stgb = [Buf() for _ in range(2)]
    stgh = [S.sbuf("stgh%d" % i, [128, NTOK], BF16) for i in range(2)]
    stghb = [Buf() for _ in range(2)]
    ks = S.sbuf("ks", [128, 4, NT], F32)
    ksb = Buf()
    NG = (NTOK + 511) // 512
    cn = {"w": 0, "p": 0, "s": 0}
    for nm, c0, ncol, sc in FM_SEGS:
        wk = cn["w"] % 2
        cn["w"] += 1
        load_cast_weight(S, w_in, wsb[wk], wsbb[wk], 8, ncol, 0, c0)
        for cb in range((ncol + 127) // 128):
            cw = min(128, ncol - cb * 128)
            sk = cn["s"] % 2
            cn["s"] += 1
            isf = nm == "xc"
            dst = stg[sk] if isf else stgh[sk]
            dstb = stgb[sk] if isf else stghb[sk]
            for gi in range(NG):
                n0 = gi * 512
                nw = min(512, NTOK - n0)
                pk = cn["p"] % 4
                cn["p"] += 1
                for kc in range(8):
                    mm(S, pM[pk][:cw, :nw], wsb[wk][:, kc, cb * 128:cb * 128 + cw], xnT[:, kc, n0:n0 + nw],
                       kc == 0, kc == 7, [wsbb[wk]] + xnTb, [pMb[pk]])
                if nm == "ka":
                    for t_ in range(nw // 128):
                        ts(S, "dve", dst[:, n0 + t_ * 128:n0 + (t_ + 1) * 128], pM[pk][:, t_ * 128:(t_ + 1) * 128],
                           1.0, None, ALU.mult, ALU.add, [pMb[pk]], [dstb, ksb],
                           accum_out=ks[:, cb, n0 // 128 + t_:n0 // 128 + t_ + 1])
                elif gi % 2 == 0:
                    act(S, dst[:cw, n0:n0 + nw], pM[pk][:cw, :nw], AF.Copy, [pMb[pk]], [dstb], scale=sc)
                else:
                    ts(S, "dve", dst[:cw, n0:n0 + nw], pM[pk][:cw, :nw], sc, None, ALU.mult, None, [pMb[pk]], [dstb])
            dma(S, "sp", outs[nm][cb * 128:cb * 128 + cw, :], dst[:cw, :], [dstb], [])
    if "ks" not in skip:
        dma(S, "sp", o_ks.rearrange("(c p) t -> p c t", p=128), ks[:], [ksb], [])
    vst = [S.sbuf("vst%d" % i, [128, 8, 65], BF16) for i in range(2)]
    vstb = [Buf() for _ in range(2)]
    for i in range(2):
        S.op("pool", lambda e, o=vst[i][:]: e.memset(o, 1.0), [], [vstb[i]])
    wst = S.sbuf("wst", [128, NT, 8], F32)
    wstb = Buf()
    cn["v"] = 0
    for nm, c0, od in ((("va", 1024, o_va), ("vb", 2560, o_vb)) if "v" not in skip else ()):
        wk = cn["w"] % 2
        cn["w"] += 1
        load_cast_weight(S, w_in, wsb[wk], wsbb[wk], 8, 512, 0, c0)
        for ti in range(NT):
            pk = cn["p"] % 4
            cn["p"] += 1
            for kc in range(8):
                mm(S, pM[pk][:], xnT[:, kc, ti * 128:(ti + 1) * 128], wsb[wk][:, kc, :],
                   kc == 0, kc == 7, [wsbb[wk]] + xnTb, [pMb[pk]])
            vk = cn["v"] % 2
            cn["v"] += 1
            cp(S, "act", vst[vk][:, :, 0:64], pM[pk][:].rearrange("p (h d) -> p h d", h=8), [pMb[pk]], [vstb[vk]])
            dma(S, "sp", od[ti * 128:(ti + 1) * 128, :], vst[vk][:].rearrange("p h d -> p (h d)"), [vstb[vk]], [])
    wk = cn["w"] % 2
    cn["w"] += 1
    load_cast_weight(S, w_in, wsb[wk], wsbb[wk], 8, 512, 0, 3648)
    for ti in (range(NT) if "wi" not in skip else ()):
        pk = cn["p"] % 4
        cn["p"] += 1
        for kc in range(8):
            mm(S, pM[pk][:, 0:8], xnT[:, kc, ti * 128:(ti + 1) * 128], wsb[wk][:, kc, 0:8],
               kc == 0, kc == 7, [wsbb[wk]] + xnTb, [pMb[pk]])
        ts(S, "dve", wst[:, ti, :], pM[pk][:, 0:8], IDX_SCALE, None, ALU.mult, None, [pMb[pk]], [wstb])
    if "wi" not in skip:
        dma(S, "sp", o_wi.rearrange("(t p) h -> p t h", p=128), wst[:], [wstb], [])
    S.finish()
    return nc


def build_rglru(SL):
    nc = new_nc()
    xc = nc.dram_tensor("xc", [128, SL], F32, kind="ExternalInput").ap()
    prm = nc.dram_tensor("prm", [128, 8], F32, kind="ExternalInput").ap()
    wr = nc.dram_tensor("wr", [128, 128], F32, kind="ExternalInput").ap()
    wi = nc.dram_tensor("wi", [128, 128], F32, kind="ExternalInput").ap()
    ho = nc.dram_tensor("h", [128, SL], F32, kind="ExternalOutput").ap()
    S = Sched(nc)
    CH = min(2048, SL)
    NCH = SL // CH
    xp = S.sbuf("xp", [128, 4 + SL], F32)
    xpb = [Buf() for _ in range(NCH)]
    padb = Buf()
    S.op("dve", lambda e: e.memset(xp[:, 0:4], 0.0), [], [padb])
    for c in range(NCH):
        dma(S, "sp", xp[:, 4 + c * CH:4 + (c + 1) * CH], xc[:, c * CH:(c + 1) * CH], [], [xpb[c]])
    P = S.sbuf("prm", [128, 8], F32)
    Pb = Buf()
    dma(S, "sp", P[:], prm[:, :], [], [Pb])
    wrs = S.sbuf("wrs", [128, 1, 128], BF16)
    wis = S.sbuf("wis", [128, 1, 128], BF16)
    wb = Buf()
    load_cast_weight(S, wr, wrs, wb, 1, 128)
    load_cast_weight(S, wi, wis, wb, 1, 128)
    c8 = S.sbuf("c8", [128, 2], F32)
    c8b = Buf()
    act(S, c8[:, 0:1], P[:, 7:8], AF.Exp, [Pb], [c8b], scale=-1.0)
    act(S, c8[:, 0:1], c8[:, 0:1], AF.Ln, [c8b], [c8b], bias=1.0)
    ts(S, "dve", c8[:, 1:2], c8[:, 0:1], -8.0, None, ALU.mult, None, [c8b], [c8b])

    def T(name, dt_=F32, n=1):
        return [S.sbuf("%s%d" % (name, i), [128, CH], dt_) for i in range(n)], [Buf() for _ in range(n)]
    y, yb = T("y", F32, 2)
    yh, yhb = T("yh", BF16, 1)
    r, rb = T("r", F32, 1)
    ig, igb = T("ig", F32, 1)
    a, ab = T("a", F32, 2)
    u, ub = T("u", F32, 2)
    h, hb = T("h", F32, 2)
    pG = [S.psum("pG%d" % i, [128, 512], F32) for i in range(4)]
    pGb = [Buf() for _ in range(4)]
    pc = 0
    for c in range(NCH):
        k = c % 2
        o = 4 + c * CH
        deps = [xpb[c], padb] + ([xpb[c - 1]] if c else [])
        ts(S, "dve", y[k][:], xp[:, o - 3:o - 3 + CH], P[:, 0:1], P[:, 4:5], ALU.mult, ALU.add, deps + [Pb], [yb[k]])
        for i in (1, 2, 3):
            stt(S, y[k][:], xp[:, o - 3 + i:o - 3 + i + CH], P[:, i:i + 1], y[k][:], ALU.mult, ALU.add,
                deps + [Pb, yb[k]], [yb[k]])
        cp(S, "pool", yh[0][:], y[k][:], [yb[k]], [yhb[0]])
        for gi in range(CH // 512):
            for (wsb_, dst, dstb, bcol) in ((wrs, r, rb, 5), (wis, ig, igb, 6)):
                pk = pc % 4
                pc += 1
                mm(S, pG[pk][:], wsb_[:, 0, :], yh[0][:, gi * 512:(gi + 1) * 512], True, True, [wb, yhb[0]], [pGb[pk]])
                act(S, dst[0][:, gi * 512:(gi + 1) * 512], pG[pk][:], AF.Sigmoid, [pGb[pk], Pb], [dstb[0]],
                    bias=P[:, bcol:bcol + 1])
        act(S, a[k][:], r[0][:], AF.Exp, [rb[0], c8b], [ab[k]], scale=c8[:, 1:2])
        tt(S, "pool", ig[0][:], ig[0][:], y[k][:], ALU.mult, [igb[0], yb[k]], [igb[0]])
        tt(S, "dve", u[k][:], a[k][:], a[k][:], ALU.mult, [ab[k]], [ub[k]])
        ts(S, "dve", u[k][:], u[k][:], -1.0, 1.0, ALU.mult, ALU.add, [ub[k]], [ub[k]])
        act(S, u[k][:], u[k][:], AF.Sqrt, [ub[k]], [ub[k]])
        tt(S, "dve", u[k][:], u[k][:], ig[0][:], ALU.mult, [ub[k], igb[0]], [ub[k]])
        init = 0.0 if c == 0 else h[1 - k][:, CH - 1:CH]
        S.op("dve", lambda e, o_=h[k][:], a_=a[k][:], u_=u[k][:], i_=init: e.tensor_tensor_scan(o_, a_, u_, i_, ALU.mult, ALU.add),
             [ab[k], ub[k]] + ([hb[1 - k]] if c else []), [hb[k]])
        dma(S, "sp", ho[:, c * CH:(c + 1) * CH], h[k][:], [hb[k]], [])
    S.finish()
    return nc


def build_tail(NT):
    nc = new_nc()
    NTOK = NT * 128
    x = nc.dram_tensor("x", [NTOK, D], F32, kind="ExternalInput").ap()
    g = nc.dram_tensor("g", [1, D], F32, kind="ExternalInput").ap()
    w_in = nc.dram_tensor("w_in", [D, DIN], F32, kind="ExternalInput").ap()
    ya = nc.dram_tensor("ya", [NTOK, 512], F32, kind="ExternalInput").ap()
    yb = nc.dram_tensor("yb", [NTOK, 512], F32, kind="ExternalInput").ap()
    hT = nc.dram_tensor("hT", [512, NTOK], F32, kind="ExternalInput").ap()
    w_pa = nc.dram_tensor("w_pa", [512, D], F32, kind="ExternalInput").ap()
    w_pb = nc.dram_tensor("w_pb", [512, D], F32, kind="ExternalInput").ap()
    w_pc = nc.dram_tensor("w_pc", [512, D], F32, kind="ExternalInput").ap()
    w_o = nc.dram_tensor("w_o", [D, D], F32, kind="ExternalInput").ap()
    ident_d = nc.dram_tensor("ident", [128, 128], F32, kind="ExternalInput").ap()
    x1 = nc.dram_tensor("x1", [NTOK, D], F32, kind="ExternalOutput").ap()
    S = Sched(nc)
    identf = S.sbuf("identf", [128, 128], F32)
    ident = S.sbuf("identh", [128, 128], BF16)
    identb = Buf()
    gbc = S.sbuf("gbc", [128, D], F32)
    gb = Buf()
    dma(S, "sp", identf[:], ident_d[:, :], [], [identb])
    cp(S, "dve", ident[:], identf[:], [identb], [identb])
    dma(S, "sp", gbc[:], g[0:1, :].partition_broadcast(128), [], [gb])
    wg = S.sbuf("wg", [128, 8, 3584], BF16)
    wgb = Buf()
    load_cast_weight(S, w_in, wg, wgb, 8, 3584, 0, 4168)
    wp = [S.sbuf("wp%d" % i, [128, 4, D], BF16) for i in range(3)]
    wpb = Buf()
    for i, wd_ in enumerate((w_pa, w_pb, w_pc)):
        load_cast_weight(S, wd_, wp[i], wpb, 4, D)
    wo = S.sbuf("wo", [128, 8, D], BF16)
    wob = Buf()
    load_cast_weight(S, w_o, wo, wob, 8, D)
    G = 4
    GT = 512
    NG = NT // G
    xt = [S.sbuf("xt%d" % i, [128, D], F32) for i in range(G)]
    xtb = [Buf() for _ in range(G)]
    xn = S.sbuf("xn", [128, D], BF16)
    xnb = Buf()
    xnT = S.sbuf("xnT", [128, 8, GT], BF16)
    xnTb = Buf()
    yT = [S.sbuf("yT%d" % i, [128, 4, GT], BF16) for i in range(3)]
    yTb = [Buf() for _ in range(3)]
    yst = [S.sbuf("yst%d" % i, [128, 512], BF16) for i in range(2)]
    ystb = [Buf() for _ in range(2)]
    mT = S.sbuf("mT", [128, 8, GT], BF16)
    mTb = [Buf() for _ in range(8)]
    ht = S.sbuf("ht", [128, GT], F32)
    htb = Buf()
    tmp = [S.sbuf("tmp%d" % i, [128, GT], F32) for i in range(4)]
    tmpb = [Buf() for _ in range(4)]
    sg = [S.sbuf("sg%d" % i, [128, GT], F32) for i in range(3)]
    sgb = [Buf() for _ in range(3)]
    mm_ = [S.sbuf("mm%d" % i, [128, GT], F32) for i in range(3)]
    mmb = [Buf() for _ in range(3)]
    xo = [S.sbuf("xo%d" % i, [128, D], F32) for i in range(2)]
    xob = [Buf() for _ in range(2)]
    junk = S.sbuf("junk", [128, D], BF16)
    junkb = Buf()
    ss = S.sbuf("ss", [128, 4], F32)
    ssb = Buf()
    rstd = S.sbuf("rstd", [128, 4], F32)
    rstdb = Buf()
    pT = [S.psum("pT%d" % i, [128, D], BF16) for i in range(2)]
    pTb = [Buf() for _ in range(2)]
    pM = [S.psum("pM%d" % i, [128, 512], F32) for i in range(4)]
    pMb = [Buf() for _ in range(4)]
    pO = [S.psum("pO%d" % i, [128, 512], F32) for i in range(2)]
    pOb = [Buf() for _ in range(2)]
    cn = {"t": 0, "p": 0, "o": 0, "y": 0, "x": 0}
    C0 = 0.7978845608028654
    for gi in range(NG):
        for t in range(G):
            ti = gi * G + t
            dma(S, "sp", xt[t][:], x[ti * 128:(ti + 1) * 128, :], [], [xtb[t]])
            k = cn["t"] % 2
            cn["t"] += 1
            norm_tile(S, xt[t][:], xtb[t], gbc[:], gb, xn[:], xnb,
                      (junk[:], junkb, ss[:, 0:1], ssb, rstd[:, 0:1], rstdb))
            for c in range(8):
                tr(S, pT[k][:, c * 128:(c + 1) * 128], xn[:, c * 128:(c + 1) * 128], ident[:], [xnb, identb], [pTb[k]])
            cp(S, "act", xnT[:, :, t * 128:(t + 1) * 128], pT[k][:].rearrange("p (c t) -> p c t", c=8), [pTb[k]], [xnTb])
            for bi, ysrc in enumerate((ya, yb)):
                yk = cn["y"] % 2
                cn["y"] += 1
                dma(S, "pool", yst[yk][:], ysrc[ti * 128:(ti + 1) * 128, :], [], [ystb[yk]])
                k = cn["t"] % 2
                cn["t"] += 1
                for c in range(4):
                    tr(S, pT[k][:, c * 128:(c + 1) * 128], yst[yk][:, c * 128:(c + 1) * 128], ident[:], [ystb[yk], identb], [pTb[k]])
                cp(S, "dve", yT[bi][:, :, t * 128:(t + 1) * 128], pT[k][:, 0:512].rearrange("p (c t) -> p c t", c=4), [pTb[k]], [yTb[bi]])
        for cb in range(4):
            pk = cn["p"] % 4
            cn["p"] += 1
            for kc in range(8):
                mm(S, pM[pk][:], wg[:, kc, cb * 128:(cb + 1) * 128], xnT[:, kc, :], kc == 0, kc == 7, [wgb, xnTb], [pMb[pk]])
            dma(S, "sp", ht[:], hT[cb * 128:(cb + 1) * 128, gi * GT:(gi + 1) * GT], [], [htb])
            xs, x2, t3 = tmp[0], tmp[1], tmp[2]
            cp(S, "act", xs[:], pM[pk][:], [pMb[pk]], [tmpb[0]])
            tt(S, "pool", x2[:], xs[:], xs[:], ALU.mult, [tmpb[0]], [tmpb[1]])
            ts(S, "dve", x2[:], x2[:], 0.044715, 1.0, ALU.mult, ALU.add, [tmpb[1]], [tmpb[1]])
            tt(S, "dve", x2[:], x2[:], xs[:], ALU.mult, [tmpb[1], tmpb[0]], [tmpb[1]])
            act(S, t3[:], x2[:], AF.Tanh, [tmpb[1]], [tmpb[2]], scale=C0)
            ts(S, "dve", t3[:], t3[:], 0.5, 0.5, ALU.mult, ALU.add, [tmpb[2]], [tmpb[2]])
            tt(S, "pool", t3[:], t3[:], xs[:], ALU.mult, [tmpb[2], tmpb[0]], [tmpb[2]])
            tt(S, "dve", yT[2][:, cb, :], t3[:], ht[:], ALU.mult, [tmpb[2], htb], [yTb[2]])
        for cb in range(8):
            for br in range(3):
                pk = cn["p"] % 4
                cn["p"] += 1
                c0 = 512 + br * 1024 + cb * 128
                for kc in range(8):
                    mm(S, pM[pk][:], wg[:, kc, c0:c0 + 128], xnT[:, kc, :], kc == 0, kc == 7, [wgb, xnTb], [pMb[pk]])
                act(S, sg[br][:], pM[pk][:], AF.Sigmoid, [pMb[pk]], [sgb[br]])
                pk = cn["p"] % 4
                cn["p"] += 1
                for c in range(4):
                    mm(S, pM[pk][:], wp[br][:, c, cb * 128:(cb + 1) * 128], yT[br][:, c, :], c == 0, c == 3, [wpb, yTb[br]], [pMb[pk]])
                tt(S, "dve", mm_[br][:], pM[pk][:], sg[br][:], ALU.mult, [pMb[pk], sgb[br]], [mmb[br]])
            tt(S, "pool", mm_[0][:], mm_[0][:], mm_[1][:], ALU.add, [mmb[0], mmb[1]], [mmb[0]])
            tt(S, "pool", mT[:, cb, :], mm_[0][:], mm_[2][:], ALU.add, [mmb[0], mmb[2]], [mTb[cb]])
        for t in range(G):
            ti = gi * G + t
            ko = cn["x"] % 2
            cn["x"] += 1
            for half in range(2):
                k = cn["o"] % 2
                cn["o"] += 1
                for cb in range(8):
                    mm(S, pO[k][:], mT[:, cb, t * 128:(t + 1) * 128], wo[:, cb, half * 512:(half + 1) * 512],
                       cb == 0, cb == 7, [mTb[cb], wob], [pOb[k]])
                tt(S, "dve", xo[ko][:, half * 512:(half + 1) * 512], pO[k][:], xt[t][:, half * 512:(half + 1) * 512],
                   ALU.add, [pOb[k], xtb[t]], [xob[ko]])
            dma(S, "sp", x1[ti * 128:(ti + 1) * 128, :], xo[ko][:], [xob[ko]], [])
    S.finish()
    return nc


NEGM = -30000.0
NSEL = 256.0


def build_attn(SL, NT, NIT=22, do_a=True, do_b=True):
    nc = new_nc()
    NTOK = NT * 128
    NKT = SL // 128
    NB = SL // 256
    qt = {"a": nc.dram_tensor("qta", [512, NTOK], BF16, kind="ExternalInput").ap(),
          "b": nc.dram_tensor("qtb", [512, NTOK], BF16, kind="ExternalInput").ap()}
    kt = {"a": nc.dram_tensor("kta", [512, SL], BF16, kind="ExternalInput").ap(),
          "b": nc.dram_tensor("ktb", [512, SL], BF16, kind="ExternalInput").ap()}
    vv = {"a": nc.dram_tensor("va", [8, 128, NKT, 65], BF16, kind="ExternalInput").ap(),
          "b": nc.dram_tensor("vb", [8, 128, NKT, 65], BF16, kind="ExternalInput").ap()}
    qit = nc.dram_tensor("qit", [512, NTOK], BF16, kind="ExternalInput").ap()
    kit = nc.dram_tensor("kit", [64, SL], BF16, kind="ExternalInput").ap()
    wi = nc.dram_tensor("wi", [NTOK, 8], F32, kind="ExternalInput").ap()
    ksum = nc.dram_tensor("ksum", [512, NKT], F32, kind="ExternalInput").ap()
    swt = nc.dram_tensor("swt", [16, 8, 128, 128], F32, kind="ExternalInput").ap()
    b31 = nc.dram_tensor("b31", [128, 16], F32, kind="ExternalInput").ap()
    cm = nc.dram_tensor("cm", [128, 512], F32, kind="ExternalInput").ap()
    vcon = nc.dram_tensor("vcon", [NT, 3, 64], F32, kind="ExternalInput").ap()
    ident_d = nc.dram_tensor("ident", [128, 128], F32, kind="ExternalInput").ap()
    yo = {"a": nc.dram_tensor("ya", [NTOK, 512], F32, kind="ExternalOutput").ap(),
          "b": nc.dram_tensor("yb", [NTOK, 512], F32, kind="ExternalOutput").ap()}
    negB = nc.dram_tensor("negB", [NT, 128, SL], BF16, kind="Internal").ap()
    negBb = [Buf() for _ in range(NT)]
    S = Sched(nc)
    identf = S.sbuf("identf", [128, 128], F32)
    ident = S.sbuf("identh", [128, 128], BF16)
    identb = Buf()
    dma(S, "sp", identf[:], ident_d[:, :], [], [identb])
    cp(S, "dve", ident[:], identf[:], [identb], [identb])
    B31 = S.sbuf("B31", [128, 16], F32)
    CM = S.sbuf("CM", [128, 512], F32)
    WI = S.sbuf("WI", [128, NT, 8], F32)
    half = S.sbuf("half", [128, 1], F32)
    cb_ = Buf()
    dma(S, "sp", B31[:], b31[:, :], [], [cb_])
    dma(S, "sp", CM[:], cm[:, :], [], [cb_])
    dma(S, "sp", WI[:], wi.rearrange("(t p) h -> p t h", p=128), [], [cb_])
    S.op("dve", lambda e: e.memset(half[:], 0.5), [], [cb_])
    sc = S.sbuf("sc", [128, SL], F32)
    scb = Buf()
    neg = S.sbuf("neg", [128, SL], BF16)
    negb = Buf()
    if do_b:
        qim = [S.sbuf("qim%d" % i, [64, 8, 128], BF16) for i in range(2)]
        qimb = [Buf() for _ in range(2)]
        kig = [S.sbuf("kig%d" % i, [64, 512], BF16) for i in range(2)]
        kigb = [Buf() for _ in range(2)]
        dg = S.sbuf("dg", [128, 8, 128], BF16)
        dgb = Buf()
        Rr = [S.sbuf("Rr%d" % i, [128, 512], BF16) for i in range(8)]
        Rrb = [Buf() for _ in range(8)]
        st = S.sbuf("st", [128, 8], F32)
        stb = Buf()
        st2 = S.sbuf("st2", [128, 2], F32)
        st2b = Buf()
        pw2 = S.sbuf("pw2", [128, NIT], F32)
        Wd = S.sbuf("Wd", [128, NIT], F32)
        Wdb = Buf()
        for k_ in range(NIT):
            S.op("pool", lambda e, o=pw2[:, k_:k_ + 1], v=2.0 ** (-k_): e.memset(o, v), [], [cb_])
        pD = [S.psum("pD%d" % i, [128, 512], F32) for i in range(2)]
        pDb = [Buf() for _ in range(2)]
        pC = S.psum("pC", [128, 512], F32)
        pCb = Buf()
        cn = {"d": 0, "k": 0}
        for m in range(NT):
            L = (m + 1) * 512
            qk = m % 2
            dma(S, "sp", qim[qk][:], qit.rearrange("(h d) t -> d h t", d=64)[:, :, m * 128:(m + 1) * 128], [], [qimb[qk]])
            for h in range(8):
                ts(S, "dve", dg[:, h, :], ident[:], WI[:, m, h:h + 1], None, ALU.mult, None, [identb, cb_], [dgb])
            for u in range(m + 1):
                kk = cn["k"] % 2
                cn["k"] += 1
                dma(S, "sp", kig[kk][:], kit[:, u * 512:(u + 1) * 512], [], [kigb[kk]])
                for h in range(8):
                    pk = cn["d"] % 2
                    cn["d"] += 1
                    mm(S, pD[pk][:], qim[qk][:, h, :], kig[kk][:], True, True, [qimb[qk], kigb[kk]], [pDb[pk]])
                    if h % 2 == 0:
                        act(S, Rr[h][:], pD[pk][:], AF.Relu, [pDb[pk]], [Rrb[h]])
                    else:
                        ts(S, "dve", Rr[h][:], pD[pk][:], 0.0, None, ALU.max, None, [pDb[pk]], [Rrb[h]])
                for h in range(8):
                    mm(S, pC[:], dg[:, h, :], Rr[h][:], h == 0, h == 7, [dgb, Rrb[h]], [pCb])
                cp(S, "act", sc[:, u * 512:(u + 1) * 512], pC[:], [pCb], [scb])
            ts(S, "dve", neg[:, :L], sc[:, :L], 1.0, None, ALU.mult, ALU.max, [scb], [negb, stb], accum_out=st[:, 7:8])
            ts(S, "dve", neg[:, :L], sc[:, :L], -1.0, None, ALU.mult, ALU.max, [scb], [negb, stb], accum_out=st[:, 6:7])
            tt(S, "dve", st[:, 7:8], st[:, 7:8], st[:, 6:7], ALU.max, [stb], [stb])
            tt(S, "dve", sc[:, L - 512:L], sc[:, L - 512:L], CM[:], ALU.add, [scb, cb_], [scb])
            ts(S, "dve", st[:, 1:2], st[:, 7:8], 1.0001, 1e-6, ALU.mult, ALU.add, [stb], [stb])
            ts(S, "dve", st[:, 0:1], st[:, 1:2], -1.0, None, ALU.mult, None, [stb], [stb])
            ts(S, "dve", Wd[:], pw2[:], st[:, 1:2], None, ALU.mult, None, [stb, cb_], [Wdb])
            for it in range(NIT):
                ts(S, "dve", st2[:, 0:1], st[:, 0:1], Wd[:, it:it + 1], -1.0, ALU.add, ALU.mult, [stb, Wdb], [st2b])
                act(S, neg[:, :L], sc[:, :L], AF.Sign, [scb, st2b], [negb, st2b], bias=st2[:, 0:1], accum_out=st2[:, 1:2])
                ts(S, "dve", st[:, 4:5], st2[:, 1:2], 2.0 * NSEL - L - 0.5, Wd[:, it:it + 1], ALU.is_ge, ALU.mult, [st2b, Wdb], [stb])
                tt(S, "dve", st[:, 0:1], st[:, 0:1], st[:, 4:5], ALU.add, [stb], [stb])
            ts(S, "dve", neg[:, :L], sc[:, :L], st[:, 0:1], NEGM, ALU.is_lt, ALU.mult, [scb, stb], [negb])
            dma(S, "sp", negB[m, :, 0:L], neg[:, :L], [negb], [negBb[m]])
    kth = S.sbuf("kth", [64, SL], BF16)
    kthb = Buf()
    vth = S.sbuf("vth", [128, NKT, 65], BF16)
    vthb = Buf()
    qth = S.sbuf("qth", [64, NTOK], BF16)
    qthb = Buf()
    swh = S.sbuf("swh", [128, 8, 128], F32)
    swhb = Buf()
    PT = [S.sbuf("PT%d" % i, [128, 512], BF16) for i in range(2)]
    PTb = [Buf() for _ in range(2)]
    ksh = S.sbuf("ksh", [64, NKT], F32)
    kmf = S.sbuf("kmf", [64, NB], F32)
    kmT = S.sbuf("kmT", [64, 64], BF16)
    kmb = Buf()
    vc = S.sbuf("vc", [128, 3, 64], F32)
    vcb = Buf()
    gp = S.sbuf("gp", [128, 64], F32)
    mx8 = S.sbuf("mx8", [128, 8], F32)
    sel = S.sbuf("sel", [128, 64], F32)
    gb_ = Buf()
    rc = S.sbuf("rc", [128, 2], F32)
    yt = [S.sbuf("yt%d" % i, [128, 64], F32) for i in range(2)]
    ytb = [Buf() for _ in range(2)]
    pS = [S.psum("pS%d" % i, [128, 512], F32) for i in range(2)]
    pSb = [Buf() for _ in range(2)]
    pO = [S.psum("pO%d" % i, [128, 128], F32) for i in range(2)]
    pOb = [Buf() for _ in range(2)]
    pG = S.psum("pG", [128, 64], F32)
    pGb = Buf()
    c2 = {"s": 0, "o": 0, "y": 0, "n": 0}
    neg2 = sc[:].bitcast(BF16)
    negs = [(neg, negb), (neg2, scb)]
    S.op("pool", lambda e: e.memset(kmT[:], 0.0), [], [kmb])
    heads = ([("b", h) for h in range(8)] if do_b else []) + ([("a", h) for h in range(8)] if do_a else [])
    for br, h in heads:
        hh = h if br == "a" else 8 + h
        dma(S, "sp", kth[:], kt[br][h * 64:(h + 1) * 64, :], [], [kthb])
        dma(S, "sp", vth[:], vv[br][h], [], [vthb])
        dma(S, "sp", qth[:], qt[br][h * 64:(h + 1) * 64, :], [], [qthb])
        dma(S, "sp", swh[:], swt[hh].rearrange("p k q -> k p q"), [], [swhb])
        if br == "a":
            dma(S, "sp", ksh[:], ksum[h * 64:(h + 1) * 64, :], [], [kmb])
            v2 = ksh[:].rearrange("d (n two) -> d n two", two=2)
            tt(S, "dve", kmf[:], v2[:, :, 0], v2[:, :, 1], ALU.add, [kmb], [kmb])
            ts(S, "dve", kmT[:, :NB], kmf[:], 1.0 / 256.0, None, ALU.mult, None, [kmb], [kmb])
        for m in range(NT):
            L = (m + 1) * 512
            qs = qth[:, m * 128:(m + 1) * 128]
            neg, negb = negs[c2["n"] % 2]
            c2["n"] += 1
            if br == "b":
                dma(S, "sp", neg[:, :L], negB[m, :, 0:L], [negBb[m]], [negb])
            else:
                dma(S, "sp", vc[:], vcon[m:m + 1].partition_broadcast(128), [], [vcb])
                mm(S, pG[:], qs, kmT[:], True, True, [qthb, kmb], [pGb])
                tt(S, "dve", gp[:], pG[:], vc[:, 0, :], ALU.add, [pGb, vcb], [gb_])
                S.op("dve", lambda e: e.max(out=mx8[:], in_=gp[:]), [gb_], [gb_])
                ts(S, "dve", sel[:], gp[:], mx8[:, 2:3], None, ALU.is_ge, None, [gb_], [gb_])
                tt(S, "dve", sel[:], sel[:], vc[:, 1, :], ALU.mult, [gb_, vcb], [gb_])
                tt(S, "dve", sel[:], sel[:], vc[:, 2, :], ALU.max, [gb_, vcb], [gb_])
                ts(S, "dve", sel[:], sel[:], -1.0, -NEGM, ALU.add, ALU.mult, [gb_], [gb_])
                nb = L // 256
                cp(S, "dve", neg[:, :L].rearrange("p (n k) -> p n k", k=256),
                   sel[:, :nb].unsqueeze(2).to_broadcast([128, nb, 256]), [gb_], [negb])
            ok = c2["o"] % 2
            c2["o"] += 1
            for u in range(m + 1):
                win = u >= m - 1
                sk = c2["s"] % 2
                c2["s"] += 1
                for tj in range(4):
                    ktile = 4 * u + tj
                    o_ = pS[sk][:, tj * 128:(tj + 1) * 128]
                    mm(S, o_, kth[:, ktile * 128:(ktile + 1) * 128], qs, True, False, [kthb, qthb], [pSb[sk]])
                    mm(S, o_, neg[:, ktile * 128:(ktile + 1) * 128], ident[:], False, not win, [negb, identb], [pSb[sk]])
                    if win:
                        p = (u - (m - 1)) * 4 + tj
                        mm(S, o_, identf[:], swh[:, p, :], False, True, [identb, swhb], [pSb[sk]])
                if win:
                    act(S, PT[sk][:], pS[sk][:], AF.Exp, [pSb[sk]], [PTb[sk]])
                else:
                    act(S, PT[sk][:], pS[sk][:], AF.Exp, [pSb[sk], cb_], [PTb[sk]], bias=B31[:, hh:hh + 1])
                for tj in range(4):
                    ktile = 4 * u + tj
                    mm(S, pO[ok][:, 0:65], PT[sk][:, tj * 128:(tj + 1) * 128], vth[:, ktile, :],
                       u == 0 and tj == 0, u == m and tj == 3, [PTb[sk], vthb], [pOb[ok]])
            yk = c2["y"] % 2
            c2["y"] += 1
            S.op("dve", lambda e, o=rc[:, 0:1], i=pO[ok][:, 64:65]: e.reciprocal(o, i), [pOb[ok]], [gb_])
            ts(S, "dve", yt[yk][:], pO[ok][:, 0:64], rc[:, 0:1], None, ALU.mult, None, [pOb[ok], gb_], [ytb[yk]])
            dma(S, "sp", yo[br][m * 128:(m + 1) * 128, h * 64:(h + 1) * 64], yt[yk][:], [ytb[yk]], [])
    S.finish()
    return nc

N_BUCKETS = 32
MAX_DISTANCE = 128


def t5_bucket_np(dist):
    n = np.maximum(dist, 0)
    max_exact = N_BUCKETS // 2
    nf = np.maximum(n, 1).astype(np.float32)
    large = max_exact + (np.log(nf / np.float32(max_exact)) / np.float32(math.log(MAX_DISTANCE / max_exact))
                         * np.float32(N_BUCKETS - max_exact)).astype(np.int32)
    large = np.minimum(large, N_BUCKETS - 1)
    return np.where(n < max_exact, n, large)


def attn_consts(j, NT, rel_bias):
    k = np.arange(128)[:, None]
    q = np.arange(128)[None, :]
    swt = np.empty((16, 8, 128, 128), np.float32)
    for p in range(8):
        delta = 4 + j - p
        d = delta * 128 + q - k
        bk = t5_bucket_np(d)
        for hh in range(16):
            swt[hh, p] = np.where(d >= 0, rel_bias[bk, hh], np.float32(-30000.0))
    b31 = np.broadcast_to(rel_bias[31][None, :], (128, 16)).astype(np.float32).copy()
    qq = np.arange(128)[:, None]
    c = np.arange(512)[None, :]
    ktile = c // 128
    cm = np.where((ktile < j) | ((ktile == j) & ((c % 128) <= qq)), np.float32(0.0), np.float32(-1e30)).astype(np.float32)
    vcon = np.zeros((NT, 3, 64), np.float32)
    n = np.arange(64)
    for m in range(NT):
        cur = (4 * m + j) // 2
        vcon[m, 0] = np.where(n < cur, 0.0, -30000.0)
        vcon[m, 1] = (n < cur).astype(np.float32)
        vcon[m, 2] = (n == cur).astype(np.float32)
    return {"swt": swt, "b31": b31, "cm": cm, "vcon": vcon, "ident": np.eye(128, dtype=np.float32)}


def own_tokens(j, NT):
    return (np.arange(NT)[:, None] * 4 + j)[:, :, None].reshape(NT, 1) * 128 + np.arange(128)[None, :]


import ml_dtypes

_PROGS = {}


def _prog(key, fn):
    if key not in _PROGS:
        _PROGS[key] = fn()
    return _PROGS[key]


def _run(nc, in_maps):
    res = run_bass_kernel_spmd(nc, in_maps, core_ids=list(range(8)))
    return res.results


def _blockdiag(w2):
    m = np.zeros((128, 128), np.float32)
    m[:64, :64] = w2[0]
    m[64:, 64:] = w2[1]
    return m


def kernel(x, rel_bias, norm_mix_g, w_in, conv_w, conv_b, w_r, b_r, w_i, b_i, lru_lambda,
           w_pa, w_pb, w_pc, w_o, norm_mlp_g, w_up, w_down, final_norm_g):
    f32 = np.float32
    x = np.asarray(x, f32)
    B, SL, _ = x.shape
    depth = int(np.asarray(w_in).shape[0])
    assert B == 2 and SL % 512 == 0
    NT = SL // 512
    NKT = SL // 128
    NTOK = NT * 128
    rel_bias = np.asarray(rel_bias, f32)
    ident = np.eye(128, dtype=f32)
    toks = [own_tokens(j, NT).reshape(-1) for j in range(4)]
    consts = [attn_consts(j, NT, rel_bias) for j in range(4)]
    x_own = [np.ascontiguousarray(x[c // 4][toks[c % 4]]) for c in range(8)]
    p_proj = _prog(("proj", NT), lambda: build_proj(NT))
    p_attn = _prog(("attn", SL, NT), lambda: build_attn(SL, NT))
    p_rg = _prog(("rg", SL), lambda: build_rglru(SL))
    p_tail = _prog(("tail", NT), lambda: build_tail(NT))
    gfin = np.asarray(final_norm_g, f32).reshape(1, -1)
    for l in range(depth):
        wl = np.ascontiguousarray(np.asarray(w_in[l], f32))
        gmix = np.asarray(norm_mix_g[l], f32).reshape(1, -1)
        r1 = _run(p_proj, [{"x": x_own[c], "g": gmix, "w_in": wl, "ident": ident} for c in range(8)])
        shared = []
        for b in range(2):
            def full_cols(name, rows, dt):
                a = np.empty((rows, SL), dt)
                for j in range(4):
                    a[:, toks[j]] = np.asarray(r1[b * 4 + j][name])[:rows]
                return a
            def full_rows(name, cols, dt):
                a = np.empty((SL, cols), dt)
                for j in range(4):
                    a[toks[j]] = np.asarray(r1[b * 4 + j][name])
                return a
            def vlay(v):
                return np.ascontiguousarray(v.reshape(NKT, 128, 8, 65).transpose(2, 1, 0, 3))
            bf = ml_dtypes.bfloat16
            ks = np.empty((512, NKT), f32)
            for j in range(4):
                ks[:, j::4] = np.asarray(r1[b * 4 + j]["o_ks"])
            shared.append({
                "kta": full_cols("o_ka", 512, bf), "ktb": full_cols("o_kb", 512, bf), "kit": full_cols("o_ki", 64, bf),
                "va": vlay(full_rows("o_va", 520, bf)), "vb": vlay(full_rows("o_vb", 520, bf)),
                "ksum": ks, "xc": full_cols("o_xc", 512, f32)})
        im = []
        for c in range(8):
            b, j = c // 4, c % 4
            d = dict(consts[j])
            sh = shared[b]
            d.update({"qta": np.asarray(r1[c]["o_qa"]), "qtb": np.asarray(r1[c]["o_qb"]), "qit": np.asarray(r1[c]["o_qi"]),
                      "wi": np.asarray(r1[c]["o_wi"]), "kta": sh["kta"], "ktb": sh["ktb"], "kit": sh["kit"],
                      "va": sh["va"], "vb": sh["vb"], "ksum": sh["ksum"]})
            im.append(d)
        r2 = _run(p_attn, im)
        im = []
        for c in range(8):
            b, cc = c // 4, c % 4
            ch = slice(cc * 128, (cc + 1) * 128)
            prm = np.stack([np.asarray(conv_w[l][0], f32)[ch], np.asarray(conv_w[l][1], f32)[ch],
                            np.asarray(conv_w[l][2], f32)[ch], np.asarray(conv_w[l][3], f32)[ch],
                            np.asarray(conv_b[l], f32)[ch], np.asarray(b_r[l], f32)[ch], np.asarray(b_i[l], f32)[ch],
                            np.asarray(lru_lambda[l], f32)[ch]], axis=1)
            im.append({"xc": np.ascontiguousarray(shared[b]["xc"][ch]), "prm": np.ascontiguousarray(prm),
                       "wr": _blockdiag(np.asarray(w_r[l], f32)[2 * cc:2 * cc + 2]),
                       "wi": _blockdiag(np.asarray(w_i[l], f32)[2 * cc:2 * cc + 2])})
        r3 = _run(p_rg, im)
        hfull = [np.concatenate([np.asarray(r3[b * 4 + cc]["h"]) for cc in range(4)], axis=0) for b in range(2)]
        im = []
        for c in range(8):
            b, j = c // 4, c % 4
            im.append({"x": x_own[c], "g": gmix, "w_in": wl, "ya": np.asarray(r2[c]["ya"]), "yb": np.asarray(r2[c]["yb"]),
                       "hT": np.ascontiguousarray(hfull[b][:, toks[j]]),
                       "w_pa": np.asarray(w_pa[l], f32), "w_pb": np.asarray(w_pb[l], f32), "w_pc": np.asarray(w_pc[l], f32),
                       "w_o": np.asarray(w_o[l], f32), "ident": ident})
        r4 = _run(p_tail, im)
        final = l == depth - 1
        p_mlp = _prog(("mlp", NT, final), lambda: build_mlp(NT, final))
        gm = np.asarray(norm_mlp_g[l], f32).reshape(1, -1)
        r5 = _run(p_mlp, [{"x1": np.asarray(r4[c]["x1"]), "g": gm, "gf": gfin, "w_up": np.asarray(w_up[l], f32),
                           "w_dn": np.asarray(w_down[l], f32), "ident": ident} for c in range(8)])
        x_own = [np.asarray(r5[c]["x2"]) for c in range(8)]
    out = np.empty((B, SL, x.shape[2]), f32)
    for c in range(8):
        out[c // 4][toks[c % 4]] = x_own[c]
    return out
```

```python
import math
import contextlib
import numpy as np
import concourse.bass as bass
import concourse.mybir as mybir
from concourse.bass_utils import run_bass_kernel_spmd

F32 = mybir.dt.float32
BF16 = mybir.dt.bfloat16
AF = mybir.ActivationFunctionType
ALU = mybir.AluOpType
AX = mybir.AxisListType

ENGS = ("pe", "act", "dve", "pool", "sp")
NDSEM = 8


class Buf:
    __slots__ = ("name", "last_w", "readers")

    def __init__(self, name=""):
        self.name = name
        self.last_w = None
        self.readers = []


class Ins:
    __slots__ = ("eng", "fn", "deps", "is_dma", "sig", "sigval", "dsem", "dval", "idx")


class Sched:
    def __init__(self, nc, strict_same_engine=("pool", "dve", "act")):
        self.nc = nc
        self.q = {e: [] for e in ENGS}
        self.ndma = {e: 0 for e in ENGS}
        self.dma_last = {e: [None] * NDSEM for e in ENGS}
        self.strict = set(strict_same_engine)
        self.es = contextlib.ExitStack()
        self.n = 0
        self.all_dmas = []

    def sbuf(self, name, shape, dtype):
        return self.es.enter_context(self.nc.sbuf_tensor("sb_" + name, list(shape), dtype))

    def psum(self, name, shape, dtype):
        return self.es.enter_context(self.nc.psum_tensor("ps_" + name, list(shape), dtype))

    def _mk(self, eng, fn, reads, writes, is_dma):
        I = Ins()
        I.eng = eng
        I.fn = fn
        I.is_dma = is_dma
        I.sig = False
        I.sigval = None
        I.dsem = None
        I.dval = None
        I.idx = self.n
        self.n += 1
        deps = []
        for b in reads:
            if b.last_w is not None:
                deps.append(b.last_w)
        for b in writes:
            if b.last_w is not None:
                deps.append(b.last_w)
            deps.extend(b.readers)
        out = []
        seen = set()
        for d in deps:
            if id(d) in seen or d is I:
                continue
            seen.add(id(d))
            if (not d.is_dma) and d.eng == eng and eng not in self.strict:
                continue
            out.append(d)
        I.deps = out
        for b in writes:
            b.last_w = I
            b.readers = []
        for b in reads:
            if b.last_w is not I:
                b.readers.append(I)
        if is_dma:
            k = self.ndma[eng]
            self.ndma[eng] = k + 1
            slot = k % NDSEM
            prev = self.dma_last[eng][slot]
            if prev is not None:
                I.deps.append(prev)
            self.dma_last[eng][slot] = I
            I.dsem = (eng, slot)
            I.dval = 16 * (k // NDSEM + 1)
            self.all_dmas.append(I)
        self.q[eng].append(I)
        return I

    def op(self, eng, fn, reads=(), writes=()):
        return self._mk(eng, fn, list(reads), list(writes), False)

    def dma(self, eng, fn, reads=(), writes=()):
        return self._mk(eng, fn, list(reads), list(writes), True)

    def barrier(self):
        lasts = []
        for e in ENGS:
            comp = [i for i in self.q[e] if not i.is_dma]
            if comp:
                lasts.append(comp[-1])
        dl = []
        for e in ENGS:
            for s in range(NDSEM):
                if self.dma_last[e][s] is not None:
                    dl.append(self.dma_last[e][s])
        for e in ENGS:
            I = Ins()
            I.eng = e
            I.fn = None
            I.is_dma = False
            I.sig = False
            I.sigval = None
            I.dsem = None
            I.dval = None
            I.idx = self.n
            self.n += 1
            I.deps = [d for d in lasts if d.eng != e] + list(dl)
            self.q[e].append(I)

    def finish(self):
        nc = self.nc
        self.barrier()
        es = self.es
        sem = {}
        for e in ("pe", "act", "dve", "pool"):
            sem[e] = es.enter_context(nc.semaphore("s_" + e))
        dsem = {}
        for e in ENGS:
            if self.ndma[e]:
                for s in range(NDSEM):
                    dsem[(e, s)] = es.enter_context(nc.semaphore("d_%s%d" % (e, s)))
        for e in ENGS:
            for I in self.q[e]:
                for d in I.deps:
                    if not d.is_dma:
                        d.sig = True
        for e in ENGS:
            c = 0
            for I in self.q[e]:
                if I.sig:
                    c += 1
                    I.sigval = c
        handles = {"pe": "tensor", "act": "scalar", "dve": "vector", "pool": "gpsimd", "sp": "sync"}
        with nc.Block() as block:
            for e in ENGS:
                lst = self.q[e]
                if not lst:
                    continue

                def body(eng, lst=lst, e=e):
                    waited = {}
                    for I in lst:
                        for d in I.deps:
                            if d.is_dma:
                                key = ("d",) + d.dsem
                                val = d.dval
                                s = dsem[d.dsem]
                            else:
                                key = ("c", d.eng)
                                val = d.sigval
                                s = sem[d.eng]
                            if waited.get(key, 0) >= val:
                                continue
                            waited[key] = val
                            eng.wait_ge(s, val)
                        if I.fn is None:
                            continue
                        r = I.fn(eng)
                        if I.is_dma:
                            r.then_inc(dsem[I.dsem], 16)
                        elif I.sig:
                            r.then_inc(sem[e], 1)

                getattr(block, handles[e])(body)
        es.close()


def mm(S, out, lhsT, rhs, start, stop, R, W):
    return S.op("pe", lambda e: e.matmul(out, lhsT, rhs, start=start, stop=stop), R, W)


def tr(S, out, in_, ident, R, W):
    return S.op("pe", lambda e: e.transpose(out, in_, ident), R, W)


def act(S, out, in_, func, R, W, bias=None, scale=None, accum_out=None, eng="act"):
    kw = {}
    if bias is not None:
        kw["bias"] = bias
    if scale is not None:
        kw["scale"] = scale
    if accum_out is not None:
        kw["accum_out"] = accum_out
    return S.op(eng, lambda e: e.activation(out, in_, func, **kw), R, W)


def ts(S, eng, out, in0, s1, s2, op0, op1, R, W, accum_out=None):
    kw = {}
    if accum_out is not None:
        kw["accum_out"] = accum_out
    if op1 is None:
        return S.op(eng, lambda e: e.tensor_scalar(out, in0, s1, None, op0, **kw), R, W)
    return S.op(eng, lambda e: e.tensor_scalar(out, in0, s1, s2, op0, op1, **kw), R, W)


def tt(S, eng, out, in0, in1, op, R, W):
    return S.op(eng, lambda e: e.tensor_tensor(out, in0, in1, op), R, W)


def stt(S, out, in0, scalar, in1, op0, op1, R, W):
    return S.op("dve", lambda e: e.scalar_tensor_tensor(out, in0, scalar, in1, op0, op1), R, W)


def cp(S, eng, out, in_, R, W):
    if eng == "act":
        return S.op("act", lambda e: e.copy(out, in_), R, W)
    return S.op(eng, lambda e: e.tensor_copy(out, in_), R, W)


def dma(S, eng, out, in_, R, W, **kw):
    return S.dma(eng, lambda e: e.dma_start(out=out, in_=in_, **kw), R, W)


class Ring:
    def __init__(self, items):
        self.items = items
        self.i = 0

    def next(self):
        it = self.items[self.i % len(self.items)]
        self.i += 1
        return it


def rmsnorm_rstd(S, x_ap, xb, junk, junkb, ss, ssb, rstd, rstdb, D, eps):
    act(S, junk, x_ap, AF.Square, [xb], [junkb, ssb], accum_out=ss)
    act(S, rstd, ss, AF.Sqrt, [ssb], [rstdb], bias=None, scale=1.0 / D)
    return None


D = 1024
DFF = 4096
EPS = 1e-6


def new_nc():
    return bass.Bass("TRN2", target_bir_lowering=False)


def load_cast_weight(S, w_dram, w_sb, wb, nk, ncols, row0=0, col0=0):
    for kc in range(nk):
        for c0 in range(0, ncols, 2048):
            cw = min(2048, ncols - c0)
            dma(S, "pool", w_sb[:, kc, c0:c0 + cw],
                w_dram[row0 + kc * 128:row0 + (kc + 1) * 128, col0 + c0:col0 + c0 + cw], [], [wb])


def norm_tile(S, x_ap, xb, g_bc, gb, xn_ap, xnb, scr, D_=D):
    junk, junkb, ss, ssb, rstd, rstdb = scr
    act(S, junk, x_ap, AF.Square, [xb], [junkb, ssb], accum_out=ss)
    ts(S, "dve", rstd, ss, 1.0 / D_, EPS, ALU.mult, ALU.add, [ssb], [rstdb])
    act(S, rstd, rstd, AF.Sqrt, [rstdb], [rstdb])
    S.op("dve", lambda e: e.reciprocal(rstd, rstd), [rstdb], [rstdb])
    stt(S, xn_ap, x_ap, rstd, g_bc, ALU.mult, ALU.mult, [xb, rstdb, gb], [xnb])


def build_mlp(NT, final):
    nc = new_nc()
    NTOK = NT * 128
    x1 = nc.dram_tensor("x1", [NTOK, D], F32, kind="ExternalInput").ap()
    g = nc.dram_tensor("g", [1, D], F32, kind="ExternalInput").ap()
    gf = nc.dram_tensor("gf", [1, D], F32, kind="ExternalInput").ap()
    w_up = nc.dram_tensor("w_up", [D, DFF], F32, kind="ExternalInput").ap()
    w_dn = nc.dram_tensor("w_dn", [DFF, D], F32, kind="ExternalInput").ap()
    ident_d = nc.dram_tensor("ident", [128, 128], F32, kind="ExternalInput").ap()
    x2 = nc.dram_tensor("x2", [NTOK, D], F32, kind="ExternalOutput").ap()
    S = Sched(nc)
    wup = S.sbuf("wup", [128, 8, DFF], BF16)
    wupb = Buf()
    wdn = S.sbuf("wdn", [128, 32, D], BF16)
    wdnb = Buf()
    identf = S.sbuf("identf", [128, 128], F32)
    ident = S.sbuf("identb16", [128, 128], BF16)
    identb = Buf()
    gbc = S.sbuf("gbc", [128, D], F32)
    gfbc = S.sbuf("gfbc", [128, D], F32)
    gb = Buf()
    dma(S, "sp", identf[:], ident_d[:, :], [], [identb])
    cp(S, "dve", ident[:], identf[:], [identb], [identb])
    dma(S, "sp", gbc[:], g[0:1, :].partition_broadcast(128), [], [gb])
    dma(S, "sp", gfbc[:], gf[0:1, :].partition_broadcast(128), [], [gb])
    load_cast_weight(S, w_up, wup, wupb, 8, DFF)
    load_cast_weight(S, w_dn, wdn, wdnb, 32, D)

    G = 2
    GT = G * 128
    NG = NT // G
    xt = [S.sbuf("xt%d" % i, [128, D], F32) for i in range(G)]
    xtb = [Buf() for _ in range(G)]
    xn = [S.sbuf("xn%d" % i, [128, D], BF16) for i in range(1)]
    xnb = [Buf() for _ in range(1)]
    xnT = [S.sbuf("xnT%d" % i, [128, 8, GT], BF16) for i in range(1)]
    xnTb = [Buf() for _ in range(1)]
    hT = S.sbuf("hT", [128, 32, GT], BF16)
    hTb = [Buf() for _ in range(32)]
    rr = [S.sbuf("rr%d" % i, [128, GT], F32) for i in range(2)]
    rrb = [Buf() for _ in range(2)]
    xo = [S.sbuf("xo%d" % i, [128, D], F32) for i in range(2)]
    xob = [Buf() for _ in range(2)]
    junk = S.sbuf("junk", [128, D], BF16)
    junkb = Buf()
    ss = S.sbuf("ss", [128, 4], F32)
    ssb = Buf()
    rstd = S.sbuf("rstd", [128, 4], F32)
    rstdb = Buf()
    pT = [S.psum("pT%d" % i, [128, D], BF16) for i in range(2)]
    pTb = [Buf() for _ in range(2)]
    pU = [S.psum("pU%d" % i, [128, GT], F32) for i in range(3)]
    pUb = [Buf() for _ in range(3)]
    pD = [S.psum("pD%d" % i, [128, 512], F32) for i in range(2)]
    pDb = [Buf() for _ in range(2)]
    cnt = {"t": 0, "u": 0, "d": 0, "o": 0}
    for gi in range(NG):
        par = 0
        for t in range(G):
            ti = gi * G + t
            xs = t
            dma(S, "sp", xt[xs][:], x1[ti * 128:(ti + 1) * 128, :], [], [xtb[xs]])
            k = cnt["t"] % 2
            cnt["t"] += 1
            norm_tile(S, xt[xs][:], xtb[xs], gbc[:], gb, xn[0][:], xnb[0],
                      (junk[:], junkb, ss[:, 0:1], ssb, rstd[:, 0:1], rstdb))
            for c in range(8):
                tr(S, pT[k][:, c * 128:(c + 1) * 128], xn[0][:, c * 128:(c + 1) * 128], ident[:],
                   [xnb[0], identb], [pTb[k]])
            cp(S, "act", xnT[par][:, :, t * 128:(t + 1) * 128],
               pT[k][:].rearrange("p (c t) -> p c t", c=8), [pTb[k]], [xnTb[par]])
        for fb in range(32):
            k = cnt["u"] % 3
            cnt["u"] += 1
            for kc in range(8):
                mm(S, pU[k][:], wup[:, kc, fb * 128:(fb + 1) * 128], xnT[par][:, kc, :],
                   kc == 0, kc == 7, [wupb, xnTb[par]], [pUb[k]])
            act(S, rr[k % 2][:], pU[k][:], AF.Relu, [pUb[k]], [rrb[k % 2]])
            tt(S, "pool", hT[:, fb, :], rr[k % 2][:], rr[k % 2][:], ALU.mult, [rrb[k % 2]], [hTb[fb]])
        for t in range(G):
            ti = gi * G + t
            xs = t
            ko = cnt["o"] % 2
            cnt["o"] += 1
            for half in range(2):
                k = cnt["d"] % 2
                cnt["d"] += 1
                for fb in range(32):
                    mm(S, pD[k][:], hT[:, fb, t * 128:(t + 1) * 128], wdn[:, fb, half * 512:(half + 1) * 512],
                       fb == 0, fb == 31, [hTb[fb], wdnb], [pDb[k]])
                tt(S, "dve", xo[ko][:, half * 512:(half + 1) * 512], pD[k][:],
                   xt[xs][:, half * 512:(half + 1) * 512], ALU.add, [pDb[k], xtb[xs]], [xob[ko]])
            if final:
                norm_tile(S, xo[ko][:], xob[ko], gfbc[:], gb, xt[xs][:], xtb[xs],
                          (junk[:], junkb, ss[:, 1:2], ssb, rstd[:, 1:2], rstdb))
                dma(S, "sp", x2[ti * 128:(ti + 1) * 128, :], xt[xs][:], [xtb[xs]], [])
            else:
                dma(S, "sp", x2[ti * 128:(ti + 1) * 128, :], xo[ko][:], [xob[ko]], [])
    S.finish()
    return nc


HD = 64
IDX_SCALE = (8 ** -0.5) * (64 ** -0.5)
FM_SEGS = [("qa", 0, 512, 0.125), ("ka", 512, 512, 1.0), ("qb", 1536, 512, 0.125), ("kb", 2048, 512, 1.0),
           ("qi", 3072, 512, 1.0), ("ki", 3584, 128, 1.0), ("xc", 3656, 512, 1.0)]
DIN = 7752


def build_proj(NT, skip=()):
    nc = new_nc()
    NTOK = NT * 128
    x = nc.dram_tensor("x", [NTOK, D], F32, kind="ExternalInput").ap()
    g = nc.dram_tensor("g", [1, D], F32, kind="ExternalInput").ap()
    w_in = nc.dram_tensor("w_in", [D, DIN], F32, kind="ExternalInput").ap()
    ident_d = nc.dram_tensor("ident", [128, 128], F32, kind="ExternalInput").ap()
    outs = {}
    for nm, c0, ncol, sc in FM_SEGS:
        dt_ = F32 if nm == "xc" else BF16
        outs[nm] = nc.dram_tensor("o_" + nm, [ncol, NTOK], dt_, kind="ExternalOutput").ap()
    o_va = nc.dram_tensor("o_va", [NTOK, 8 * 65], BF16, kind="ExternalOutput").ap()
    o_vb = nc.dram_tensor("o_vb", [NTOK, 8 * 65], BF16, kind="ExternalOutput").ap()
    o_wi = nc.dram_tensor("o_wi", [NTOK, 8], F32, kind="ExternalOutput").ap()
    o_ks = nc.dram_tensor("o_ks", [512, NT], F32, kind="ExternalOutput").ap()
    S = Sched(nc)
    identf = S.sbuf("identf", [128, 128], F32)
    ident = S.sbuf("identh", [128, 128], BF16)
    identb = Buf()
    gbc = S.sbuf("gbc", [128, D], F32)
    gb = Buf()
    dma(S, "sp", identf[:], ident_d[:, :], [], [identb])
    cp(S, "dve", ident[:], identf[:], [identb], [identb])
    dma(S, "sp", gbc[:], g[0:1, :].partition_broadcast(128), [], [gb])
    xnT = S.sbuf("xnT", [128, 8, NTOK], BF16)
    xnTb = [Buf() for _ in range(NT)]
    xt = [S.sbuf("xt%d" % i, [128, D], F32) for i in range(2)]
    xtb = [Buf() for _ in range(2)]
    xn = S.sbuf("xn", [128, D], BF16)
    xnb = Buf()
    junk = S.sbuf("junk", [128, D], BF16)
    junkb = Buf()
    ss = S.sbuf("ss", [128, 4], F32)
    ssb = Buf()
    rstd = S.sbuf("rstd", [128, 4], F32)
    rstdb = Buf()
    pT = [S.psum("pT%d" % i, [128, D], BF16) for i in range(2)]
    pTb = [Buf() for _ in range(2)]
    pM = [S.psum("pM%d" % i, [128, 512], F32) for i in range(4)]
    pMb = [Buf() for _ in range(4)]
    for ti in range(NT):
        k = ti % 2
        dma(S, "sp", xt[k][:], x[ti * 128:(ti + 1) * 128, :], [], [xtb[k]])
        norm_tile(S, xt[k][:], xtb[k], gbc[:], gb, xn[:], xnb,
                  (junk[:], junkb, ss[:, 0:1], ssb, rstd[:, 0:1], rstdb))
        for c in range(8):
            tr(S, pT[k][:, c * 128:(c + 1) * 128], xn[:, c * 128:(c + 1) * 128], ident[:],
               [xnb, identb], [pTb[k]])
        cp(S, "act", xnT[:, :, ti * 128:(ti + 1) * 128],
           pT[k][:].rearrange("p (c t) -> p c t", c=8), [pTb[k]], [xnTb[ti]])
    wsb = [S.sbuf("wsb%d" % i, [128, 8, 512], BF16) for i in range(2)]
    wsbb = [Buf() for _ in range(2)]
    stg = [S.sbuf("stg%d" % i, [128, NTOK], F32) for i in range(2)]
    stgb = [Buf() for _ in range(2)]
    stgh = [S.sbuf("stgh%d" % i, [128, NTOK], BF16) for i in range(2)]
    stghb = [Buf() for _ in range(2)]
    ks = S.sbuf("ks", [128, 4, NT], F32)
    ksb = Buf()
    NG = (NTOK + 511) // 512
    cn = {"w": 0, "p": 0, "s": 0}
    for nm, c0, ncol, sc in FM_SEGS:
        wk = cn["w"] % 2
        cn["w"] += 1
        load_cast_weight(S, w_in, wsb[wk], wsbb[wk], 8, ncol, 0, c0)
        for cb in range((ncol + 127) // 128):
            cw = min(128, ncol - cb * 128)
            sk = cn["s"] % 2
            cn["s"] += 1
            isf = nm == "xc"
            dst = stg[sk] if isf else stgh[sk]
            dstb = stgb[sk] if isf else stghb[sk]
            for gi in range(NG):
                n0 = gi * 512
                nw = min(512, NTOK - n0)
                pk = cn["p"] % 4
                cn["p"] += 1
                for kc in range(8):
                    mm(S, pM[pk][:cw, :nw], wsb[wk][:, kc, cb * 128:cb * 128 + cw], xnT[:, kc, n0:n0 + nw],
                       kc == 0, kc == 7, [wsbb[wk]] + xnTb, [pMb[pk]])
                if nm == "ka":
                    for t_ in range(nw // 128):
                        ts(S, "dve", dst[:, n0 + t_ * 128:n0 + (t_ + 1) * 128], pM[pk][:, t_ * 128:(t_ + 1) * 128],
                           1.0, None, ALU.mult, ALU.add, [pMb[pk]], [dstb, ksb],
                           accum_out=ks[:, cb, n0 // 128 + t_:n0 // 128 + t_ + 1])
                elif gi % 2 == 0:
                    act(S, dst[:cw, n0:n0 + nw], pM[pk][:cw, :nw], AF.Copy, [pMb[pk]], [dstb], scale=sc)
                else:
                    ts(S, "dve", dst[:cw, n0:n0 + nw], pM[pk][:cw, :nw], sc, None, ALU.mult, None, [pMb[pk]], [dstb])
            dma(S, "sp", outs[nm][cb * 128:cb * 128 + cw, :], dst[:cw, :], [dstb], [])
    if "ks" not in skip:
        dma(S, "sp", o_ks.rearrange("(c p) t -> p c t", p=128), ks[:], [ksb], [])
    vst = [S.sbuf("vst%d" % i, [128, 8, 65], BF16) for i in range(2)]
    vstb = [Buf() for _ in range(2)]
    for i in range(2):
        S.op("pool", lambda e, o=vst[i][:]: e.memset(o, 1.0), [], [vstb[i]])
    wst = S.sbuf("wst", [128, NT, 8], F32)
    wstb = Buf()
    cn["v"] = 0
    for nm, c0, od in ((("va", 1024, o_va), ("vb", 2560, o_vb)) if "v" not in skip else ()):
        wk = cn["w"] % 2
        cn["w"] += 1
        load_cast_weight(S, w_in, wsb[wk], wsbb[wk], 8, 512, 0, c0)
        for ti in range(NT):
            pk = cn["p"] % 4
            cn["p"] += 1
            for kc in range(8):
                mm(S, pM[pk][:], xnT[:, kc, ti * 128:(ti + 1) * 128], wsb[wk][:, kc, :],
                   kc == 0, kc == 7, [wsbb[wk]] + xnTb, [pMb[pk]])
            vk = cn["v"] % 2
            cn["v"] += 1
            cp(S, "act", vst[vk][:, :, 0:64], pM[pk][:].rearrange("p (h d) -> p h d", h=8), [pMb[pk]], [vstb[vk]])
            dma(S, "sp", od[ti * 128:(ti + 1) * 128, :], vst[vk][:].rearrange("p h d -> p (h d)"), [vstb[vk]], [])
    wk = cn["w"] % 2
    cn["w"] += 1
    load_cast_weight(S, w_in, wsb[wk], wsbb[wk], 8, 512, 0, 3648)
    for ti in (range(NT) if "wi" not in skip else ()):
        pk = cn["p"] % 4
        cn["p"] += 1
        for kc in range(8):
            mm(S, pM[pk][:, 0:8], xnT[:, kc, ti * 128:(ti + 1) * 128], wsb[wk][:, kc, 0:8],
               kc == 0, kc == 7, [wsbb[wk]] + xnTb, [pMb[pk]])
        ts(S, "dve", wst[:, ti, :], pM[pk][:, 0:8], IDX_SCALE, None, ALU.mult, None, [pMb[pk]], [wstb])
    if "wi" not in skip:
        dma(S, "sp", o_wi.rearrange("(t p) h -> p t h", p=128), wst[:], [wstb], [])
    S.finish()
    return nc


def build_rglru(SL):
    nc = new_nc()
    xc = nc.dram_tensor("xc", [128, SL], F32, kind="ExternalInput").ap()
    prm = nc.dram_tensor("prm", [128, 8], F32, kind="ExternalInput").ap()
    wr = nc.dram_tensor("wr", [128, 128], F32, kind="ExternalInput").ap()
    wi = nc.dram_tensor("wi", [128, 128], F32, kind="ExternalInput").ap()
    ho = nc.dram_tensor("h", [128, SL], F32, kind="ExternalOutput").ap()
    S = Sched(nc)
    CH = min(2048, SL)
    NCH = SL // CH
    xp = S.sbuf("xp", [128, 4 + SL], F32)
    xpb = [Buf() for _ in range(NCH)]
    padb = Buf()
    S.op("dve", lambda e: e.memset(xp[:, 0:4], 0.0), [], [padb])
    for c in range(NCH):
        dma(S, "sp", xp[:, 4 + c * CH:4 + (c + 1) * CH], xc[:, c * CH:(c + 1) * CH], [], [xpb[c]])
    P = S.sbuf("prm", [128, 8], F32)
    Pb = Buf()
    dma(S, "sp", P[:], prm[:, :], [], [Pb])
    wrs = S.sbuf("wrs", [128, 1, 128], BF16)
    wis = S.sbuf("wis", [128, 1, 128], BF16)
    wb = Buf()
    load_cast_weight(S, wr, wrs, wb, 1, 128)
    load_cast_weight(S, wi, wis, wb, 1, 128)
    c8 = S.sbuf("c8", [128, 2], F32)
    c8b = Buf()
    act(S, c8[:, 0:1], P[:, 7:8], AF.Exp, [Pb], [c8b], scale=-1.0)
    act(S, c8[:, 0:1], c8[:, 0:1], AF.Ln, [c8b], [c8b], bias=1.0)
    ts(S, "dve", c8[:, 1:2], c8[:, 0:1], -8.0, None, ALU.mult, None, [c8b], [c8b])

    def T(name, dt_=F32, n=1):
        return [S.sbuf("%s%d" % (name, i), [128, CH], dt_) for i in range(n)], [Buf() for _ in range(n)]
    y, yb = T("y", F32, 2)
    yh, yhb = T("yh", BF16, 1)
    r, rb = T("r", F32, 1)
    ig, igb = T("ig", F32, 1)
    a, ab = T("a", F32, 2)
    u, ub = T("u", F32, 2)
    h, hb = T("h", F32, 2)
    pG = [S.psum("pG%d" % i, [128, 512], F32) for i in range(4)]
    pGb = [Buf() for _ in range(4)]
    pc = 0
    for c in range(NCH):
        k = c % 2
        o = 4 + c * CH
        deps = [xpb[c], padb] + ([xpb[c - 1]] if c else [])
        ts(S, "dve", y[k][:], xp[:, o - 3:o - 3 + CH], P[:, 0:1], P[:, 4:5], ALU.mult, ALU.add, deps + [Pb], [yb[k]])
        for i in (1, 2, 3):
            stt(S, y[k][:], xp[:, o - 3 + i:o - 3 + i + CH], P[:, i:i + 1], y[k][:], ALU.mult, ALU.add,
                deps + [Pb, yb[k]], [yb[k]])
        cp(S, "pool", yh[0][:], y[k][:], [yb[k]], [yhb[0]])
        for gi in range(CH // 512):
            for (wsb_, dst, dstb, bcol) in ((wrs, r, rb, 5), (wis, ig, igb, 6)):
                pk = pc % 4
                pc += 1
                mm(S, pG[pk][:], wsb_[:, 0, :], yh[0][:, gi * 512:(gi + 1) * 512], True, True, [wb, yhb[0]], [pGb[pk]])
                act(S, dst[0][:, gi * 512:(gi + 1) * 512], pG[pk][:], AF.Sigmoid, [pGb[pk], Pb], [dstb[0]],
                    bias=P[:, bcol:bcol + 1])
        act(S, a[k][:], r[0][:], AF.Exp, [rb[0], c8b], [ab[k]], scale=c8[:, 1:2])
        tt(S, "pool", ig[0][:], ig[0][:], y[k][:], ALU.mult, [igb[0], yb[k]], [igb[0]])
        tt(S, "dve", u[k][:], a[k][:], a[k][:], ALU.mult, [ab[k]], [ub[k]])
        ts(S, "dve", u[k][:], u[k][:], -1.0, 1.0, ALU.mult, ALU.add, [ub[k]], [ub[k]])
        act(S, u[k][:], u[k][:], AF.Sqrt, [ub[k]], [ub[k]])
        tt(S, "dve", u[k][:], u[k][:], ig[0][:], ALU.mult, [ub[k], igb[0]], [ub[k]])
        init = 0.0 if c == 0 else h[1 - k][:, CH - 1:CH]
        S.op("dve", lambda e, o_=h[k][:], a_=a[k][:], u_=u[k][:], i_=init: e.tensor_tensor_scan(o_, a_, u_, i_, ALU.mult, ALU.add),
             [ab[k], ub[k]] + ([hb[1 - k]] if c else []), [hb[k]])
        dma(S, "sp", ho[:, c * CH:(c + 1) * CH], h[k][:], [hb[k]], [])
    S.finish()
    return nc


def build_tail(NT):
    nc = new_nc()
    NTOK = NT * 128
    x = nc.dram_tensor("x", [NTOK, D], F32, kind="ExternalInput").ap()
    g = nc.dram_tensor("g", [1, D], F32, kind="ExternalInput").ap()
    w_in = nc.dram_tensor("w_in", [D, DIN], F32, kind="ExternalInput").ap()
    ya = nc.dram_tensor("ya", [NTOK, 512], F32, kind="ExternalInput").ap()
    yb = nc.dram_tensor("yb", [NTOK, 512], F32, kind="ExternalInput").ap()
    hT = nc.dram_tensor("hT", [512, NTOK], F32, kind="ExternalInput").ap()
    w_pa = nc.dram_tensor("w_pa", [512, D], F32, kind="ExternalInput").ap()
    w_pb = nc.dram_tensor("w_pb", [512, D], F32, kind="ExternalInput").ap()
    w_pc = nc.dram_tensor("w_pc", [512, D], F32, kind="ExternalInput").ap()
    w_o = nc.dram_tensor("w_o", [D, D], F32, kind="ExternalInput").ap()
    ident_d = nc.dram_tensor("ident", [128, 128], F32, kind="ExternalInput").ap()
    x1 = nc.dram_tensor("x1", [NTOK, D], F32, kind="ExternalOutput").ap()
    S = Sched(nc)
    identf = S.sbuf("identf", [128, 128], F32)
    ident = S.sbuf("identh", [128, 128], BF16)
    identb = Buf()
    gbc = S.sbuf("gbc", [128, D], F32)
    gb = Buf()
    dma(S, "sp", identf[:], ident_d[:, :], [], [identb])
    cp(S, "dve", ident[:], identf[:], [identb], [identb])
    dma(S, "sp", gbc[:], g[0:1, :].partition_broadcast(128), [], [gb])
    wg = S.sbuf("wg", [128, 8, 3584], BF16)
    wgb = Buf()
    load_cast_weight(S, w_in, wg, wgb, 8, 3584, 0, 4168)
    wp = [S.sbuf("wp%d" % i, [128, 4, D], BF16) for i in range(3)]
    wpb = Buf()
    for i, wd_ in enumerate((w_pa, w_pb, w_pc)):
        load_cast_weight(S, wd_, wp[i], wpb, 4, D)
    wo = S.sbuf("wo", [128, 8, D], BF16)
    wob = Buf()
    load_cast_weight(S, w_o, wo, wob, 8, D)
    G = 4
    GT = 512
    NG = NT // G
    xt = [S.sbuf("xt%d" % i, [128, D], F32) for i in range(G)]
    xtb = [Buf() for _ in range(G)]
    xn = S.sbuf("xn", [128, D], BF16)
    xnb = Buf()
    xnT = S.sbuf("xnT", [128, 8, GT], BF16)
    xnTb = Buf()
    yT = [S.sbuf("yT%d" % i, [128, 4, GT], BF16) for i in range(3)]
    yTb = [Buf() for _ in range(3)]
    yst = [S.sbuf("yst%d" % i, [128, 512], BF16) for i in range(2)]
    ystb = [Buf() for _ in range(2)]
    mT = S.sbuf("mT", [128, 8, GT], BF16)
    mTb = [Buf() for _ in range(8)]
    ht = S.sbuf("ht", [128, GT], F32)
    htb = Buf()
    tmp = [S.sbuf("tmp%d" % i, [128, GT], F32) for i in range(4)]
    tmpb = [Buf() for _ in range(4)]
    sg = [S.sbuf("sg%d" % i, [128, GT], F32) for i in range(3)]
    sgb = [Buf() for _ in range(3)]
    mm_ = [S.sbuf("mm%d" % i, [128, GT], F32) for i in range(3)]
    mmb = [Buf() for _ in range(3)]
    xo = [S.sbuf("xo%d" % i, [128, D], F32) for i in range(2)]
    xob = [Buf() for _ in range(2)]
    junk = S.sbuf("junk", [128, D], BF16)
    junkb = Buf()
    ss = S.sbuf("ss", [128, 4], F32)
    ssb = Buf()
    rstd = S.sbuf("rstd", [128, 4], F32)
    rstdb = Buf()
    pT = [S.psum("pT%d" % i, [128, D], BF16) for i in range(2)]
    pTb = [Buf() for _ in range(2)]
    pM = [S.psum("pM%d" % i, [128, 512], F32) for i in range(4)]
    pMb = [Buf() for _ in range(4)]
    pO = [S.psum("pO%d" % i, [128, 512], F32) for i in range(2)]
    pOb = [Buf() for _ in range(2)]
    cn = {"t": 0, "p": 0, "o": 0, "y": 0, "x": 0}
    C0 = 0.7978845608028654
    for gi in range(NG):
        for t in range(G):
            ti = gi * G + t
            dma(S, "sp", xt[t][:], x[ti * 128:(ti + 1) * 128, :], [], [xtb[t]])
            k = cn["t"] % 2
            cn["t"] += 1
            norm_tile(S, xt[t][:], xtb[t], gbc[:], gb, xn[:], xnb,
                      (junk[:], junkb, ss[:, 0:1], ssb, rstd[:, 0:1], rstdb))
            for c in range(8):
                tr(S, pT[k][:, c * 128:(c + 1) * 128], xn[:, c * 128:(c + 1) * 128], ident[:], [xnb, identb], [pTb[k]])
            cp(S, "act", xnT[:, :, t * 128:(t + 1) * 128], pT[k][:].rearrange("p (c t) -> p c t", c=8), [pTb[k]], [xnTb])
            for bi, ysrc in enumerate((ya, yb)):
                yk = cn["y"] % 2
                cn["y"] += 1
                dma(S, "pool", yst[yk][:], ysrc[ti * 128:(ti + 1) * 128, :], [], [ystb[yk]])
                k = cn["t"] % 2
                cn["t"] += 1
                for c in range(4):
                    tr(S, pT[k][:, c * 128:(c + 1) * 128], yst[yk][:, c * 128:(c + 1) * 128], ident[:], [ystb[yk], identb], [pTb[k]])
                cp(S, "dve", yT[bi][:, :, t * 128:(t + 1) * 128], pT[k][:, 0:512].rearrange("p (c t) -> p c t", c=4), [pTb[k]], [yTb[bi]])
        for cb in range(4):
            pk = cn["p"] % 4
            cn["p"] += 1
            for kc in range(8):
                mm(S, pM[pk][:], wg[:, kc, cb * 128:(cb + 1) * 128], xnT[:, kc, :], kc == 0, kc == 7, [wgb, xnTb], [pMb[pk]])
            dma(S, "sp", ht[:], hT[cb * 128:(cb + 1) * 128, gi * GT:(gi + 1) * GT], [], [htb])
            xs, x2, t3 = tmp[0], tmp[1], tmp[2]
            cp(S, "act", xs[:], pM[pk][:], [pMb[pk]], [tmpb[0]])
            tt(S, "pool", x2[:], xs[:], xs[:], ALU.mult, [tmpb[0]], [tmpb[1]])
            ts(S, "dve", x2[:], x2[:], 0.044715, 1.0, ALU.mult, ALU.add, [tmpb[1]], [tmpb[1]])
            tt(S, "dve", x2[:], x2[:], xs[:], ALU.mult, [tmpb[1], tmpb[0]], [tmpb[1]])
            act(S, t3[:], x2[:], AF.Tanh, [tmpb[1]], [tmpb[2]], scale=C0)
            ts(S, "dve", t3[:], t3[:], 0.5, 0.5, ALU.mult, ALU.add, [tmpb[2]], [tmpb[2]])
            tt(S, "pool", t3[:], t3[:], xs[:], ALU.mult, [tmpb[2], tmpb[0]], [tmpb[2]])
            tt(S, "dve", yT[2][:, cb, :], t3[:], ht[:], ALU.mult, [tmpb[2], htb], [yTb[2]])
        for cb in range(8):
            for br in range(3):
                pk = cn["p"] % 4
                cn["p"] += 1
                c0 = 512 + br * 1024 + cb * 128
                for kc in range(8):
                    mm(S, pM[pk][:], wg[:, kc, c0:c0 + 128], xnT[:, kc, :], kc == 0, kc == 7, [wgb, xnTb], [pMb[pk]])
                act(S, sg[br][:], pM[pk][:], AF.Sigmoid, [pMb[pk]], [sgb[br]])
                pk = cn["p"] % 4
                cn["p"] += 1
                for c in range(4):
                    mm(S, pM[pk][:], wp[br][:, c, cb * 128:(cb + 1) * 128], yT[br][:, c, :], c == 0, c == 3, [wpb, yTb[br]], [pMb[pk]])
                tt(S, "dve", mm_[br][:], pM[pk][:], sg[br][:], ALU.mult, [pMb[pk], sgb[br]], [mmb[br]])
            tt(S, "pool", mm_[0][:], mm_[0][:], mm_[1][:], ALU.add, [mmb[0], mmb[1]], [mmb[0]])
            tt(S, "pool", mT[:, cb, :], mm_[0][:], mm_[2][:], ALU.add, [mmb[0], mmb[2]], [mTb[cb]])
        for t in range(G):
            ti = gi * G + t
            ko = cn["x"] % 2
            cn["x"] += 1
            for half in range(2):
                k = cn["o"] % 2
                cn["o"] += 1
                for cb in range(8):
                    mm(S, pO[k][:], mT[:, cb, t * 128:(t + 1) * 128], wo[:, cb, half * 512:(half + 1) * 512],
                       cb == 0, cb == 7, [mTb[cb], wob], [pOb[k]])
                tt(S, "dve", xo[ko][:, half * 512:(half + 1) * 512], pO[k][:], xt[t][:, half * 512:(half + 1) * 512],
                   ALU.add, [pOb[k], xtb[t]], [xob[ko]])
            dma(S, "sp", x1[ti * 128:(ti + 1) * 128, :], xo[ko][:], [xob[ko]], [])
    S.finish()
    return nc


NEGM = -30000.0
NSEL = 256.0


def build_attn(SL, NT, NIT=22, do_a=True, do_b=True):
    nc = new_nc()
    NTOK = NT * 128
    NKT = SL // 128
    NB = SL // 256
    qt = {"a": nc.dram_tensor("qta", [512, NTOK], BF16, kind="ExternalInput").ap(),
          "b": nc.dram_tensor("qtb", [512, NTOK], BF16, kind="ExternalInput").ap()}
    kt = {"a": nc.dram_tensor("kta", [512, SL], BF16, kind="ExternalInput").ap(),
          "b": nc.dram_tensor("ktb", [512, SL], BF16, kind="ExternalInput").ap()}
    vv = {"a": nc.dram_tensor("va", [8, 128, NKT, 65], BF16, kind="ExternalInput").ap(),
          "b": nc.dram_tensor("vb", [8, 128, NKT, 65], BF16, kind="ExternalInput").ap()}
    qit = nc.dram_tensor("qit", [512, NTOK], BF16, kind="ExternalInput").ap()
    kit = nc.dram_tensor("kit", [64, SL], BF16, kind="ExternalInput").ap()
    wi = nc.dram_tensor("wi", [NTOK, 8], F32, kind="ExternalInput").ap()
    ksum = nc.dram_tensor("ksum", [512, NKT], F32, kind="ExternalInput").ap()
    swt = nc.dram_tensor("swt", [16, 8, 128, 128], F32, kind="ExternalInput").ap()
    b31 = nc.dram_tensor("b31", [128, 16], F32, kind="ExternalInput").ap()
    cm = nc.dram_tensor("cm", [128, 512], F32, kind="ExternalInput").ap()
    vcon = nc.dram_tensor("vcon", [NT, 3, 64], F32, kind="ExternalInput").ap()
    ident_d = nc.dram_tensor("ident", [128, 128], F32, kind="ExternalInput").ap()
    yo = {"a": nc.dram_tensor("ya", [NTOK, 512], F32, kind="ExternalOutput").ap(),
          "b": nc.dram_tensor("yb", [NTOK, 512], F32, kind="ExternalOutput").ap()}
    negB = nc.dram_tensor("negB", [NT, 128, SL], BF16, kind="Internal").ap()
    negBb = [Buf() for _ in range(NT)]
    S = Sched(nc)
    identf = S.sbuf("identf", [128, 128], F32)
    ident = S.sbuf("identh", [128, 128], BF16)
    identb = Buf()
    dma(S, "sp", identf[:], ident_d[:, :], [], [identb])
    cp(S, "dve", ident[:], identf[:], [identb], [identb])
    B31 = S.sbuf("B31", [128, 16], F32)
    CM = S.sbuf("CM", [128, 512], F32)
    WI = S.sbuf("WI", [128, NT, 8], F32)
    half = S.sbuf("half", [128, 1], F32)
    cb_ = Buf()
    dma(S, "sp", B31[:], b31[:, :], [], [cb_])
    dma(S, "sp", CM[:], cm[:, :], [], [cb_])
    dma(S, "sp", WI[:], wi.rearrange("(t p) h -> p t h", p=128), [], [cb_])
    S.op("dve", lambda e: e.memset(half[:], 0.5), [], [cb_])
    sc = S.sbuf("sc", [128, SL], F32)
    scb = Buf()
    neg = S.sbuf("neg", [128, SL], BF16)
    negb = Buf()
    if do_b:
        qim = [S.sbuf("qim%d" % i, [64, 8, 128], BF16) for i in range(2)]
        qimb = [Buf() for _ in range(2)]
        kig = [S.sbuf("kig%d" % i, [64, 512], BF16) for i in range(2)]
        kigb = [Buf() for _ in range(2)]
        dg = S.sbuf("dg", [128, 8, 128], BF16)
        dgb = Buf()
        Rr = [S.sbuf("Rr%d" % i, [128, 512], BF16) for i in range(8)]
        Rrb = [Buf() for _ in range(8)]
        st = S.sbuf("st", [128, 8], F32)
        stb = Buf()
        st2 = S.sbuf("st2", [128, 2], F32)
        st2b = Buf()
        pw2 = S.sbuf("pw2", [128, NIT], F32)
        Wd = S.sbuf("Wd", [128, NIT], F32)
        Wdb = Buf()
        for k_ in range(NIT):
            S.op("pool", lambda e, o=pw2[:, k_:k_ + 1], v=2.0 ** (-k_): e.memset(o, v), [], [cb_])
        pD = [S.psum("pD%d" % i, [128, 512], F32) for i in range(2)]
        pDb = [Buf() for _ in range(2)]
        pC = S.psum("pC", [128, 512], F32)
        pCb = Buf()
        cn = {"d": 0, "k": 0}
        for m in range(NT):
            L = (m + 1) * 512
            qk = m % 2
            dma(S, "sp", qim[qk][:], qit.rearrange("(h d) t -> d h t", d=64)[:, :, m * 128:(m + 1) * 128], [], [qimb[qk]])
            for h in range(8):
                ts(S, "dve", dg[:, h, :], ident[:], WI[:, m, h:h + 1], None, ALU.mult, None, [identb, cb_], [dgb])
            for u in range(m + 1):
                kk = cn["k"] % 2
                cn["k"] += 1
                dma(S, "sp", kig[kk][:], kit[:, u * 512:(u + 1) * 512], [], [kigb[kk]])
                for h in range(8):
                    pk = cn["d"] % 2
                    cn["d"] += 1
                    mm(S, pD[pk][:], qim[qk][:, h, :], kig[kk][:], True, True, [qimb[qk], kigb[kk]], [pDb[pk]])
                    if h % 2 == 0:
                        act(S, Rr[h][:], pD[pk][:], AF.Relu, [pDb[pk]], [Rrb[h]])
                    else:
                        ts(S, "dve", Rr[h][:], pD[pk][:], 0.0, None, ALU.max, None, [pDb[pk]], [Rrb[h]])
                for h in range(8):
                    mm(S, pC[:], dg[:, h, :], Rr[h][:], h == 0, h == 7, [dgb, Rrb[h]], [pCb])
                cp(S, "act", sc[:, u * 512:(u + 1) * 512], pC[:], [pCb], [scb])
            ts(S, "dve", neg[:, :L], sc[:, :L], 1.0, None, ALU.mult, ALU.max, [scb], [negb, stb], accum_out=st[:, 7:8])
            ts(S, "dve", neg[:, :L], sc[:, :L], -1.0, None, ALU.mult, ALU.max, [scb], [negb, stb], accum_out=st[:, 6:7])
            tt(S, "dve", st[:, 7:8], st[:, 7:8], st[:, 6:7], ALU.max, [stb], [stb])
            tt(S, "dve", sc[:, L - 512:L], sc[:, L - 512:L], CM[:], ALU.add, [scb, cb_], [scb])
            ts(S, "dve", st[:, 1:2], st[:, 7:8], 1.0001, 1e-6, ALU.mult, ALU.add, [stb], [stb])
            ts(S, "dve", st[:, 0:1], st[:, 1:2], -1.0, None, ALU.mult, None, [stb], [stb])
            ts(S, "dve", Wd[:], pw2[:], st[:, 1:2], None, ALU.mult, None, [stb, cb_], [Wdb])
            for it in range(NIT):
                ts(S, "dve", st2[:, 0:1], st[:, 0:1], Wd[:, it:it + 1], -1.0, ALU.add, ALU.mult, [stb, Wdb], [st2b])
                act(S, neg[:, :L], sc[:, :L], AF.Sign, [scb, st2b], [negb, st2b], bias=st2[:, 0:1], accum_out=st2[:, 1:2])
                ts(S, "dve", st[:, 4:5], st2[:, 1:2], 2.0 * NSEL - L - 0.5, Wd[:, it:it + 1], ALU.is_ge, ALU.mult, [st2b, Wdb], [stb])
                tt(S, "dve", st[:, 0:1], st[:, 0:1], st[:, 4:5], ALU.add, [stb], [stb])
            ts(S, "dve", neg[:, :L], sc[:, :L], st[:, 0:1], NEGM, ALU.is_lt, ALU.mult, [scb, stb], [negb])
            dma(S, "sp", negB[m, :, 0:L], neg[:, :L], [negb], [negBb[m]])
    kth = S.sbuf("kth", [64, SL], BF16)
    kthb = Buf()
    vth = S.sbuf("vth", [128, NKT, 65], BF16)
    vthb = Buf()
    qth = S.sbuf("qth", [64, NTOK], BF16)
    qthb = Buf()
    swh = S.sbuf("swh", [128, 8, 128], F32)
    swhb = Buf()
    PT = [S.sbuf("PT%d" % i, [128, 512], BF16) for i in range(2)]
    PTb = [Buf() for _ in range(2)]
    ksh = S.sbuf("ksh", [64, NKT], F32)
    kmf = S.sbuf("kmf", [64, NB], F32)
    kmT = S.sbuf("kmT", [64, 64], BF16)
    kmb = Buf()
    vc = S.sbuf("vc", [128, 3, 64], F32)
    vcb = Buf()
    gp = S.sbuf("gp", [128, 64], F32)
    mx8 = S.sbuf("mx8", [128, 8], F32)
    sel = S.sbuf("sel", [128, 64], F32)
    gb_ = Buf()
    rc = S.sbuf("rc", [128, 2], F32)
    yt = [S.sbuf("yt%d" % i, [128, 64], F32) for i in range(2)]
    ytb = [Buf() for _ in range(2)]
    pS = [S.psum("pS%d" % i, [128, 512], F32) for i in range(2)]
    pSb = [Buf() for _ in range(2)]
    pO = [S.psum("pO%d" % i, [128, 128], F32) for i in range(2)]
    pOb = [Buf() for _ in range(2)]
    pG = S.psum("pG", [128, 64], F32)
    pGb = Buf()
    c2 = {"s": 0, "o": 0, "y": 0, "n": 0}
    neg2 = sc[:].bitcast(BF16)
    negs = [(neg, negb), (neg2, scb)]
    S.op("pool", lambda e: e.memset(kmT[:], 0.0), [], [kmb])
    heads = ([("b", h) for h in range(8)] if do_b else []) + ([("a", h) for h in range(8)] if do_a else [])
    for br, h in heads:
        hh = h if br == "a" else 8 + h
        dma(S, "sp", kth[:], kt[br][h * 64:(h + 1) * 64, :], [], [kthb])
        dma(S, "sp", vth[:], vv[br][h], [], [vthb])
        dma(S, "sp", qth[:], qt[br][h * 64:(h + 1) * 64, :], [], [qthb])
        dma(S, "sp", swh[:], swt[hh].rearrange("p k q -> k p q"), [], [swhb])
        if br == "a":
            dma(S, "sp", ksh[:], ksum[h * 64:(h + 1) * 64, :], [], [kmb])
            v2 = ksh[:].rearrange("d (n two) -> d n two", two=2)
            tt(S, "dve", kmf[:], v2[:, :, 0], v2[:, :, 1], ALU.add, [kmb], [kmb])
            ts(S, "dve", kmT[:, :NB], kmf[:], 1.0 / 256.0, None, ALU.mult, None, [kmb], [kmb])
        for m in range(NT):
            L = (m + 1) * 512
            qs = qth[:, m * 128:(m + 1) * 128]
            neg, negb = negs[c2["n"] % 2]
            c2["n"] += 1
            if br == "b":
                dma(S, "sp", neg[:, :L], negB[m, :, 0:L], [negBb[m]], [negb])
            else:
                dma(S, "sp", vc[:], vcon[m:m + 1].partition_broadcast(128), [], [vcb])
                mm(S, pG[:], qs, kmT[:], True, True, [qthb, kmb], [pGb])
                tt(S, "dve", gp[:], pG[:], vc[:, 0, :], ALU.add, [pGb, vcb], [gb_])
                S.op("dve", lambda e: e.max(out=mx8[:], in_=gp[:]), [gb_], [gb_])
                ts(S, "dve", sel[:], gp[:], mx8[:, 2:3], None, ALU.is_ge, None, [gb_], [gb_])
                tt(S, "dve", sel[:], sel[:], vc[:, 1, :], ALU.mult, [gb_, vcb], [gb_])
                tt(S, "dve", sel[:], sel[:], vc[:, 2, :], ALU.max, [gb_, vcb], [gb_])
                ts(S, "dve", sel[:], sel[:], -1.0, -NEGM, ALU.add, ALU.mult, [gb_], [gb_])
                nb = L // 256
                cp(S, "dve", neg[:, :L].rearrange("p (n k) -> p n k", k=256),
                   sel[:, :nb].unsqueeze(2).to_broadcast([128, nb, 256]), [gb_], [negb])
            ok = c2["o"] % 2
            c2["o"] += 1

            def emit_qk(u, sk):
                win = u >= m - 1
                for tj in range(4):
                    ktile = 4 * u + tj
                    o_ = pS[sk][:, tj * 128:(tj + 1) * 128]
                    mm(S, o_, kth[:, ktile * 128:(ktile + 1) * 128], qs, True, False, [kthb, qthb], [pSb[sk]])
                    mm(S, o_, neg[:, ktile * 128:(ktile + 1) * 128], ident[:], False, not win, [negb, identb], [pSb[sk]])
                    if win:
                        p = (u - (m - 1)) * 4 + tj
                        mm(S, o_, identf[:], swh[:, p, :], False, True, [identb, swhb], [pSb[sk]])

            sks = []
            for u in range(m + 1):
                sks.append(c2["s"] % 2)
                c2["s"] += 1
            emit_qk(0, sks[0])
            for u in range(m + 1):
                win = u >= m - 1
                sk = sks[u]
                if win:
                    act(S, PT[sk][:], pS[sk][:], AF.Exp, [pSb[sk]], [PTb[sk]])
                else:
                    act(S, PT[sk][:], pS[sk][:], AF.Exp, [pSb[sk], cb_], [PTb[sk]], bias=B31[:, hh:hh + 1])
                if u + 1 <= m:
                    emit_qk(u + 1, sks[u + 1])
                for tj in range(4):
                    ktile = 4 * u + tj
                    mm(S, pO[ok][:, 0:65], PT[sk][:, tj * 128:(tj + 1) * 128], vth[:, ktile, :],
                       u == 0 and tj == 0, u == m and tj == 3, [PTb[sk], vthb], [pOb[ok]])
            yk = c2["y"] % 2
            c2["y"] += 1
            S.op("dve", lambda e, o=rc[:, 0:1], i=pO[ok][:, 64:65]: e.reciprocal(o, i), [pOb[ok]], [gb_])
            ts(S, "dve", yt[yk][:], pO[ok][:, 0:64], rc[:, 0:1], None, ALU.mult, None, [pOb[ok], gb_], [ytb[yk]])
            dma(S, "sp", yo[br][m * 128:(m + 1) * 128, h * 64:(h + 1) * 64], yt[yk][:], [ytb[yk]], [])
    S.finish()
    return nc

N_BUCKETS = 32
MAX_DISTANCE = 128


def t5_bucket_np(dist):
    n = np.maximum(dist, 0)
    max_exact = N_BUCKETS // 2
    nf = np.maximum(n, 1).astype(np.float32)
    large = max_exact + (np.log(nf / np.float32(max_exact)) / np.float32(math.log(MAX_DISTANCE / max_exact))
                         * np.float32(N_BUCKETS - max_exact)).astype(np.int32)
    large = np.minimum(large, N_BUCKETS - 1)
    return np.where(n < max_exact, n, large)


def attn_consts(j, NT, rel_bias):
    k = np.arange(128)[:, None]
    q = np.arange(128)[None, :]
    swt = np.empty((16, 8, 128, 128), np.float32)
    for p in range(8):
        delta = 4 + j - p
        d = delta * 128 + q - k
        bk = t5_bucket_np(d)
        for hh in range(16):
            swt[hh, p] = np.where(d >= 0, rel_bias[bk, hh], np.float32(-30000.0))
    b31 = np.broadcast_to(rel_bias[31][None, :], (128, 16)).astype(np.float32).copy()
    qq = np.arange(128)[:, None]
    c = np.arange(512)[None, :]
    ktile = c // 128
    cm = np.where((ktile < j) | ((ktile == j) & ((c % 128) <= qq)), np.float32(0.0), np.float32(-1e30)).astype(np.float32)
    vcon = np.zeros((NT, 3, 64), np.float32)
    n = np.arange(64)
    for m in range(NT):
        cur = (4 * m + j) // 2
        vcon[m, 0] = np.where(n < cur, 0.0, -30000.0)
        vcon[m, 1] = (n < cur).astype(np.float32)
        vcon[m, 2] = (n == cur).astype(np.float32)
    return {"swt": swt, "b31": b31, "cm": cm, "vcon": vcon, "ident": np.eye(128, dtype=np.float32)}


def own_tokens(j, NT):
    return (np.arange(NT)[:, None] * 4 + j)[:, :, None].reshape(NT, 1) * 128 + np.arange(128)[None, :]


import ml_dtypes

_PROGS = {}


def _prog(key, fn):
    if key not in _PROGS:
        _PROGS[key] = fn()
    return _PROGS[key]


def _run(nc, in_maps):
    res = run_bass_kernel_spmd(nc, in_maps, core_ids=list(range(8)))
    return res.results


def _blockdiag(w2):
    m = np.zeros((128, 128), np.float32)
    m[:64, :64] = w2[0]
    m[64:, 64:] = w2[1]
    return m


def kernel(x, rel_bias, norm_mix_g, w_in, conv_w, conv_b, w_r, b_r, w_i, b_i, lru_lambda,
           w_pa, w_pb, w_pc, w_o, norm_mlp_g, w_up, w_down, final_norm_g):
    f32 = np.float32
    x = np.asarray(x, f32)
    B, SL, _ = x.shape
    depth = int(np.asarray(w_in).shape[0])
    assert B == 2 and SL % 512 == 0
    NT = SL // 512
    NKT = SL // 128
    NTOK = NT * 128
    rel_bias = np.asarray(rel_bias, f32)
    ident = np.eye(128, dtype=f32)
    toks = [own_tokens(j, NT).reshape(-1) for j in range(4)]
    consts = [attn_consts(j, NT, rel_bias) for j in range(4)]
    x_own = [np.ascontiguousarray(x[c // 4][toks[c % 4]]) for c in range(8)]
    p_proj = _prog(("proj", NT), lambda: build_proj(NT))
    p_attn = _prog(("attn", SL, NT), lambda: build_attn(SL, NT))
    p_rg = _prog(("rg", SL), lambda: build_rglru(SL))
    p_tail = _prog(("tail", NT), lambda: build_tail(NT))
    gfin = np.asarray(final_norm_g, f32).reshape(1, -1)
    for l in range(depth):
        wl = np.ascontiguousarray(np.asarray(w_in[l], f32))
        gmix = np.asarray(norm_mix_g[l], f32).reshape(1, -1)
        r1 = _run(p_proj, [{"x": x_own[c], "g": gmix, "w_in": wl, "ident": ident} for c in range(8)])
        shared = []
        for b in range(2):
            def full_cols(name, rows, dt):
                a = np.empty((rows, SL), dt)
                for j in range(4):
                    a[:, toks[j]] = np.asarray(r1[b * 4 + j][name])[:rows]
                return a
            def full_rows(name, cols, dt):
                a = np.empty((SL, cols), dt)
                for j in range(4):
                    a[toks[j]] = np.asarray(r1[b * 4 + j][name])
                return a
            def vlay(v):
                return np.ascontiguousarray(v.reshape(NKT, 128, 8, 65).transpose(2, 1, 0, 3))
            bf = ml_dtypes.bfloat16
            ks = np.empty((512, NKT), f32)
            for j in range(4):
                ks[:, j::4] = np.asarray(r1[b * 4 + j]["o_ks"])
            shared.append({
                "kta": full_cols("o_ka", 512, bf), "ktb": full_cols("o_kb", 512, bf), "kit": full_cols("o_ki", 64, bf),
                "va": vlay(full_rows("o_va", 520, bf)), "vb": vlay(full_rows("o_vb", 520, bf)),
                "ksum": ks, "xc": full_cols("o_xc", 512, f32)})
        im = []
        for c in range(8):
            b, j = c // 4, c % 4
            d = dict(consts[j])
            sh = shared[b]
            d.update({"qta": np.asarray(r1[c]["o_qa"]), "qtb": np.asarray(r1[c]["o_qb"]), "qit": np.asarray(r1[c]["o_qi"]),
                      "wi": np.asarray(r1[c]["o_wi"]), "kta": sh["kta"], "ktb": sh["ktb"], "kit": sh["kit"],
                      "va": sh["va"], "vb": sh["vb"], "ksum": sh["ksum"]})
            im.append(d)
        r2 = _run(p_attn, im)
        im = []
        for c in range(8):
            b, cc = c // 4, c % 4
            ch = slice(cc * 128, (cc + 1) * 128)
            prm = np.stack([np.asarray(conv_w[l][0], f32)[ch], np.asarray(conv_w[l][1], f32)[ch],
                            np.asarray(conv_w[l][2], f32)[ch], np.asarray(conv_w[l][3], f32)[ch],
                            np.asarray(conv_b[l], f32)[ch], np.asarray(b_r[l], f32)[ch], np.asarray(b_i[l], f32)[ch],
                            np.asarray(lru_lambda[l], f32)[ch]], axis=1)
            im.append({"xc": np.ascontiguousarray(shared[b]["xc"][ch]), "prm": np.ascontiguousarray(prm),
                       "wr": _blockdiag(np.asarray(w_r[l], f32)[2 * cc:2 * cc + 2]),
                       "wi": _blockdiag(np.asarray(w_i[l], f32)[2 * cc:2 * cc + 2])})
        r3 = _run(p_rg, im)
        hfull = [np.concatenate([np.asarray(r3[b * 4 + cc]["h"]) for cc in range(4)], axis=0) for b in range(2)]
        im = []
        for c in range(8):
            b, j = c // 4, c % 4
            im.append({"x": x_own[c], "g": gmix, "w_in": wl, "ya": np.asarray(r2[c]["ya"]), "yb": np.asarray(r2[c]["yb"]),
                       "hT": np.ascontiguousarray(hfull[b][:, toks[j]]),
                       "w_pa": np.asarray(w_pa[l], f32), "w_pb": np.asarray(w_pb[l], f32), "w_pc": np.asarray(w_pc[l], f32),
                       "w_o": np.asarray(w_o[l], f32), "ident": ident})
        r4 = _run(p_tail, im)
        final = l == depth - 1
        p_mlp = _prog(("mlp", NT, final), lambda: build_mlp(NT, final))
        gm = np.asarray(norm_mlp_g[l], f32).reshape(1, -1)
        r5 = _run(p_mlp, [{"x1": np.asarray(r4[c]["x1"]), "g": gm, "gf": gfin, "w_up": np.asarray(w_up[l], f32),
                           "w_dn": np.asarray(w_down[l], f32), "ident": ident} for c in range(8)])
        x_own = [np.asarray(r5[c]["x2"]) for c in range(8)]
    out = np.empty((B, SL, x.shape[2]), f32)
    for c in range(8):
        out[c // 4][toks[c % 4]] = x_own[c]
    return out
```

```python
import math
import contextlib
import numpy as np
import concourse.bass as bass
import concourse.mybir as mybir
from concourse.bass_utils import run_bass_kernel_spmd

F32 = mybir.dt.float32
BF16 = mybir.dt.bfloat16
AF = mybir.ActivationFunctionType
ALU = mybir.AluOpType
AX = mybir.AxisListType

ENGS = ("pe", "act", "dve", "pool", "sp")
NDSEM = 8


class Buf:
    __slots__ = ("name", "last_w", "readers")

    def __init__(self, name=""):
        self.name = name
        self.last_w = None
        self.readers = []


class Ins:
    __slots__ = ("eng", "fn", "deps", "is_dma", "sig", "sigval", "dsem", "dval", "idx")


class Sched:
    def __init__(self, nc, strict_same_engine=("pool", "dve", "act")):
        self.nc = nc
        self.q = {e: [] for e in ENGS}
        self.ndma = {e: 0 for e in ENGS}
        self.dma_last = {e: [None] * NDSEM for e in ENGS}
        self.strict = set(strict_same_engine)
        self.es = contextlib.ExitStack()
        self.n = 0
        self.all_dmas = []

    def sbuf(self, name, shape, dtype):
        return self.es.enter_context(self.nc.sbuf_tensor("sb_" + name, list(shape), dtype))

    def psum(self, name, shape, dtype):
        return self.es.enter_context(self.nc.psum_tensor("ps_" + name, list(shape), dtype))

    def _mk(self, eng, fn, reads, writes, is_dma):
        I = Ins()
        I.eng = eng
        I.fn = fn
        I.is_dma = is_dma
        I.sig = False
        I.sigval = None
        I.dsem = None
        I.dval = None
        I.idx = self.n
        self.n += 1
        deps = []
        for b in reads:
            if b.last_w is not None:
                deps.append(b.last_w)
        for b in writes:
            if b.last_w is not None:
                deps.append(b.last_w)
            deps.extend(b.readers)
        out = []
        seen = set()
        for d in deps:
            if id(d) in seen or d is I:
                continue
            seen.add(id(d))
            if (not d.is_dma) and d.eng == eng and eng not in self.strict:
                continue
            out.append(d)
        I.deps = out
        for b in writes:
            b.last_w = I
            b.readers = []
        for b in reads:
            if b.last_w is not I:
                b.readers.append(I)
        if is_dma:
            k = self.ndma[eng]
            self.ndma[eng] = k + 1
            slot = k % NDSEM
            prev = self.dma_last[eng][slot]
            if prev is not None:
                I.deps.append(prev)
            self.dma_last[eng][slot] = I
            I.dsem = (eng, slot)
            I.dval = 16 * (k // NDSEM + 1)
            self.all_dmas.append(I)
        self.q[eng].append(I)
        return I

    def op(self, eng, fn, reads=(), writes=()):
        return self._mk(eng, fn, list(reads), list(writes), False)

    def dma(self, eng, fn, reads=(), writes=()):
        return self._mk(eng, fn, list(reads), list(writes), True)

    def barrier(self):
        lasts = []
        for e in ENGS:
            comp = [i for i in self.q[e] if not i.is_dma]
            if comp:
                lasts.append(comp[-1])
        dl = []
        for e in ENGS:
            for s in range(NDSEM):
                if self.dma_last[e][s] is not None:
                    dl.append(self.dma_last[e][s])
        for e in ENGS:
            I = Ins()
            I.eng = e
            I.fn = None
            I.is_dma = False
            I.sig = False
            I.sigval = None
            I.dsem = None
            I.dval = None
            I.idx = self.n
            self.n += 1
            I.deps = [d for d in lasts if d.eng != e] + list(dl)
            self.q[e].append(I)

    def finish(self):
        nc = self.nc
        self.barrier()
        es = self.es
        sem = {}
        for e in ("pe", "act", "dve", "pool"):
            sem[e] = es.enter_context(nc.semaphore("s_" + e))
        dsem = {}
        for e in ENGS:
            if self.ndma[e]:
                for s in range(NDSEM):
                    dsem[(e, s)] = es.enter_context(nc.semaphore("d_%s%d" % (e, s)))
        for e in ENGS:
            for I in self.q[e]:
                for d in I.deps:
                    if not d.is_dma:
                        d.sig = True
        for e in ENGS:
            c = 0
            for I in self.q[e]:
                if I.sig:
                    c += 1
                    I.sigval = c
        handles = {"pe": "tensor", "act": "scalar", "dve": "vector", "pool": "gpsimd", "sp": "sync"}
        with nc.Block() as block:
            for e in ENGS:
                lst = self.q[e]
                if not lst:
                    continue

                def body(eng, lst=lst, e=e):
                    waited = {}
                    for I in lst:
                        for d in I.deps:
                            if d.is_dma:
                                key = ("d",) + d.dsem
                                val = d.dval
                                s = dsem[d.dsem]
                            else:
                                key = ("c", d.eng)
                                val = d.sigval
                                s = sem[d.eng]
                            if waited.get(key, 0) >= val:
                                continue
                            waited[key] = val
                            eng.wait_ge(s, val)
                        if I.fn is None:
                            continue
                        r = I.fn(eng)
                        if I.is_dma:
                            r.then_inc(dsem[I.dsem], 16)
                        elif I.sig:
                            r.then_inc(sem[e], 1)

                getattr(block, handles[e])(body)
        es.close()


def mm(S, out, lhsT, rhs, start, stop, R, W):
    return S.op("pe", lambda e: e.matmul(out, lhsT, rhs, start=start, stop=stop), R, W)


def tr(S, out, in_, ident, R, W):
    return S.op("pe", lambda e: e.transpose(out, in_, ident), R, W)


def act(S, out, in_, func, R, W, bias=None, scale=None, accum_out=None, eng="act"):
    kw = {}
    if bias is not None:
        kw["bias"] = bias
    if scale is not None:
        kw["scale"] = scale
    if accum_out is not None:
        kw["accum_out"] = accum_out
    return S.op(eng, lambda e: e.activation(out, in_, func, **kw), R, W)


def ts(S, eng, out, in0, s1, s2, op0, op1, R, W, accum_out=None):
    kw = {}
    if accum_out is not None:
        kw["accum_out"] = accum_out
    if op1 is None:
        return S.op(eng, lambda e: e.tensor_scalar(out, in0, s1, None, op0, **kw), R, W)
    return S.op(eng, lambda e: e.tensor_scalar(out, in0, s1, s2, op0, op1, **kw), R, W)


def tt(S, eng, out, in0, in1, op, R, W):
    return S.op(eng, lambda e: e.tensor_tensor(out, in0, in1, op), R, W)


def stt(S, out, in0, scalar, in1, op0, op1, R, W):
    return S.op("dve", lambda e: e.scalar_tensor_tensor(out, in0, scalar, in1, op0, op1), R, W)


def cp(S, eng, out, in_, R, W):
    if eng == "act":
        return S.op("act", lambda e: e.copy(out, in_), R, W)
    return S.op(eng, lambda e: e.tensor_copy(out, in_), R, W)


def dma(S, eng, out, in_, R, W, **kw):
    return S.dma(eng, lambda e: e.dma_start(out=out, in_=in_, **kw), R, W)


class Ring:
    def __init__(self, items):
        self.items = items
        self.i = 0

    def next(self):
        it = self.items[self.i % len(self.items)]
        self.i += 1
        return it


def rmsnorm_rstd(S, x_ap, xb, junk, junkb, ss, ssb, rstd, rstdb, D, eps):
    act(S, junk, x_ap, AF.Square, [xb], [junkb, ssb], accum_out=ss)
    act(S, rstd, ss, AF.Sqrt, [ssb], [rstdb], bias=None, scale=1.0 / D)
    return None


D = 1024
DFF = 4096
EPS = 1e-6


def new_nc():
    return bass.Bass("TRN2", target_bir_lowering=False)


def load_cast_weight(S, w_dram, w_sb, wb, nk, ncols, row0=0, col0=0):
    for kc in range(nk):
        for c0 in range(0, ncols, 2048):
            cw = min(2048, ncols - c0)
            dma(S, "pool", w_sb[:, kc, c0:c0 + cw],
                w_dram[row0 + kc * 128:row0 + (kc + 1) * 128, col0 + c0:col0 + c0 + cw], [], [wb])


def norm_tile(S, x_ap, xb, g_bc, gb, xn_ap, xnb, scr, D_=D):
    junk, junkb, ss, ssb, rstd, rstdb = scr
    act(S, junk, x_ap, AF.Square, [xb], [junkb, ssb], accum_out=ss)
    ts(S, "dve", rstd, ss, 1.0 / D_, EPS, ALU.mult, ALU.add, [ssb], [rstdb])
    act(S, rstd, rstd, AF.Sqrt, [rstdb], [rstdb])
    S.op("dve", lambda e: e.reciprocal(rstd, rstd), [rstdb], [rstdb])
    stt(S, xn_ap, x_ap, rstd, g_bc, ALU.mult, ALU.mult, [xb, rstdb, gb], [xnb])


def build_mlp(NT, final):
    nc = new_nc()
    NTOK = NT * 128
    x1 = nc.dram_tensor("x1", [NTOK, D], F32, kind="ExternalInput").ap()
    g = nc.dram_tensor("g", [1, D], F32, kind="ExternalInput").ap()
    gf = nc.dram_tensor("gf", [1, D], F32, kind="ExternalInput").ap()
    w_up = nc.dram_tensor("w_up", [D, DFF], F32, kind="ExternalInput").ap()
    w_dn = nc.dram_tensor("w_dn", [DFF, D], F32, kind="ExternalInput").ap()
    ident_d = nc.dram_tensor("ident", [128, 128], F32, kind="ExternalInput").ap()
    x2 = nc.dram_tensor("x2", [NTOK, D], F32, kind="ExternalOutput").ap()
    S = Sched(nc)
    wup = S.sbuf("wup", [128, 8, DFF], BF16)
    wupb = Buf()
    wdn = S.sbuf("wdn", [128, 32, D], BF16)
    wdnb = Buf()
    identf = S.sbuf("identf", [128, 128], F32)
    ident = S.sbuf("identb16", [128, 128], BF16)
    identb = Buf()
    gbc = S.sbuf("gbc", [128, D], F32)
    gfbc = S.sbuf("gfbc", [128, D], F32)
    gb = Buf()
    dma(S, "sp", identf[:], ident_d[:, :], [], [identb])
    cp(S, "dve", ident[:], identf[:], [identb], [identb])
    dma(S, "sp", gbc[:], g[0:1, :].partition_broadcast(128), [], [gb])
    dma(S, "sp", gfbc[:], gf[0:1, :].partition_broadcast(128), [], [gb])
    load_cast_weight(S, w_up, wup, wupb, 8, DFF)
    load_cast_weight(S, w_dn, wdn, wdnb, 32, D)

    G = 2
    GT = G * 128
    NG = NT // G
    xt = [S.sbuf("xt%d" % i, [128, D], F32) for i in range(G)]
    xtb = [Buf() for _ in range(G)]
    xn = [S.sbuf("xn%d" % i, [128, D], BF16) for i in range(1)]
    xnb = [Buf() for _ in range(1)]
    xnT = [S.sbuf("xnT%d" % i, [128, 8, GT], BF16) for i in range(1)]
    xnTb = [Buf() for _ in range(1)]
    hT = S.sbuf("hT", [128, 32, GT], BF16)
    hTb = [Buf() for _ in range(32)]
    rr = [S.sbuf("rr%d" % i, [128, GT], F32) for i in range(2)]
    rrb = [Buf() for _ in range(2)]
    xo = [S.sbuf("xo%d" % i, [128, D], F32) for i in range(2)]
    xob = [Buf() for _ in range(2)]
    junk = S.sbuf("junk", [128, D], BF16)
    junkb = Buf()
    ss = S.sbuf("ss", [128, 4], F32)
    ssb = Buf()
    rstd = S.sbuf("rstd", [128, 4], F32)
    rstdb = Buf()
    pT = [S.psum("pT%d" % i, [128, D], BF16) for i in range(2)]
    pTb = [Buf() for _ in range(2)]
    pU = [S.psum("pU%d" % i, [128, GT], F32) for i in range(3)]
    pUb = [Buf() for _ in range(3)]
    pD = [S.psum("pD%d" % i, [128, 512], F32) for i in range(2)]
    pDb = [Buf() for _ in range(2)]
    cnt = {"t": 0, "u": 0, "d": 0, "o": 0}
    for gi in range(NG):
        par = 0
        for t in range(G):
            ti = gi * G + t
            xs = t
            dma(S, "sp", xt[xs][:], x1[ti * 128:(ti + 1) * 128, :], [], [xtb[xs]])
            k = cnt["t"] % 2
            cnt["t"] += 1
            norm_tile(S, xt[xs][:], xtb[xs], gbc[:], gb, xn[0][:], xnb[0],
                      (junk[:], junkb, ss[:, 0:1], ssb, rstd[:, 0:1], rstdb))
            for c in range(8):
                tr(S, pT[k][:, c * 128:(c + 1) * 128], xn[0][:, c * 128:(c + 1) * 128], ident[:],
                   [xnb[0], identb], [pTb[k]])
            cp(S, "act", xnT[par][:, :, t * 128:(t + 1) * 128],
               pT[k][:].rearrange("p (c t) -> p c t", c=8), [pTb[k]], [xnTb[par]])
        for fb in range(32):
            k = cnt["u"] % 3
            cnt["u"] += 1
            for kc in range(8):
                mm(S, pU[k][:], wup[:, kc, fb * 128:(fb + 1) * 128], xnT[par][:, kc, :],
                   kc == 0, kc == 7, [wupb, xnTb[par]], [pUb[k]])
            act(S, rr[k % 2][:], pU[k][:], AF.Relu, [pUb[k]], [rrb[k % 2]])
            tt(S, "pool", hT[:, fb, :], rr[k % 2][:], rr[k % 2][:], ALU.mult, [rrb[k % 2]], [hTb[fb]])
        for t in range(G):
            ti = gi * G + t
            xs = t
            ko = cnt["o"] % 2
            cnt["o"] += 1
            for half in range(2):
                k = cnt["d"] % 2
                cnt["d"] += 1
                for fb in range(32):
                    mm(S, pD[k][:], hT[:, fb, t * 128:(t + 1) * 128], wdn[:, fb, half * 512:(half + 1) * 512],
                       fb == 0, fb == 31, [hTb[fb], wdnb], [pDb[k]])
                tt(S, "dve", xo[ko][:, half * 512:(half + 1) * 512], pD[k][:],
                   xt[xs][:, half * 512:(half + 1) * 512], ALU.add, [pDb[k], xtb[xs]], [xob[ko]])
            if final:
                norm_tile(S, xo[ko][:], xob[ko], gfbc[:], gb, xt[xs][:], xtb[xs],
                          (junk[:], junkb, ss[:, 1:2], ssb, rstd[:, 1:2], rstdb))
                dma(S, "sp", x2[ti * 128:(ti + 1) * 128, :], xt[xs][:], [xtb[xs]], [])
            else:
                dma(S, "sp", x2[ti * 128:(ti + 1) * 128, :], xo[ko][:], [xob[ko]], [])
    S.finish()
    return nc


HD = 64
IDX_SCALE = (8 ** -0.5) * (64 ** -0.5)
FM_SEGS = [("qa", 0, 512, 0.125), ("ka", 512, 512, 1.0), ("qb", 1536, 512, 0.125), ("kb", 2048, 512, 1.0),
           ("qi", 3072, 512, 1.0), ("ki", 3584, 128, 1.0), ("xc", 3656, 512, 1.0)]
DIN = 7752


def build_proj(NT, skip=()):
    nc = new_nc()
    NTOK = NT * 128
    x = nc.dram_tensor("x", [NTOK, D], F32, kind="ExternalInput").ap()
    g = nc.dram_tensor("g", [1, D], F32, kind="ExternalInput").ap()
    w_in = nc.dram_tensor("w_in", [D, DIN], F32, kind="ExternalInput").ap()
    ident_d = nc.dram_tensor("ident", [128, 128], F32, kind="ExternalInput").ap()
    outs = {}
    for nm, c0, ncol, sc in FM_SEGS:
        dt_ = F32 if nm == "xc" else BF16
        outs[nm] = nc.dram_tensor("o_" + nm, [ncol, NTOK], dt_, kind="ExternalOutput").ap()
    o_va = nc.dram_tensor("o_va", [NTOK, 8 * 65], BF16, kind="ExternalOutput").ap()
    o_vb = nc.dram_tensor("o_vb", [NTOK, 8 * 65], BF16, kind="ExternalOutput").ap()
    o_wi = nc.dram_tensor("o_wi", [NTOK, 8], F32, kind="ExternalOutput").ap()
    o_ks = nc.dram_tensor("o_ks", [512, NT], F32, kind="ExternalOutput").ap()
    S = Sched(nc)
    identf = S.sbuf("identf", [128, 128], F32)
    ident = S.sbuf("identh", [128, 128], BF16)
    identb = Buf()
    gbc = S.sbuf("gbc", [128, D], F32)
    gb = Buf()
    dma(S, "sp", identf[:], ident_d[:, :], [], [identb])
    cp(S, "dve", ident[:], identf[:], [identb], [identb])
    dma(S, "sp", gbc[:], g[0:1, :].partition_broadcast(128), [], [gb])
    xnT = S.sbuf("xnT", [128, 8, NTOK], BF16)
    xnTb = [Buf() for _ in range(NT)]
    xt = [S.sbuf("xt%d" % i, [128, D], F32) for i in range(2)]
    xtb = [Buf() for _ in range(2)]
    xn = S.sbuf("xn", [128, D], BF16)
    xnb = Buf()
    junk = S.sbuf("junk", [128, D], BF16)
    junkb = Buf()
    ss = S.sbuf("ss", [128, 4], F32)
    ssb = Buf()
    rstd = S.sbuf("rstd", [128, 4], F32)
    rstdb = Buf()
    pT = [S.psum("pT%d" % i, [128, D], BF16) for i in range(2)]
    pTb = [Buf() for _ in range(2)]
    pM = [S.psum("pM%d" % i, [128, 512], F32) for i in range(4)]
    pMb = [Buf() for _ in range(4)]
    for ti in range(NT):
        k = ti % 2
        dma(S, "sp", xt[k][:], x[ti * 128:(ti + 1) * 128, :], [], [xtb[k]])
        norm_tile(S, xt[k][:], xtb[k], gbc[:], gb, xn[:], xnb,
                  (junk[:], junkb, ss[:, 0:1], ssb, rstd[:, 0:1], rstdb))
        for c in range(8):
            tr(S, pT[k][:, c * 128:(c + 1) * 128], xn[:, c * 128:(c + 1) * 128], ident[:],
               [xnb, identb], [pTb[k]])
        cp(S, "act", xnT[:, :, ti * 128:(ti + 1) * 128],
           pT[k][:].rearrange("p (c t) -> p c t", c=8), [pTb[k]], [xnTb[ti]])
    wsb = [S.sbuf("wsb%d" % i, [128, 8, 512], BF16) for i in range(2)]
    wsbb = [Buf() for _ in range(2)]
    stg = [S.sbuf("stg%d" % i, [128, NTOK], F32) for i in range(2)]
    stgb = [Buf() for _ in range(2)]
    stgh = [S.sbuf("stgh%d" % i, [128, NTOK], BF16) for i in range(2)]
    stghb = [Buf() for _ in range(2)]
    ks = S.sbuf("ks", [128, 4, NT], F32)
    ksb = Buf()
    NG = (NTOK + 511) // 512
    cn = {"w": 0, "p": 0, "s": 0}
    for nm, c0, ncol, sc in FM_SEGS:
        wk = cn["w"] % 2
        cn["w"] += 1
        load_cast_weight(S, w_in, wsb[wk], wsbb[wk], 8, ncol, 0, c0)
        for cb in range((ncol + 127) // 128):
            cw = min(128, ncol - cb * 128)
            sk = cn["s"] % 2
            cn["s"] += 1
            isf = nm == "xc"
            dst = stg[sk] if isf else stgh[sk]
            dstb = stgb[sk] if isf else stghb[sk]
            for gi in range(NG):
                n0 = gi * 512
                nw = min(512, NTOK - n0)
                pk = cn["p"] % 4
                cn["p"] += 1
                for kc in range(8):
                    mm(S, pM[pk][:cw, :nw], wsb[wk][:, kc, cb * 128:cb * 128 + cw], xnT[:, kc, n0:n0 + nw],
                       kc == 0, kc == 7, [wsbb[wk]] + xnTb, [pMb[pk]])
                if nm == "ka":
                    for t_ in range(nw // 128):
                        ts(S, "dve", dst[:, n0 + t_ * 128:n0 + (t_ + 1) * 128], pM[pk][:, t_ * 128:(t_ + 1) * 128],
                           1.0, None, ALU.mult, ALU.add, [pMb[pk]], [dstb, ksb],
                           accum_out=ks[:, cb, n0 // 128 + t_:n0 // 128 + t_ + 1])
                elif gi % 2 == 0:
                    act(S, dst[:cw, n0:n0 + nw], pM[pk][:cw, :nw], AF.Copy, [pMb[pk]], [dstb], scale=sc)
                else:
                    ts(S, "dve", dst[:cw, n0:n0 + nw], pM[pk][:cw, :nw], sc, None, ALU.mult, None, [pMb[pk]], [dstb])
            dma(S, "sp", outs[nm][cb * 128:cb * 128 + cw, :], dst[:cw, :], [dstb], [])
    if "ks" not in skip:
        dma(S, "sp", o_ks.rearrange("(c p) t -> p c t", p=128), ks[:], [ksb], [])
    vst = [S.sbuf("vst%d" % i, [128, 8, 65], BF16) for i in range(2)]
    vstb = [Buf() for _ in range(2)]
    for i in range(2):
        S.op("pool", lambda e, o=vst[i][:]: e.memset(o, 1.0), [], [vstb[i]])
    wst = S.sbuf("wst", [128, NT, 8], F32)
    wstb = Buf()
    cn["v"] = 0
    for nm, c0, od in ((("va", 1024, o_va), ("vb", 2560, o_vb)) if "v" not in skip else ()):
        wk = cn["w"] % 2
        cn["w"] += 1
        load_cast_weight(S, w_in, wsb[wk], wsbb[wk], 8, 512, 0, c0)
        for ti in range(NT):
            pk = cn["p"] % 4
            cn["p"] += 1
            for kc in range(8):
                mm(S, pM[pk][:], xnT[:, kc, ti * 128:(ti + 1) * 128], wsb[wk][:, kc, :],
                   kc == 0, kc == 7, [wsbb[wk]] + xnTb, [pMb[pk]])
            vk = cn["v"] % 2
            cn["v"] += 1
            cp(S, "act", vst[vk][:, :, 0:64], pM[pk][:].rearrange("p (h d) -> p h d", h=8), [pMb[pk]], [vstb[vk]])
            dma(S, "sp", od[ti * 128:(ti + 1) * 128, :], vst[vk][:].rearrange("p h d -> p (h d)"), [vstb[vk]], [])
    wk = cn["w"] % 2
    cn["w"] += 1
    load_cast_weight(S, w_in, wsb[wk], wsbb[wk], 8, 512, 0, 3648)
    for ti in (range(NT) if "wi" not in skip else ()):
        pk = cn["p"] % 4
        cn["p"] += 1
        for kc in range(8):
            mm(S, pM[pk][:, 0:8], xnT[:, kc, ti * 128:(ti + 1) * 128], wsb[wk][:, kc, 0:8],
               kc == 0, kc == 7, [wsbb[wk]] + xnTb, [pMb[pk]])
        ts(S, "dve", wst[:, ti, :], pM[pk][:, 0:8], IDX_SCALE, None, ALU.mult, None, [pMb[pk]], [wstb])
    if "wi" not in skip:
        dma(S, "sp", o_wi.rearrange("(t p) h -> p t h", p=128), wst[:], [wstb], [])
    S.finish()
    return nc


def build_rglru(SL):
    nc = new_nc()
    xc = nc.dram_tensor("xc", [128, SL], F32, kind="ExternalInput").ap()
    prm = nc.dram_tensor("prm", [128, 8], F32, kind="ExternalInput").ap()
    wr = nc.dram_tensor("wr", [128, 128], F32, kind="ExternalInput").ap()
    wi = nc.dram_tensor("wi", [128, 128], F32, kind="ExternalInput").ap()
    ho = nc.dram_tensor("h", [128, SL], F32, kind="ExternalOutput").ap()
    S = Sched(nc)
    CH = min(2048, SL)
    NCH = SL // CH
    xp = S.sbuf("xp", [128, 4 + SL], F32)
    xpb = [Buf() for _ in range(NCH)]
    padb = Buf()
    S.op("dve", lambda e: e.memset(xp[:, 0:4], 0.0), [], [padb])
    for c in range(NCH):
        dma(S, "sp", xp[:, 4 + c * CH:4 + (c + 1) * CH], xc[:, c * CH:(c + 1) * CH], [], [xpb[c]])
    P = S.sbuf("prm", [128, 8], F32)
    Pb = Buf()
    dma(S, "sp", P[:], prm[:, :], [], [Pb])
    wrs = S.sbuf("wrs", [128, 1, 128], BF16)
    wis = S.sbuf("wis", [128, 1, 128], BF16)
    wb = Buf()
    load_cast_weight(S, wr, wrs, wb, 1, 128)
    load_cast_weight(S, wi, wis, wb, 1, 128)
    c8 = S.sbuf("c8", [128, 2], F32)
    c8b = Buf()
    act(S, c8[:, 0:1], P[:, 7:8], AF.Exp, [Pb], [c8b], scale=-1.0)
    act(S, c8[:, 0:1], c8[:, 0:1], AF.Ln, [c8b], [c8b], bias=1.0)
    ts(S, "dve", c8[:, 1:2], c8[:, 0:1], -8.0, None, ALU.mult, None, [c8b], [c8b])

    def T(name, dt_=F32, n=1):
        return [S.sbuf("%s%d" % (name, i), [128, CH], dt_) for i in range(n)], [Buf() for _ in range(n)]
    y, yb = T("y", F32, 2)
    yh, yhb = T("yh", BF16, 1)
    r, rb = T("r", F32, 1)
    ig, igb = T("ig", F32, 1)
    a, ab = T("a", F32, 2)
    u, ub = T("u", F32, 2)
    h, hb = T("h", F32, 2)
    pG = [S.psum("pG%d" % i, [128, 512], F32) for i in range(4)]
    pGb = [Buf() for _ in range(4)]
    pc = 0
    for c in range(NCH):
        k = c % 2
        o = 4 + c * CH
        deps = [xpb[c], padb] + ([xpb[c - 1]] if c else [])
        ts(S, "dve", y[k][:], xp[:, o - 3:o - 3 + CH], P[:, 0:1], P[:, 4:5], ALU.mult, ALU.add, deps + [Pb], [yb[k]])
        for i in (1, 2, 3):
            stt(S, y[k][:], xp[:, o - 3 + i:o - 3 + i + CH], P[:, i:i + 1], y[k][:], ALU.mult, ALU.add,
                deps + [Pb, yb[k]], [yb[k]])
        cp(S, "pool", yh[0][:], y[k][:], [yb[k]], [yhb[0]])
        for gi in range(CH // 512):
            for (wsb_, dst, dstb, bcol) in ((wrs, r, rb, 5), (wis, ig, igb, 6)):
                pk = pc % 4
                pc += 1
                mm(S, pG[pk][:], wsb_[:, 0, :], yh[0][:, gi * 512:(gi + 1) * 512], True, True, [wb, yhb[0]], [pGb[pk]])
                act(S, dst[0][:, gi * 512:(gi + 1) * 512], pG[pk][:], AF.Sigmoid, [pGb[pk], Pb], [dstb[0]],
                    bias=P[:, bcol:bcol + 1])
        act(S, a[k][:], r[0][:], AF.Exp, [rb[0], c8b], [ab[k]], scale=c8[:, 1:2])
        tt(S, "pool", ig[0][:], ig[0][:], y[k][:], ALU.mult, [igb[0], yb[k]], [igb[0]])
        tt(S, "dve", u[k][:], a[k][:], a[k][:], ALU.mult, [ab[k]], [ub[k]])
        ts(S, "dve", u[k][:], u[k][:], -1.0, 1.0, ALU.mult, ALU.add, [ub[k]], [ub[k]])
        act(S, u[k][:], u[k][:], AF.Sqrt, [ub[k]], [ub[k]])
        tt(S, "dve", u[k][:], u[k][:], ig[0][:], ALU.mult, [ub[k], igb[0]], [ub[k]])
        init = 0.0 if c == 0 else h[1 - k][:, CH - 1:CH]
        S.op("dve", lambda e, o_=h[k][:], a_=a[k][:], u_=u[k][:], i_=init: e.tensor_tensor_scan(o_, a_, u_, i_, ALU.mult, ALU.add),
             [ab[k], ub[k]] + ([hb[1 - k]] if c else []), [hb[k]])
        dma(S, "sp", ho[:, c * CH:(c + 1) * CH], h[k][:], [hb[k]], [])
    S.finish()
    return nc


def build_tail(NT):
    nc = new_nc()
    NTOK = NT * 128
    x = nc.dram_tensor("x", [NTOK, D], F32, kind="ExternalInput").ap()
    g = nc.dram_tensor("g", [1, D], F32, kind="ExternalInput").ap()
    w_in = nc.dram_tensor("w_in", [D, DIN], F32, kind="ExternalInput").ap()
    ya = nc.dram_tensor("ya", [NTOK, 512], F32, kind="ExternalInput").ap()
    yb = nc.dram_tensor("yb", [NTOK, 512], F32, kind="ExternalInput").ap()
    hT = nc.dram_tensor("hT", [512, NTOK], F32, kind="ExternalInput").ap()
    w_pa = nc.dram_tensor("w_pa", [512, D], F32, kind="ExternalInput").ap()
    w_pb = nc.dram_tensor("w_pb", [512, D], F32, kind="ExternalInput").ap()
    w_pc = nc.dram_tensor("w_pc", [512, D], F32, kind="ExternalInput").ap()
    w_o = nc.dram_tensor("w_o", [D, D], F32, kind="ExternalInput").ap()
    ident_d = nc.dram_tensor("ident", [128, 128], F32, kind="ExternalInput").ap()
    x1 = nc.dram_tensor("x1", [NTOK, D], F32, kind="ExternalOutput").ap()
    S = Sched(nc)
    identf = S.sbuf("identf", [128, 128], F32)
    ident = S.sbuf("identh", [128, 128], BF16)
    identb = Buf()
    gbc = S.sbuf("gbc", [128, D], F32)
    gb = Buf()
    dma(S, "sp", identf[:], ident_d[:, :], [], [identb])
    cp(S, "dve", ident[:], identf[:], [identb], [identb])
    dma(S, "sp", gbc[:], g[0:1, :].partition_broadcast(128), [], [gb])
    wg = S.sbuf("wg", [128, 8, 3584], BF16)
    wgb = Buf()
    load_cast_weight(S, w_in, wg, wgb, 8, 3584, 0, 4168)
    wp = [S.sbuf("wp%d" % i, [128, 4, D], BF16) for i in range(3)]
    wpb = Buf()
    for i, wd_ in enumerate((w_pa, w_pb, w_pc)):
        load_cast_weight(S, wd_, wp[i], wpb, 4, D)
    wo = S.sbuf("wo", [128, 8, D], BF16)
    wob = Buf()
    load_cast_weight(S, w_o, wo, wob, 8, D)
    G = 4
    GT = 512
    NG = NT // G
    xt = [S.sbuf("xt%d" % i, [128, D], F32) for i in range(G)]
    xtb = [Buf() for _ in range(G)]
    xn = S.sbuf("xn", [128, D], BF16)
    xnb = Buf()
    xnT = S.sbuf("xnT", [128, 8, GT], BF16)
    xnTb = Buf()
    yT = [S.sbuf("yT%d" % i, [128, 4, GT], BF16) for i in range(3)]
    yTb = [Buf() for _ in range(3)]
    yst = [S.sbuf("yst%d" % i, [128, 512], BF16) for i in range(2)]
    ystb = [Buf() for _ in range(2)]
    mT = S.sbuf("mT", [128, 8, GT], BF16)
    mTb = [Buf() for _ in range(8)]
    ht = S.sbuf("ht", [128, GT], F32)
    htb = Buf()
    tmp = [S.sbuf("tmp%d" % i, [128, GT], F32) for i in range(4)]
    tmpb = [Buf() for _ in range(4)]
    sg = [S.sbuf("sg%d" % i, [128, GT], F32) for i in range(3)]
    sgb = [Buf() for _ in range(3)]
    mm_ = [S.sbuf("mm%d" % i, [128, GT], F32) for i in range(3)]
    mmb = [Buf() for _ in range(3)]
    xo = [S.sbuf("xo%d" % i, [128, D], F32) for i in range(2)]
    xob = [Buf() for _ in range(2)]
    junk = S.sbuf("junk", [128, D], BF16)
    junkb = Buf()
    ss = S.sbuf("ss", [128, 4], F32)
    ssb = Buf()
    rstd = S.sbuf("rstd", [128, 4], F32)
    rstdb = Buf()
    pT = [S.psum("pT%d" % i, [128, D], BF16) for i in range(2)]
    pTb = [Buf() for _ in range(2)]
    pM = [S.psum("pM%d" % i, [128, 512], F32) for i in range(4)]
    pMb = [Buf() for _ in range(4)]
    pO = [S.psum("pO%d" % i, [128, 512], F32) for i in range(2)]
    pOb = [Buf() for _ in range(2)]
    cn = {"t": 0, "p": 0, "o": 0, "y": 0, "x": 0}
    C0 = 0.7978845608028654
    for gi in range(NG):
        for t in range(G):
            ti = gi * G + t
            dma(S, "sp", xt[t][:], x[ti * 128:(ti + 1) * 128, :], [], [xtb[t]])
            k = cn["t"] % 2
            cn["t"] += 1
            norm_tile(S, xt[t][:], xtb[t], gbc[:], gb, xn[:], xnb,
                      (junk[:], junkb, ss[:, 0:1], ssb, rstd[:, 0:1], rstdb))
            for c in range(8):
                tr(S, pT[k][:, c * 128:(c + 1) * 128], xn[:, c * 128:(c + 1) * 128], ident[:], [xnb, identb], [pTb[k]])
            cp(S, "act", xnT[:, :, t * 128:(t + 1) * 128], pT[k][:].rearrange("p (c t) -> p c t", c=8), [pTb[k]], [xnTb])
            for bi, ysrc in enumerate((ya, yb)):
                yk = cn["y"] % 2
                cn["y"] += 1
                dma(S, "pool", yst[yk][:], ysrc[ti * 128:(ti + 1) * 128, :], [], [ystb[yk]])
                k = cn["t"] % 2
                cn["t"] += 1
                for c in range(4):
                    tr(S, pT[k][:, c * 128:(c + 1) * 128], yst[yk][:, c * 128:(c + 1) * 128], ident[:], [ystb[yk], identb], [pTb[k]])
                cp(S, "dve", yT[bi][:, :, t * 128:(t + 1) * 128], pT[k][:, 0:512].rearrange("p (c t) -> p c t", c=4), [pTb[k]], [yTb[bi]])
        for cb in range(4):
            pk = cn["p"] % 4
            cn["p"] += 1
            for kc in range(8):
                mm(S, pM[pk][:], wg[:, kc, cb * 128:(cb + 1) * 128], xnT[:, kc, :], kc == 0, kc == 7, [wgb, xnTb], [pMb[pk]])
            dma(S, "sp", ht[:], hT[cb * 128:(cb + 1) * 128, gi * GT:(gi + 1) * GT], [], [htb])
            xs, x2, t3 = tmp[0], tmp[1], tmp[2]
            cp(S, "act", xs[:], pM[pk][:], [pMb[pk]], [tmpb[0]])
            tt(S, "pool", x2[:], xs[:], xs[:], ALU.mult, [tmpb[0]], [tmpb[1]])
            ts(S, "dve", x2[:], x2[:], 0.044715, 1.0, ALU.mult, ALU.add, [tmpb[1]], [tmpb[1]])
            tt(S, "dve", x2[:], x2[:], xs[:], ALU.mult, [tmpb[1], tmpb[0]], [tmpb[1]])
            act(S, t3[:], x2[:], AF.Tanh, [tmpb[1]], [tmpb[2]], scale=C0)
            ts(S, "dve", t3[:], t3[:], 0.5, 0.5, ALU.mult, ALU.add, [tmpb[2]], [tmpb[2]])
            tt(S, "pool", t3[:], t3[:], xs[:], ALU.mult, [tmpb[2], tmpb[0]], [tmpb[2]])
            tt(S, "dve", yT[2][:, cb, :], t3[:], ht[:], ALU.mult, [tmpb[2], htb], [yTb[2]])
        for cb in range(8):
            for br in range(3):
                pk = cn["p"] % 4
                cn["p"] += 1
                c0 = 512 + br * 1024 + cb * 128
                for kc in range(8):
                    mm(S, pM[pk][:], wg[:, kc, c0:c0 + 128], xnT[:, kc, :], kc == 0, kc == 7, [wgb, xnTb], [pMb[pk]])
                act(S, sg[br][:], pM[pk][:], AF.Sigmoid, [pMb[pk]], [sgb[br]])
                pk = cn["p"] % 4
                cn["p"] += 1
                for c in range(4):
                    mm(S, pM[pk][:], wp[br][:, c, cb * 128:(cb + 1) * 128], yT[br][:, c, :], c == 0, c == 3, [wpb, yTb[br]], [pMb[pk]])
                tt(S, "dve", mm_[br][:], pM[pk][:], sg[br][:], ALU.mult, [pMb[pk], sgb[br]], [mmb[br]])
            tt(S, "pool", mm_[0][:], mm_[0][:], mm_[1][:], ALU.add, [mmb[0], mmb[1]], [mmb[0]])
            tt(S, "pool", mT[:, cb, :], mm_[0][:], mm_[2][:], ALU.add, [mmb[0], mmb[2]], [mTb[cb]])
        for t in range(G):
            ti = gi * G + t
            ko = cn["x"] % 2
            cn["x"] += 1
            for half in range(2):
                k = cn["o"] % 2
                cn["o"] += 1
                for cb in range(8):
                    mm(S, pO[k][:], mT[:, cb, t * 128:(t + 1) * 128], wo[:, cb, half * 512:(half + 1) * 512],
                       cb == 0, cb == 7, [mTb[cb], wob], [pOb[k]])
                tt(S, "dve", xo[ko][:, half * 512:(half + 1) * 512], pO[k][:], xt[t][:, half * 512:(half + 1) * 512],
                   ALU.add, [pOb[k], xtb[t]], [xob[ko]])
            dma(S, "sp", x1[ti * 128:(ti + 1) * 128, :], xo[ko][:], [xob[ko]], [])
    S.finish()
    return nc


NEGM = -30000.0
NSEL = 256.0


def build_attn(SL, NT, NIT=22, do_a=True, do_b=True):
    nc = new_nc()
    NTOK = NT * 128
    NKT = SL // 128
    NB = SL // 256
    qt = {"a": nc.dram_tensor("qta", [512, NTOK], BF16, kind="ExternalInput").ap(),
          "b": nc.dram_tensor("qtb", [512, NTOK], BF16, kind="ExternalInput").ap()}
    kt = {"a": nc.dram_tensor("kta", [512, SL], BF16, kind="ExternalInput").ap(),
          "b": nc.dram_tensor("ktb", [512, SL], BF16, kind="ExternalInput").ap()}
    vv = {"a": nc.dram_tensor("va", [8, 128, NKT, 65], BF16, kind="ExternalInput").ap(),
          "b": nc.dram_tensor("vb", [8, 128, NKT, 65], BF16, kind="ExternalInput").ap()}
    qit = nc.dram_tensor("qit", [512, NTOK], BF16, kind="ExternalInput").ap()
    kit = nc.dram_tensor("kit", [64, SL], BF16, kind="ExternalInput").ap()
    wi = nc.dram_tensor("wi", [NTOK, 8], F32, kind="ExternalInput").ap()
    ksum = nc.dram_tensor("ksum", [512, NKT], F32, kind="ExternalInput").ap()
    swt = nc.dram_tensor("swt", [16, 8, 128, 128], F32, kind="ExternalInput").ap()
    b31 = nc.dram_tensor("b31", [128, 16], F32, kind="ExternalInput").ap()
    cm = nc.dram_tensor("cm", [128, 512], F32, kind="ExternalInput").ap()
    vcon = nc.dram_tensor("vcon", [NT, 3, 64], F32, kind="ExternalInput").ap()
    ident_d = nc.dram_tensor("ident", [128, 128], F32, kind="ExternalInput").ap()
    yo = {"a": nc.dram_tensor("ya", [NTOK, 512], F32, kind="ExternalOutput").ap(),
          "b": nc.dram_tensor("yb", [NTOK, 512], F32, kind="ExternalOutput").ap()}
    negB = nc.dram_tensor("negB", [NT, 128, SL], BF16, kind="Internal").ap()
    negBb = [Buf() for _ in range(NT)]
    S = Sched(nc)
    identf = S.sbuf("identf", [128, 128], F32)
    ident = S.sbuf("identh", [128, 128], BF16)
    identb = Buf()
    dma(S, "sp", identf[:], ident_d[:, :], [], [identb])
    cp(S, "dve", ident[:], identf[:], [identb], [identb])
    B31 = S.sbuf("B31", [128, 16], F32)
    CM = S.sbuf("CM", [128, 512], F32)
    WI = S.sbuf("WI", [128, NT, 8], F32)
    half = S.sbuf("half", [128, 1], F32)
    cb_ = Buf()
    dma(S, "sp", B31[:], b31[:, :], [], [cb_])
    dma(S, "sp", CM[:], cm[:, :], [], [cb_])
    dma(S, "sp", WI[:], wi.rearrange("(t p) h -> p t h", p=128), [], [cb_])
    S.op("dve", lambda e: e.memset(half[:], 0.5), [], [cb_])
    sc = S.sbuf("sc", [128, SL], F32)
    scb = Buf()
    neg = S.sbuf("neg", [128, SL], BF16)
    negb = Buf()
    if do_b:
        qim = [S.sbuf("qim%d" % i, [64, 8, 128], BF16) for i in range(2)]
        qimb = [Buf() for _ in range(2)]
        kig = [S.sbuf("kig%d" % i, [64, 512], BF16) for i in range(2)]
        kigb = [Buf() for _ in range(2)]
        dg = S.sbuf("dg", [128, 8, 128], BF16)
        dgb = Buf()
        Rr = [S.sbuf("Rr%d" % i, [128, 512], BF16) for i in range(8)]
        Rrb = [Buf() for _ in range(8)]
        st = S.sbuf("st", [128, 8], F32)
        stb = Buf()
        st2 = S.sbuf("st2", [128, 2], F32)
        st2b = Buf()
        pw2 = S.sbuf("pw2", [128, NIT], F32)
        Wd = S.sbuf("Wd", [128, NIT], F32)
        Wdb = Buf()
        for k_ in range(NIT):
            S.op("pool", lambda e, o=pw2[:, k_:k_ + 1], v=2.0 ** (-k_): e.memset(o, v), [], [cb_])
        pD = [S.psum("pD%d" % i, [128, 512], F32) for i in range(2)]
        pDb = [Buf() for _ in range(2)]
        pC = S.psum("pC", [128, 512], F32)
        pCb = Buf()
        cn = {"d": 0, "k": 0}
        for m in range(NT):
            L = (m + 1) * 512
            qk = m % 2
            dma(S, "sp", qim[qk][:], qit.rearrange("(h d) t -> d h t", d=64)[:, :, m * 128:(m + 1) * 128], [], [qimb[qk]])
            for h in range(8):
                ts(S, "dve", dg[:, h, :], ident[:], WI[:, m, h:h + 1], None, ALU.mult, None, [identb, cb_], [dgb])
            for u in range(m + 1):
                kk = cn["k"] % 2
                cn["k"] += 1
                dma(S, "sp", kig[kk][:], kit[:, u * 512:(u + 1) * 512], [], [kigb[kk]])
                for h in range(8):
                    pk = cn["d"] % 2
                    cn["d"] += 1
                    mm(S, pD[pk][:], qim[qk][:, h, :], kig[kk][:], True, True, [qimb[qk], kigb[kk]], [pDb[pk]])
                    if h % 2 == 0:
                        act(S, Rr[h][:], pD[pk][:], AF.Relu, [pDb[pk]], [Rrb[h]])
                    else:
                        ts(S, "dve", Rr[h][:], pD[pk][:], 0.0, None, ALU.max, None, [pDb[pk]], [Rrb[h]])
                for h in range(8):
                    mm(S, pC[:], dg[:, h, :], Rr[h][:], h == 0, h == 7, [dgb, Rrb[h]], [pCb])
                cp(S, "act", sc[:, u * 512:(u + 1) * 512], pC[:], [pCb], [scb])
            ts(S, "dve", neg[:, :L], sc[:, :L], 1.0, None, ALU.mult, ALU.max, [scb], [negb, stb], accum_out=st[:, 7:8])
            ts(S, "dve", neg[:, :L], sc[:, :L], -1.0, None, ALU.mult, ALU.max, [scb], [negb, stb], accum_out=st[:, 6:7])
            tt(S, "dve", st[:, 7:8], st[:, 7:8], st[:, 6:7], ALU.max, [stb], [stb])
            tt(S, "dve", sc[:, L - 512:L], sc[:, L - 512:L], CM[:], ALU.add, [scb, cb_], [scb])
            ts(S, "dve", st[:, 1:2], st[:, 7:8], 1.0001, 1e-6, ALU.mult, ALU.add, [stb], [stb])
            ts(S, "dve", st[:, 0:1], st[:, 1:2], -1.0, None, ALU.mult, None, [stb], [stb])
            ts(S, "dve", Wd[:], pw2[:], st[:, 1:2], None, ALU.mult, None, [stb, cb_], [Wdb])
            for it in range(NIT):
                ts(S, "dve", st2[:, 0:1], st[:, 0:1], Wd[:, it:it + 1], -1.0, ALU.add, ALU.mult, [stb, Wdb], [st2b])
                act(S, neg[:, :L], sc[:, :L], AF.Sign, [scb, st2b], [negb, st2b], bias=st2[:, 0:1], accum_out=st2[:, 1:2])
                ts(S, "dve", st[:, 4:5], st2[:, 1:2], 2.0 * NSEL - L - 0.5, Wd[:, it:it + 1], ALU.is_ge, ALU.mult, [st2b, Wdb], [stb])
                tt(S, "dve", st[:, 0:1], st[:, 0:1], st[:, 4:5], ALU.add, [stb], [stb])
            ts(S, "dve", neg[:, :L], sc[:, :L], st[:, 0:1], NEGM, ALU.is_lt, ALU.mult, [scb, stb], [negb])
            dma(S, "sp", negB[m, :, 0:L], neg[:, :L], [negb], [negBb[m]])
    kth = S.sbuf("kth", [64, SL], BF16)
    kthb = Buf()
    vth = S.sbuf("vth", [128, NKT, 65], BF16)
    vthb = Buf()
    qth = S.sbuf("qth", [64, NTOK], BF16)
    qthb = Buf()
    swh = S.sbuf("swh", [128, 8, 128], F32)
    swhi = S.sbuf("swhi", [128, 8, 128], BF16)
    swlo = S.sbuf("swlo", [128, 8, 128], BF16)
    swhb = Buf()
    swsb = Buf()
    PT = [S.sbuf("PT%d" % i, [128, 512], BF16) for i in range(2)]
    PTb = [Buf() for _ in range(2)]
    ksh = S.sbuf("ksh", [64, NKT], F32)
    kmf = S.sbuf("kmf", [64, NB], F32)
    kmT = S.sbuf("kmT", [64, 64], BF16)
    kmb = Buf()
    vc = S.sbuf("vc", [128, 3, 64], F32)
    vcb = Buf()
    gp = S.sbuf("gp", [128, 64], F32)
    mx8 = S.sbuf("mx8", [128, 8], F32)
    sel = S.sbuf("sel", [128, 64], F32)
    gb_ = Buf()
    rc = S.sbuf("rc", [128, 2], F32)
    yt = [S.sbuf("yt%d" % i, [128, 64], F32) for i in range(2)]
    ytb = [Buf() for _ in range(2)]
    pS = [S.psum("pS%d" % i, [128, 512], F32) for i in range(2)]
    pSb = [Buf() for _ in range(2)]
    pO = [S.psum("pO%d" % i, [128, 128], F32) for i in range(2)]
    pOb = [Buf() for _ in range(2)]
    pG = S.psum("pG", [128, 64], F32)
    pGb = Buf()
    c2 = {"s": 0, "o": 0, "y": 0, "n": 0}
    neg2 = sc[:].bitcast(BF16)
    negs = [(neg, negb), (neg2, scb)]
    S.op("pool", lambda e: e.memset(kmT[:], 0.0), [], [kmb])
    heads = ([("b", h) for h in range(8)] if do_b else []) + ([("a", h) for h in range(8)] if do_a else [])
    for br, h in heads:
        hh = h if br == "a" else 8 + h
        dma(S, "sp", kth[:], kt[br][h * 64:(h + 1) * 64, :], [], [kthb])
        dma(S, "sp", vth[:], vv[br][h], [], [vthb])
        dma(S, "sp", qth[:], qt[br][h * 64:(h + 1) * 64, :], [], [qthb])
        dma(S, "sp", swh[:], swt[hh].rearrange("p k q -> k p q"), [], [swhb])
        cp(S, "pool", swhi[:], swh[:], [swhb], [swsb])
        tt(S, "pool", swlo[:], swh[:], swhi[:], ALU.subtract, [swhb, swsb], [swsb])
        if br == "a":
            dma(S, "sp", ksh[:], ksum[h * 64:(h + 1) * 64, :], [], [kmb])
            v2 = ksh[:].rearrange("d (n two) -> d n two", two=2)
            tt(S, "dve", kmf[:], v2[:, :, 0], v2[:, :, 1], ALU.add, [kmb], [kmb])
            ts(S, "dve", kmT[:, :NB], kmf[:], 1.0 / 256.0, None, ALU.mult, None, [kmb], [kmb])
        for m in range(NT):
            L = (m + 1) * 512
            qs = qth[:, m * 128:(m + 1) * 128]
            neg, negb = negs[c2["n"] % 2]
            c2["n"] += 1
            if br == "b":
                dma(S, "sp", neg[:, :L], negB[m, :, 0:L], [negBb[m]], [negb])
            else:
                dma(S, "sp", vc[:], vcon[m:m + 1].partition_broadcast(128), [], [vcb])
                mm(S, pG[:], qs, kmT[:], True, True, [qthb, kmb], [pGb])
                tt(S, "dve", gp[:], pG[:], vc[:, 0, :], ALU.add, [pGb, vcb], [gb_])
                S.op("dve", lambda e: e.max(out=mx8[:], in_=gp[:]), [gb_], [gb_])
                ts(S, "dve", sel[:], gp[:], mx8[:, 2:3], None, ALU.is_ge, None, [gb_], [gb_])
                tt(S, "dve", sel[:], sel[:], vc[:, 1, :], ALU.mult, [gb_, vcb], [gb_])
                tt(S, "dve", sel[:], sel[:], vc[:, 2, :], ALU.max, [gb_, vcb], [gb_])
                ts(S, "dve", sel[:], sel[:], -1.0, -NEGM, ALU.add, ALU.mult, [gb_], [gb_])
                nb = L // 256
                cp(S, "dve", neg[:, :L].rearrange("p (n k) -> p n k", k=256),
                   sel[:, :nb].unsqueeze(2).to_broadcast([128, nb, 256]), [gb_], [negb])
            ok = c2["o"] % 2
            c2["o"] += 1

            def emit_qk(u, sk):
                win = u >= m - 1
                for tj in range(4):
                    ktile = 4 * u + tj
                    o_ = pS[sk][:, tj * 128:(tj + 1) * 128]
                    mm(S, o_, kth[:, ktile * 128:(ktile + 1) * 128], qs, True, False, [kthb, qthb], [pSb[sk]])
                    mm(S, o_, neg[:, ktile * 128:(ktile + 1) * 128], ident[:], False, not win, [negb, identb], [pSb[sk]])
                    if win:
                        p = (u - (m - 1)) * 4 + tj
                        mm(S, o_, ident[:], swhi[:, p, :], False, False, [identb, swsb], [pSb[sk]])
                        mm(S, o_, ident[:], swlo[:, p, :], False, True, [identb, swsb], [pSb[sk]])

            sks = []
            for u in range(m + 1):
                sks.append(c2["s"] % 2)
                c2["s"] += 1
            emit_qk(0, sks[0])
            for u in range(m + 1):
                win = u >= m - 1
                sk = sks[u]
                if win:
                    act(S, PT[sk][:], pS[sk][:], AF.Exp, [pSb[sk]], [PTb[sk]])
                else:
                    act(S, PT[sk][:], pS[sk][:], AF.Exp, [pSb[sk], cb_], [PTb[sk]], bias=B31[:, hh:hh + 1])
                if u + 1 <= m:
                    emit_qk(u + 1, sks[u + 1])
                for tj in range(4):
                    ktile = 4 * u + tj
                    mm(S, pO[ok][:, 0:65], PT[sk][:, tj * 128:(tj + 1) * 128], vth[:, ktile, :],
                       u == 0 and tj == 0, u == m and tj == 3, [PTb[sk], vthb], [pOb[ok]])
            yk = c2["y"] % 2
            c2["y"] += 1
            S.op("dve", lambda e, o=rc[:, 0:1], i=pO[ok][:, 64:65]: e.reciprocal(o, i), [pOb[ok]], [gb_])
            ts(S, "dve", yt[yk][:], pO[ok][:, 0:64], rc[:, 0:1], None, ALU.mult, None, [pOb[ok], gb_], [ytb[yk]])
            dma(S, "sp", yo[br][m * 128:(m + 1) * 128, h * 64:(h + 1) * 64], yt[yk][:], [ytb[yk]], [])
    S.finish()
    return nc

N_BUCKETS = 32
MAX_DISTANCE = 128


def t5_bucket_np(dist):
    n = np.maximum(dist, 0)
    max_exact = N_BUCKETS // 2
    nf = np.maximum(n, 1).astype(np.float32)
    large = max_exact + (np.log(nf / np.float32(max_exact)) / np.float32(math.log(MAX_DISTANCE / max_exact))
                         * np.float32(N_BUCKETS - max_exact)).astype(np.int32)
    large = np.minimum(large, N_BUCKETS - 1)
    return np.where(n < max_exact, n, large)


def attn_consts(j, NT, rel_bias):
    k = np.arange(128)[:, None]
    q = np.arange(128)[None, :]
    swt = np.empty((16, 8, 128, 128), np.float32)
    for p in range(8):
        delta = 4 + j - p
        d = delta * 128 + q - k
        bk = t5_bucket_np(d)
        for hh in range(16):
            swt[hh, p] = np.where(d >= 0, rel_bias[bk, hh], np.float32(-30000.0))
    b31 = np.broadcast_to(rel_bias[31][None, :], (128, 16)).astype(np.float32).copy()
    qq = np.arange(128)[:, None]
    c = np.arange(512)[None, :]
    ktile = c // 128
    cm = np.where((ktile < j) | ((ktile == j) & ((c % 128) <= qq)), np.float32(0.0), np.float32(-1e30)).astype(np.float32)
    vcon = np.zeros((NT, 3, 64), np.float32)
    n = np.arange(64)
    for m in range(NT):
        cur = (4 * m + j) // 2
        vcon[m, 0] = np.where(n < cur, 0.0, -30000.0)
        vcon[m, 1] = (n < cur).astype(np.float32)
        vcon[m, 2] = (n == cur).astype(np.float32)
    return {"swt": swt, "b31": b31, "cm": cm, "vcon": vcon, "ident": np.eye(128, dtype=np.float32)}


def own_tokens(j, NT):
    return (np.arange(NT)[:, None] * 4 + j)[:, :, None].reshape(NT, 1) * 128 + np.arange(128)[None, :]


import ml_dtypes

_PROGS = {}


def _prog(key, fn):
    if key not in _PROGS:
        _PROGS[key] = fn()
    return _PROGS[key]


def _run(nc, in_maps):
    res = run_bass_kernel_spmd(nc, in_maps, core_ids=list(range(8)))
    return res.results


def _blockdiag(w2):
    m = np.zeros((128, 128), np.float32)
    m[:64, :64] = w2[0]
    m[64:, 64:] = w2[1]
    return m


def kernel(x, rel_bias, norm_mix_g, w_in, conv_w, conv_b, w_r, b_r, w_i, b_i, lru_lambda,
           w_pa, w_pb, w_pc, w_o, norm_mlp_g, w_up, w_down, final_norm_g):
    f32 = np.float32
    x = np.asarray(x, f32)
    B, SL, _ = x.shape
    depth = int(np.asarray(w_in).shape[0])
    assert B == 2 and SL % 512 == 0
    NT = SL // 512
    NKT = SL // 128
    NTOK = NT * 128
    rel_bias = np.asarray(rel_bias, f32)
    ident = np.eye(128, dtype=f32)
    toks = [own_tokens(j, NT).reshape(-1) for j in range(4)]
    consts = [attn_consts(j, NT, rel_bias) for j in range(4)]
    x_own = [np.ascontiguousarray(x[c // 4][toks[c % 4]]) for c in range(8)]
    p_proj = _prog(("proj", NT), lambda: build_proj(NT))
    p_attn = _prog(("attn", SL, NT), lambda: build_attn(SL, NT))
    p_rg = _prog(("rg", SL), lambda: build_rglru(SL))
    p_tail = _prog(("tail", NT), lambda: build_tail(NT))
    gfin = np.asarray(final_norm_g, f32).reshape(1, -1)
    for l in range(depth):
        wl = np.ascontiguousarray(np.asarray(w_in[l], f32))
        gmix = np.asarray(norm_mix_g[l], f32).reshape(1, -1)
        r1 = _run(p_proj, [{"x": x_own[c], "g": gmix, "w_in": wl, "ident": ident} for c in range(8)])
        shared = []
        for b in range(2):
            def full_cols(name, rows, dt):
                a = np.empty((rows, SL), dt)
                for j in range(4):
                    a[:, toks[j]] = np.asarray(r1[b * 4 + j][name])[:rows]
                return a
            def full_rows(name, cols, dt):
                a = np.empty((SL, cols), dt)
                for j in range(4):
                    a[toks[j]] = np.asarray(r1[b * 4 + j][name])
                return a
            def vlay(v):
                return np.ascontiguousarray(v.reshape(NKT, 128, 8, 65).transpose(2, 1, 0, 3))
            bf = ml_dtypes.bfloat16
            ks = np.empty((512, NKT), f32)
            for j in range(4):
                ks[:, j::4] = np.asarray(r1[b * 4 + j]["o_ks"])
            shared.append({
                "kta": full_cols("o_ka", 512, bf), "ktb": full_cols("o_kb", 512, bf), "kit": full_cols("o_ki", 64, bf),
                "va": vlay(full_rows("o_va", 520, bf)), "vb": vlay(full_rows("o_vb", 520, bf)),
                "ksum": ks, "xc": full_cols("o_xc", 512, f32)})
        im = []
        for c in range(8):
            b, j = c // 4, c % 4
            d = dict(consts[j])
            sh = shared[b]
            d.update({"qta": np.asarray(r1[c]["o_qa"]), "qtb": np.asarray(r1[c]["o_qb"]), "qit": np.asarray(r1[c]["o_qi"]),
                      "wi": np.asarray(r1[c]["o_wi"]), "kta": sh["kta"], "ktb": sh["ktb"], "kit": sh["kit"],
                      "va": sh["va"], "vb": sh["vb"], "ksum": sh["ksum"]})
            im.append(d)
        r2 = _run(p_attn, im)
        im = []
        for c in range(8):
            b, cc = c // 4, c % 4
            ch = slice(cc * 128, (cc + 1) * 128)
            prm = np.stack([np.asarray(conv_w[l][0], f32)[ch], np.asarray(conv_w[l][1], f32)[ch],
                            np.asarray(conv_w[l][2], f32)[ch], np.asarray(conv_w[l][3], f32)[ch],
                            np.asarray(conv_b[l], f32)[ch], np.asarray(b_r[l], f32)[ch], np.asarray(b_i[l], f32)[ch],
                            np.asarray(lru_lambda[l], f32)[ch]], axis=1)
            im.append({"xc": np.ascontiguousarray(shared[b]["xc"][ch]), "prm": np.ascontiguousarray(prm),
                       "wr": _blockdiag(np.asarray(w_r[l], f32)[2 * cc:2 * cc + 2]),
                       "wi": _blockdiag(np.asarray(w_i[l], f32)[2 * cc:2 * cc + 2])})
        r3 = _run(p_rg, im)
        hfull = [np.concatenate([np.asarray(r3[b * 4 + cc]["h"]) for cc in range(4)], axis=0) for b in range(2)]
        im = []
        for c in range(8):
            b, j = c // 4, c % 4
            im.append({"x": x_own[c], "g": gmix, "w_in": wl, "ya": np.asarray(r2[c]["ya"]), "yb": np.asarray(r2[c]["yb"]),
                       "hT": np.ascontiguousarray(hfull[b][:, toks[j]]),
                       "w_pa": np.asarray(w_pa[l], f32), "w_pb": np.asarray(w_pb[l], f32), "w_pc": np.asarray(w_pc[l], f32),
                       "w_o": np.asarray(w_o[l], f32), "ident": ident})
        r4 = _run(p_tail, im)
        final = l == depth - 1
        p_mlp = _prog(("mlp", NT, final), lambda: build_mlp(NT, final))
        gm = np.asarray(norm_mlp_g[l], f32).reshape(1, -1)
        r5 = _run(p_mlp, [{"x1": np.asarray(r4[c]["x1"]), "g": gm, "gf": gfin, "w_up": np.asarray(w_up[l], f32),
                           "w_dn": np.asarray(w_down[l], f32), "ident": ident} for c in range(8)])
        x_own = [np.asarray(r5[c]["x2"]) for c in range(8)]
    out = np.empty((B, SL, x.shape[2]), f32)
    for c in range(8):
        out[c // 4][toks[c % 4]] = x_own[c]
    return out
```
